# Optimizing a Trainium2 kernel written in Bass

```python
import jax, jax.numpy as jnp
from jax import lax
import numpy as np

D_MODEL = 1024
BATCH = 8
SEQ = 2048
DEPTH = 1

MIX_WIDTH = D_MODEL
HEAD_DIM = 64
ATTN_WIDTH = MIX_WIDTH // 2
N_Q_HEADS = ATTN_WIDTH // HEAD_DIM
N_KV_HEADS = 2
KV_GROUP = N_Q_HEADS // N_KV_HEADS
KV_WIDTH = N_KV_HEADS * HEAD_DIM
WINDOW = 128
ATTN_BLOCK = 128
LRU_WIDTH = MIX_WIDTH - ATTN_WIDTH
LRU_BLOCKS = 8
LRU_BLOCK_W = LRU_WIDTH // LRU_BLOCKS
CONV_WIDTH = 4
LRU_C = 8.0
IN_SPLITS = [ATTN_WIDTH, ATTN_WIDTH + KV_WIDTH, ATTN_WIDTH + 2 * KV_WIDTH, ATTN_WIDTH + 2 * KV_WIDTH + LRU_WIDTH]
IN_COLS = ATTN_WIDTH + 2 * KV_WIDTH + 2 * LRU_WIDTH
N_EXPERTS = 32
TOP_K = 4
D_EXPERT = D_MODEL
SWIGLU_LIMIT = 7.0
SWIGLU_ALPHA = 1.702
MOE_BLOCK = 256
EPS = 1e-6

kernel_name = "hymba_griffin_swa_sink_moe_adaln"


def rms_norm(x, g):
    xf = x.astype(jnp.float32)
    y = xf * lax.rsqrt(jnp.mean(xf * xf, axis=-1, keepdims=True) + EPS)
    return (y * g.astype(jnp.float32)).astype(x.dtype)


def causal_depthwise_conv(x, w, b):
    y = lax.conv_general_dilated(
        x, w[:, None, :].astype(x.dtype), window_strides=(1,),
        padding=[(CONV_WIDTH - 1, 0)], dimension_numbers=("NWC", "WIO", "NWC"),
        feature_group_count=x.shape[-1])
    return y + b


def rg_lru(x, w_a, b_a, w_x, b_x, lam):
    B, S, C = x.shape
    xf = x.astype(jnp.float32)
    xh = xf.reshape(B, S, LRU_BLOCKS, LRU_BLOCK_W)
    r = jax.nn.sigmoid(jnp.einsum("bshi,hij->bshj", xh, w_a.astype(jnp.float32)) + b_a).reshape(B, S, C)
    i = jax.nn.sigmoid(jnp.einsum("bshi,hij->bshj", xh, w_x.astype(jnp.float32)) + b_x).reshape(B, S, C)
    log_a = LRU_C * r * jax.nn.log_sigmoid(lam.astype(jnp.float32))
    a = jnp.exp(log_a)
    mult = jnp.sqrt(jnp.maximum(-jnp.expm1(2.0 * log_a), 0.0))
    u = mult * (i * xf)

    def combine(left, right):
        a1, b1 = left
        a2, b2 = right
        return a1 * a2, a2 * b1 + b2

    _, h = lax.associative_scan(combine, (a, u), axis=1)
    return h.astype(x.dtype)


def sliding_window_sink_attention(q, k, v, sinks):
    B, S = q.shape[0], q.shape[1]
    nblk = S // ATTN_BLOCK
    qb = q.reshape(B, nblk, ATTN_BLOCK, N_KV_HEADS, KV_GROUP, HEAD_DIM)

    def band(t):
        tp = jnp.pad(t, ((0, 0), (ATTN_BLOCK, 0), (0, 0), (0, 0)))
        prev = tp[:, :S].reshape(B, nblk, ATTN_BLOCK, N_KV_HEADS, HEAD_DIM)
        cur = t.reshape(B, nblk, ATTN_BLOCK, N_KV_HEADS, HEAD_DIM)
        return jnp.concatenate([prev, cur], axis=2)

    kw, vw = band(k), band(v)
    scores = jnp.einsum("bnqkgd,bnskd->bnkgqs", qb, kw).astype(jnp.float32) * (HEAD_DIM ** -0.5)
    qi = jnp.arange(ATTN_BLOCK)[:, None]
    sj = jnp.arange(2 * ATTN_BLOCK)[None, :]
    rel = qi + ATTN_BLOCK - sj
    key_pos = jnp.arange(nblk)[:, None, None] * ATTN_BLOCK - ATTN_BLOCK + sj[None]
    mask = ((rel >= 0) & (rel < WINDOW))[None] & (key_pos >= 0)
    scores = jnp.where(mask[None, :, None, None], scores, jnp.finfo(jnp.float32).min)
    sink = jnp.broadcast_to(sinks.astype(jnp.float32).reshape(1, 1, N_KV_HEADS, KV_GROUP, 1, 1),
                            scores.shape[:-1] + (1,))
    probs = jax.nn.softmax(jnp.concatenate([scores, sink], axis=-1), axis=-1)[..., :-1]
    out = jnp.einsum("bnkgqs,bnskd->bnqkgd", probs.astype(v.dtype), vw)
    return out.reshape(B, S, N_Q_HEADS * HEAD_DIM)


def parallel_mixer(h, w_in, conv_w, conv_b, w_rg_a, b_rg_a, w_rg_x, b_rg_x, lru_lambda,
                   sinks, g_attn_out, g_lru_out, w_out):
    B, S, _ = h.shape
    p = h @ w_in
    q, k, v, xr, xg = jnp.split(p, IN_SPLITS, axis=-1)
    attn = sliding_window_sink_attention(
        q.reshape(B, S, N_Q_HEADS, HEAD_DIM), k.reshape(B, S, N_KV_HEADS, HEAD_DIM),
        v.reshape(B, S, N_KV_HEADS, HEAD_DIM), sinks)
    xc = causal_depthwise_conv(xr, conv_w, conv_b)
    lru = rg_lru(xc, w_rg_a, b_rg_a, w_rg_x, b_rg_x, lru_lambda) * jax.nn.gelu(xg)
    merged = jnp.concatenate([rms_norm(attn, g_attn_out), rms_norm(lru, g_lru_out)], axis=-1)
    return merged @ w_out


def moe_ffn(h, w_router, b_router, w_gu, b_gu, w_down, b_down):
    B, S, D = h.shape
    T = B * S
    A = T * TOP_K
    R = ((A + N_EXPERTS * (MOE_BLOCK - 1) + MOE_BLOCK - 1) // MOE_BLOCK) * MOE_BLOCK
    nb = R // MOE_BLOCK
    hf = h.reshape(T, D)
    logits = (hf @ w_router + b_router).astype(jnp.float32)
    top_vals, top_idx = lax.top_k(logits, TOP_K)
    gates = jax.nn.softmax(top_vals, axis=-1)
    flat_e = top_idx.reshape(A).astype(jnp.int32)
    flat_tok = jnp.repeat(jnp.arange(T, dtype=jnp.int32), TOP_K)
    flat_w = gates.reshape(A)
    order = jnp.argsort(flat_e)
    se = flat_e[order]
    sizes = jnp.bincount(flat_e, length=N_EXPERTS)
    padded = ((sizes + MOE_BLOCK - 1) // MOE_BLOCK) * MOE_BLOCK
    pad_end = jnp.cumsum(padded)
    pad_start = pad_end - padded
    start = jnp.cumsum(sizes) - sizes
    dest = pad_start[se] + (jnp.arange(A, dtype=jnp.int32) - start[se])
    row_tok = jnp.zeros((R,), jnp.int32).at[dest].set(flat_tok[order])
    row_w = jnp.zeros((R,), jnp.float32).at[dest].set(flat_w[order])
    blk_e = jnp.minimum(jnp.searchsorted(pad_end, jnp.arange(nb) * MOE_BLOCK, side="right"),
                        N_EXPERTS - 1).astype(jnp.int32)
    xs = hf[row_tok].reshape(nb, MOE_BLOCK, D)

    def expert_block(args):
        xb, e = args
        gu = xb @ w_gu[e] + b_gu[e]
        gate = jnp.minimum(gu[:, :D_EXPERT], SWIGLU_LIMIT)
        up = jnp.clip(gu[:, D_EXPERT:], -SWIGLU_LIMIT, SWIGLU_LIMIT)
        glu = gate * jax.nn.sigmoid(SWIGLU_ALPHA * gate)
        return ((up + 1.0) * glu) @ w_down[e] + b_down[e]

    ys = lax.map(expert_block, (xs, blk_e)).reshape(R, D)
    ys = ys.astype(jnp.float32) * row_w[:, None]
    out = jax.ops.segment_sum(ys, row_tok, num_segments=T)
    return out.reshape(B, S, D).astype(h.dtype)


def setup_inputs(seed: int = 0) -> dict:
    key = jax.random.key(seed)
    ks = jax.random.split(key, 26)
    L, D, E, F = DEPTH, D_MODEL, N_EXPERTS, D_EXPERT
    nrm = lambda k, shape, s: jax.random.normal(k, shape, jnp.float32) * s
    a0 = jax.random.uniform(ks[13], (L, LRU_WIDTH), jnp.float32, minval=0.9, maxval=0.999)
    root = a0 ** (1.0 / LRU_C)
    return {
        "x": nrm(ks[0], (BATCH, SEQ, D), 1.0),
        "c": nrm(ks[1], (BATCH, D), 1.0),
        "w_ada": nrm(ks[2], (L, D, 6 * D), 0.5 * D ** -0.5),
        "b_ada": nrm(ks[3], (L, 6 * D), 0.02),
        "g_mix": 1.0 + nrm(ks[4], (L, D), 0.05),
        "w_in": nrm(ks[5], (L, D, IN_COLS), D ** -0.5),
        "conv_w": nrm(ks[6], (L, CONV_WIDTH, LRU_WIDTH), CONV_WIDTH ** -0.5),
        "conv_b": nrm(ks[7], (L, LRU_WIDTH), 0.02),
        "w_rg_a": nrm(ks[8], (L, LRU_BLOCKS, LRU_BLOCK_W, LRU_BLOCK_W), LRU_BLOCK_W ** -0.5),
        "b_rg_a": nrm(ks[9], (L, LRU_BLOCKS, LRU_BLOCK_W), 0.02),
        "w_rg_x": nrm(ks[10], (L, LRU_BLOCKS, LRU_BLOCK_W, LRU_BLOCK_W), LRU_BLOCK_W ** -0.5),
        "b_rg_x": nrm(ks[11], (L, LRU_BLOCKS, LRU_BLOCK_W), 0.02),
        "lru_lambda": jnp.log(root) - jnp.log1p(-root),
        "sinks": nrm(ks[12], (L, N_Q_HEADS), 0.5),
        "g_attn_out": 1.0 + nrm(ks[14], (L, ATTN_WIDTH), 0.05),
        "g_lru_out": 1.0 + nrm(ks[15], (L, LRU_WIDTH), 0.05),
        "w_out": nrm(ks[16], (L, MIX_WIDTH, D), MIX_WIDTH ** -0.5),
        "g_ffn": 1.0 + nrm(ks[17], (L, D), 0.05),
        "w_router": nrm(ks[18], (L, D, E), D ** -0.5),
        "b_router": nrm(ks[19], (L, E), 0.01),
        "w_gu": nrm(ks[20], (L, E, D, 2 * F), D ** -0.5),
        "b_gu": nrm(ks[21], (L, E, 2 * F), 0.02),
        "w_down": nrm(ks[22], (L, E, F, D), F ** -0.5),
        "b_down": nrm(ks[23], (L, E, D), 0.02),
        "g_final": 1.0 + nrm(ks[24], (D,), 0.05),
    }


def reference(x, c, w_ada, b_ada, g_mix, w_in, conv_w, conv_b, w_rg_a, b_rg_a, w_rg_x, b_rg_x,
              lru_lambda, sinks, g_attn_out, g_lru_out, w_out, g_ffn, w_router, b_router,
              w_gu, b_gu, w_down, b_down, g_final):
    for l in range(DEPTH):
        mod = (c @ w_ada[l] + b_ada[l])[:, None, :]
        sh1, sc1, gt1, sh2, sc2, gt2 = jnp.split(mod, 6, axis=-1)
        h = rms_norm(x, g_mix[l]) * (1.0 + sc1) + sh1
        x = x + gt1 * parallel_mixer(h, w_in[l], conv_w[l], conv_b[l], w_rg_a[l], b_rg_a[l],
                                     w_rg_x[l], b_rg_x[l], lru_lambda[l], sinks[l],
                                     g_attn_out[l], g_lru_out[l], w_out[l])
        h = rms_norm(x, g_ffn[l]) * (1.0 + sc2) + sh2
        x = x + gt2 * moe_ffn(h, w_router[l], b_router[l], w_gu[l], b_gu[l], w_down[l], b_down[l])
    return rms_norm(x, g_final)
```

```python
import numpy as np
import concourse.bass as bass
import concourse.mybir as mybir
from concourse.bass_utils import run_bass_kernel_spmd
from contextlib import ExitStack

F32 = mybir.dt.float32
F32R = mybir.dt.float32r
U32 = mybir.dt.uint32
I32 = mybir.dt.int32
ALU = mybir.AluOpType
AF = mybir.ActivationFunctionType

S = 2048
D = 1024
NT = 16
E = 32
EREG = 2048
BSZ = 256
NJ = BSZ // 128
NB = S * 4 // BSZ + E
NSLOT = E * EREG
BIG = 1000000.0
EPS = 1e-6
NEG = -30000.0

ENGS = ("tensor", "vector", "scalar", "gpsimd", "sync")
NDS = 40
SAME_ENG_SYNC = True

C_ID, C_ONE, C_U, C_MD, C_MP = 0, 128, 256, 384, 896
C_IOTA, C_RB, C_BR, C_SINK = 1408, 1440, 1472, 1504
C_CW, C_CB, C_BA, C_BX, C_LAM, C_GA, C_GL = 1512, 1528, 1532, 1536, 1540, 1544, 1548
C_PID, C_J = 1552, 1560
NCONST = 1624
ARENA = 52352


class T:
    __slots__ = ("w", "r")

    def __init__(self):
        self.w = None
        self.r = {}


class Prog:
    def __init__(self, nc, es):
        self.nc = nc
        self.q = {e: [] for e in ENGS}
        self.sem = {e: es.enter_context(nc.semaphore("sem_" + e)) for e in ENGS[:4]}
        self.cnt = {e: 0 for e in ENGS}
        self.dsem = [es.enter_context(nc.semaphore(f"dsem{i}")) for i in range(NDS)]
        self.dcnt = [0] * NDS
        self.dnext = 0
        self.seen = {}

    def _need(self, eng, key, val):
        if key == ("e", eng) and not SAME_ENG_SYNC:
            return
        if self.seen.get((eng, key), 0) >= val:
            return
        self.seen[(eng, key)] = val
        self.q[eng].append(("wait", key, val))

    def _deps(self, eng, reads, writes):
        for t in reads:
            if t.w:
                self._need(eng, *t.w)
        for t in writes:
            if t.w:
                self._need(eng, *t.w)
            for k, v in t.r.items():
                self._need(eng, k, v)

    def _mark(self, ev, reads, writes):
        for t in reads:
            if t.r.get(ev[0], 0) < ev[1]:
                t.r[ev[0]] = ev[1]
        for t in writes:
            t.w = ev
            t.r = {}

    def op(self, eng, fn, reads=(), writes=()):
        self._deps(eng, reads, writes)
        self.cnt[eng] += 1
        ev = (("e", eng), self.cnt[eng])
        self.q[eng].append(("op", fn))
        self._mark(ev, reads, writes)

    def dma(self, eng, fn, reads=(), writes=()):
        i = self.dnext
        self.dnext = (i + 1) % NDS
        key = ("d", i)
        if self.dcnt[i] > 0:
            self._need(eng, key, self.dcnt[i])
        self._deps(eng, reads, writes)
        self.dcnt[i] += 16
        ev = (key, self.dcnt[i])
        self.q[eng].append(("dma", fn, i))
        self._mark(ev, reads, writes)

    def barrier(self):
        for eng in ENGS:
            for i in range(NDS):
                if self.dcnt[i] > 0:
                    self._need(eng, ("d", i), self.dcnt[i])
            for e in ENGS[:4]:
                if self.cnt[e] > 0 and e != eng:
                    self._need(eng, ("e", e), self.cnt[e])

    def _semobj(self, key):
        return self.sem[key[1]] if key[0] == "e" else self.dsem[key[1]]

    def emit(self):
        with self.nc.Block() as block:
            for e in ENGS:
                if not self.q[e]:
                    continue

                def body(engh, e=e):
                    for item in self.q[e]:
                        if item[0] == "wait":
                            engh.wait_ge(self._semobj(item[1]), item[2])
                        elif item[0] == "op":
                            item[1](engh).then_inc(self.sem[e], 1)
                        else:
                            item[1](engh).then_inc(self.dsem[item[2]], 16)

                getattr(block, e)(body)


def build(debug=False, stop_after=99):
    nc = bass.Bass("TRN2", target_bir_lowering=False)

    def din(name, shape, dtype=F32):
        return nc.dram_tensor(name, shape, dtype, kind="ExternalInput").ap()

    skind = "ExternalOutput" if debug else "Internal"
    x_d = din("x", [S, D])
    cB_d = din("cB", [128, 8 * 128])
    wada_d = din("w_ada", [D, 6 * D])
    bada_d = din("b_ada_b", [128, 6 * D])
    gmix_d = din("g_mix_b", [128, D])
    gffn_d = din("g_ffn_b", [128, D])
    gfin_d = din("g_final_b", [128, D])
    win_d = din("w_in", [D, 1792])
    bda_d = din("bd_a", [128, 4 * 128])
    bdx_d = din("bd_x", [128, 4 * 128])
    wout_d = din("w_out", [D, D])
    wr_d = din("w_router", [D, E])
    wgur_d = din("w_gu_r", [E * 1024, 2048])
    wdnr_d = din("w_dn_r", [E * 512, 2048])
    bgur_d = din("b_gu_r", [E * 128, 16])
    bdn_d = din("b_down", [E, D])
    const_d = din("consts", [128, NCONST])
    out_d = nc.dram_tensor("out", [S, D], F32, kind="ExternalOutput").ap()
    x1_d = nc.dram_tensor("x1_s", [S, D], F32, kind=skind).ap()
    xs_d = nc.dram_tensor("xs_s", [NSLOT, D], F32, kind=skind).ap()
    ys_d = nc.dram_tensor("ys_s", [NSLOT, D], F32, kind=skind).ap()
    gt2_d = nc.dram_tensor("gt2_s", [128, D], F32, kind=skind).ap()
    dbg_d = nc.dram_tensor("dbg", [128, 8 * S], F32, kind="ExternalOutput").ap() if debug else None

    es = ExitStack()
    with es:
        P = Prog(nc, es)
        AR = es.enter_context(nc.sbuf_tensor("arena", [128, ARENA], F32))
        pbank = [es.enter_context(nc.psum_tensor(f"pb{i}", [128, 512], F32)) for i in range(8)]
        pT = [T() for _ in range(8)]
        pstate = [0]

        def bank():
            i = pstate[0]
            pstate[0] = (i + 1) % 8
            return pbank[i], pT[i]

        ar_addr = [m.memorylocations[0].addr for m in nc.allocations if m.name == "arena_set"][0]
        vcount = [0]

        def V(off, n, dt=F32):
            assert off + n <= ARENA
            vcount[0] += 1
            t = nc.alloc_sbuf_tensor_at(f"v{vcount[0]}", [128, n], dt, offset=ar_addr + off * 4)
            return t[:, :]

        def V3(off, a, b, dt=F32):
            return V(off, a * b, dt).rearrange("p (a b) -> p a b", a=a)

        def mm(out, lhsT, rhs, start, stop, reads, writes):
            P.op("tensor", lambda e: e.matmul(out, lhsT=lhsT, rhs=rhs, start=start, stop=stop), reads, writes)

        def tr(out, in_, ident, reads, writes):
            P.op("tensor", lambda e: e.transpose(out=out, in_=in_, identity=ident), reads, writes)

        def act(out, in_, func, reads, writes, **kw):
            P.op("scalar", lambda e: e.activation(out=out, in_=in_, func=func, **kw), reads, writes)

        def tt(eng, out, in0, in1, op, reads, writes):
            P.op(eng, lambda e: e.tensor_tensor(out=out, in0=in0, in1=in1, op=op), reads, writes)

        def ts(eng, out, in0, s1, s2, op0, op1, reads, writes, **kw):
            if op1 is None:
                P.op(eng, lambda e: e.tensor_scalar(out=out, in0=in0, scalar1=s1, scalar2=None, op0=op0, **kw), reads, writes)
            else:
                P.op(eng, lambda e: e.tensor_scalar(out=out, in0=in0, scalar1=s1, scalar2=s2, op0=op0, op1=op1, **kw), reads, writes)

        def stt(out, in0, scalar, in1, op0, op1, reads, writes, **kw):
            P.op("vector", lambda e: e.scalar_tensor_tensor(out=out, in0=in0, scalar=scalar, in1=in1, op0=op0, op1=op1, **kw), reads, writes)

        def cp(eng, out, in_, reads, writes):
            if eng == "scalar":
                P.op("scalar", lambda e: e.copy(out=out, in_=in_), reads, writes)
            else:
                P.op(eng, lambda e: e.tensor_copy(out=out, in_=in_), reads, writes)

        def dma(eng, out, in_, reads, writes):
            P.dma(eng, lambda e: e.dma_start(out=out, in_=in_), reads, writes)

        CO = 0
        CR = 600
        SM = 1752
        MOD = 2112
        PH = 8256
        cT = T()
        crT = T()
        cot = V(CO, 600)
        crt = V(CR, 1152, F32R)

        def C(off, n):
            o = off if off < 384 else 384 + off - C_IOTA
            return cot[:, o:o + n]

        dma("sync", cot[:, 0:384], const_d[:, 0:384], [], [cT])
        dma("sync", cot[:, 384:600], const_d[:, C_IOTA:NCONST], [], [cT])
        dma("gpsimd", crt[:, 0:128], const_d[:, C_ID:C_ID + 128], [], [crT])
        dma("gpsimd", crt[:, 128:1152], const_d[:, C_MD:C_MD + 1024], [], [crT])
        identF = C(C_ID, 128)
        onesF = C(C_ONE, 128)
        UF = C(C_U, 128)
        identR = crt[:, 0:128]
        maskDR = crt[:, 128:640]
        maskPR = crt[:, 640:1152]
        sm = [SM]

        def small(n):
            o = sm[0]
            sm[0] += (n + 7) // 8 * 8
            assert sm[0] <= MOD
            return V(o, n), T()

        sinkexp, sinkexpT = small(8)
        sp8, sp8T = small(4)
        ls8, ls8T = small(4)
        gates_all, gatesT = small(64)
        destf_all, destfT = small(64)
        desti_o = sm[0]; sm[0] += 64
        desti_all = V(desti_o, 64, I32); destiT = T()
        cum, cumT = small(32)

        modrow = V(MOD, 6 * D)
        modT = [T() for _ in range(6)]
        msl = lambda j: modrow[:, j * D:(j + 1) * D]
        sh1b, s1b, gt1b, sh2b, s2b, gt2b = [msl(j) for j in range(6)]

        cBs = V3(PH, 8, 128, F32R); cBT = T()
        dma("gpsimd", cBs, cB_d.rearrange("p (a b) -> p a b", a=8), [], [cBT])
        for j in range(6):
            dma("sync", msl(j), bada_d[:, j * D:(j + 1) * D], [], [modT[j]])
        WA = [V3(PH + 1024 + k * 4096, 8, 512, F32R) for k in range(2)]
        WAT = [T(), T()]
        wada_v = wada_d.rearrange("(kc p) n -> p kc n", p=128)
        for g in range(12):
            k = g % 2
            dma("gpsimd", WA[k], wada_v[:, :, g * 512:(g + 1) * 512], [], [WAT[k]])
            pb, pt = bank()
            for kc in range(8):
                mm(pb[:, :], cBs[:, kc, :], WA[k][:, kc, :], kc == 0, kc == 7, [cBT, WAT[k]], [pt])
            mt = modT[g // 2]
            sl = modrow[:, g * 512:(g + 1) * 512]
            tt("vector", sl, pb[:, :], sl, ALU.add, [pt, mt], [mt])
        gtmp = V(PH + 1024 + 8192, D); gtmpT = T()
        dma("sync", gtmp, gmix_d, [], [gtmpT])
        stt(s1b, s1b, 1.0, gtmp, ALU.add, ALU.mult, [modT[1], gtmpT], [modT[1]])
        dma("sync", gtmp, gffn_d, [], [gtmpT])
        stt(s2b, s2b, 1.0, gtmp, ALU.add, ALU.mult, [modT[4], gtmpT], [modT[4]])
        dma("sync", gt2_d, gt2b, [modT[5]], [])
        act(sinkexp, C(C_SINK, 8), AF.Exp, [cT], [sinkexpT])
        act(sp8, C(C_LAM, 4), AF.Exp, [cT], [sp8T], scale=-1.0)
        act(sp8, sp8, AF.Ln, [sp8T], [sp8T], bias=1.0)
        ts("vector", sp8, sp8, 8.0, None, ALU.mult, None, [sp8T], [sp8T])
        ts("vector", ls8, sp8, -1.0, None, ALU.mult, None, [sp8T], [ls8T])
        P.barrier()

        HT = PH
        hT = V3(HT, 8, S, F32R)
        hTT = [T() for _ in range(4)]
        XS0 = PH + 16384
        xst = [V(XS0 + k * 1024, D) for k in range(2)]; xstT = [T(), T()]
        h1t = [V(XS0 + 2048 + k * 1024, D) for k in range(2)]; h1T = [T(), T()]
        ss_, ssT = small(2)
        rs_, rsT = small(2)

        def rms_rstd(src, srcT, junk, junkT, ss, ssT_, rs, rsT_, n):
            act(junk, src, AF.Square, [srcT], [junkT, ssT_], accum_out=ss)
            act(rs, ss, AF.Sqrt, [ssT_], [rsT_], scale=1.0 / n, bias=EPS)
            P.op("vector", lambda e: e.reciprocal(out=rs, in_=rs), [rsT_], [rsT_])

        def transpose8(src, srcT, dstfn, dstT, k):
            for half in range(2):
                pb, pt = bank()
                for q in range(4):
                    kc = half * 4 + q
                    tr(pb[:, q * 128:(q + 1) * 128], src[:, kc * 128:(kc + 1) * 128], identF, [srcT, cT], [pt])
                dst = dstfn(half)
                cp("scalar" if (half + k) % 2 == 0 else "vector", dst, pb[:, :].rearrange("p (a b) -> p a b", a=4), [pt], [dstT])

        for i in range(NT):
            k = i % 2
            dma("sync", xst[k], x_d[i * 128:(i + 1) * 128, :], [], [xstT[k]])
            rms_rstd(xst[k], xstT[k], h1t[k], h1T[k], ss_[:, k:k + 1], ssT, rs_[:, k:k + 1], rsT, D)
            stt(h1t[k], xst[k], rs_[:, k:k + 1], s1b, ALU.mult, ALU.mult, [xstT[k], rsT, modT[1]], [h1T[k]])
            tt("gpsimd", h1t[k], h1t[k], sh1b, ALU.add, [h1T[k], modT[0]], [h1T[k]])
            transpose8(h1t[k], h1T[k], lambda half, i=i: hT[:, half * 4:(half + 1) * 4, i * 128:(i + 1) * 128], hTT[i // 4], k)
        P.barrier()
        if stop_after == 1:
            return finish_debug(nc, P, dbg_d, hT.bitcast(F32).rearrange('p a b -> p (a b)'), out_d)

        WI0 = PH + 16384
        WI = [V3(WI0 + k * 1024, 8, 128, F32R) for k in range(2)]; WIT = [T(), T()]
        wi_state = [0]
        BD0 = WI0 + 2048
        bdA = V3(BD0, 4, 128, F32R); bdX = V3(BD0 + 512, 4, 128, F32R); bdT = T()
        dma("gpsimd", bdA, bda_d.rearrange("p (a b) -> p a b", a=4), [], [bdT])
        dma("gpsimd", bdX, bdx_d.rearrange("p (a b) -> p a b", a=4), [], [bdT])
        LB0 = BD0 + 1024
        LBR = [V(LB0 + k * S, S, F32R) if k == 1 else None for k in range(8)]
        LB = [LBR[k].bitcast(F32) if k == 1 else V(LB0 + k * S, S) for k in range(8)]
        LBT = [T() for _ in range(8)]
        LRU0 = LB0 + 8 * S
        assert LRU0 + 4 * S <= ARENA, LRU0 + 4 * S
        lruT = V3(LRU0, 4, S, F32R)
        lru0 = lruT.bitcast(F32)
        lruTT = [T() for _ in range(4)]
        win_v = win_d.rearrange("(kc p) n -> p kc n", p=128)
        evac_state = [0]

        def load_wi(col_specs):
            k = wi_state[0]
            wi_state[0] = 1 - k
            for (dst0, c0, n) in col_specs:
                dma("gpsimd", WI[k][:, :, dst0:dst0 + n], win_v[:, :, c0:c0 + n], [], [WIT[k]])
            return WI[k], WIT[k]

        def inproj_fm(w, wT_, dstfn, dstT):
            for tb in range(4):
                pb, pt = bank()
                for kc in range(8):
                    mm(pb[:, :], w[:, kc, :], hT[:, kc, tb * 512:(tb + 1) * 512], kc == 0, kc == 7, [wT_, hTT[tb]], [pt])
                evac_state[0] += 1
                cp("scalar" if evac_state[0] % 2 == 0 else "vector", dstfn(tb), pb[:, :], [pt], [dstT])

        cw = lambda c, k: C(C_CW + c * 4 + k, 1)
        colc = lambda base, c: C(base + c, 1)
        acc = LB[7]; accT = LBT[7]
        for c in range(4):
            w, wT_ = load_wi([(0, 768 + c * 128, 128)])
            inproj_fm(w, wT_, lambda tb: LB[0][:, tb * 512:(tb + 1) * 512], LBT[0])
            w, wT_ = load_wi([(0, 1280 + c * 128, 128)])
            inproj_fm(w, wT_, lambda tb: LB[2][:, tb * 512:(tb + 1) * 512], LBT[2])
            xr, xc, xg = LB[0], LB[1], LB[2]
            ts("vector", LBR[1], xr, cw(c, 3), colc(C_CB, c), ALU.mult, ALU.add, [LBT[0], cT], [LBT[1]])
            for sh in (1, 2, 3):
                stt(LBR[1][:, sh:], xr[:, :S - sh], cw(c, 3 - sh), xc[:, sh:], ALU.mult, ALU.add, [LBT[0], LBT[1], cT], [LBT[1]])
            for gi, (bd, bcol, dst) in enumerate(((bdA, C_BA, 3), (bdX, C_BX, 4))):
                for tb in range(4):
                    pb, pt = bank()
                    mm(pb[:, :], bd[:, c, :], LBR[1][:, tb * 512:(tb + 1) * 512], True, True, [bdT, LBT[1]], [pt])
                    act(LB[dst][:, tb * 512:(tb + 1) * 512], pb[:, :], AF.Sigmoid, [pt, cT], [LBT[dst]], bias=colc(bcol, c))
            r, ig = LB[3], LB[4]
            act(LB[6], r, AF.Tanh, [LBT[3], sp8T], [LBT[6]], scale=sp8[:, c:c + 1])
            act(LB[3], r, AF.Exp, [LBT[3], ls8T], [LBT[3]], scale=ls8[:, c:c + 1])
            a = LB[3]
            tt("gpsimd", LB[5], a, a, ALU.mult, [LBT[3]], [LBT[5]])
            stt(LB[5], LB[5], 1.0, LB[6], ALU.add, ALU.mult, [LBT[5], LBT[6]], [LBT[5]])
            act(LB[5], LB[5], AF.Sqrt, [LBT[5]], [LBT[5]])
            tt("gpsimd", LB[4], ig, xc, ALU.mult, [LBT[4], LBT[1]], [LBT[4]])
            tt("vector", LB[4], LB[4], LB[5], ALU.mult, [LBT[4], LBT[5]], [LBT[4]])
            P.op("vector", lambda e, a=a: e.tensor_tensor_scan(out=LB[0], data0=a, data1=LB[4], initial=0.0, op0=ALU.mult, op1=ALU.add),
                 [LBT[3], LBT[4], LBT[0]], [LBT[0]])
            act(LB[6], xg, AF.Square, [LBT[2]], [LBT[6]])
            ts("gpsimd", LB[6], LB[6], 0.044715, 1.0, ALU.mult, ALU.add, [LBT[6]], [LBT[6]])
            tt("gpsimd", LB[6], LB[6], xg, ALU.mult, [LBT[6], LBT[2]], [LBT[6]])
            act(LB[6], LB[6], AF.Sigmoid, [LBT[6]], [LBT[6]], scale=1.5957691216057308)
            tt("gpsimd", LB[6], LB[6], xg, ALU.mult, [LBT[6], LBT[2]], [LBT[6]])
            tt("vector", lruT[:, c, :], LB[0], LB[6], ALU.mult, [LBT[0], LBT[6]], [lruTT[c]])
            if c == 0:
                tt("gpsimd", acc, lru0[:, c, :], lru0[:, c, :], ALU.mult, [lruTT[c]], [accT])
            else:
                tt("gpsimd", LB[5], lru0[:, c, :], lru0[:, c, :], ALU.mult, [lruTT[c]], [LBT[5]])
                tt("gpsimd", acc, acc, LB[5], ALU.add, [accT, LBT[5]], [accT])
        for tb in range(4):
            pb, pt = bank()
            mm(pb[:, :], onesF, acc[:, tb * 512:(tb + 1) * 512], True, True, [cT, accT], [pt])
            act(LB[6][:, tb * 512:(tb + 1) * 512], pb[:, :], AF.Sqrt, [pt], [LBT[6]], scale=1.0 / 512, bias=EPS)
        P.op("vector", lambda e: e.reciprocal(out=LB[6], in_=LB[6]), [LBT[6]], [LBT[6]])
        for c in range(4):
            stt(lruT[:, c, :], lru0[:, c, :], colc(C_GL, c), LB[6], ALU.mult, ALU.mult, [lruTT[c], LBT[6], cT], [lruTT[c]])
        P.barrier()
        if stop_after == 2:
            return finish_debug(nc, P, dbg_d, lruT.bitcast(F32).rearrange('p a b -> p (a b)'), out_d)

        Q0 = LB0
        qT = V3(Q0, 4, S, F32R); qTT = [T() for _ in range(4)]
        kk = V3(Q0 + 4 * S, 2, S, F32R); kkT = [T(), T()]
        VA0 = Q0 + 6 * S
        vflat = V(VA0, NT * 132, F32R)
        vaug = vflat.rearrange("p (t g d) -> p t g d", t=NT, g=2)
        vT = T()
        assert VA0 + NT * 132 <= LRU0
        ones4 = onesF[:, 0:32].rearrange("p (t g d) -> p t g d", t=NT, g=2)
        cp("vector", vaug[:, :, :, 64:65], ones4, [cT], [vT])
        ts("vector", vaug[:, :, :, 65:66], ones4, 0.0, None, ALU.mult, None, [cT], [vT])
        for c in range(4):
            w, wT_ = load_wi([(0, c * 128, 128)])
            inproj_fm(w, wT_, lambda tb, c=c: qT[:, c, tb * 512:(tb + 1) * 512], qTT[c])
        for g in range(2):
            w, wT_ = load_wi([(0, 512 + g * 64, 64), (64, 512 + g * 64, 64)])
            inproj_fm(w, wT_, lambda tb, g=g: kk[:, g, tb * 512:(tb + 1) * 512], kkT[g])
        w, wT_ = load_wi([(0, 640, 128)])
        for i in range(NT):
            pb, pt = bank()
            for kc in range(8):
                mm(pb[:, 0:128], hT[:, kc, i * 128:(i + 1) * 128], w[:, kc, :], kc == 0, kc == 7, [wT_, hTT[i // 4]], [pt])
            cp("scalar" if i % 2 == 0 else "vector", vaug[:, i, :, 0:64], pb[:, 0:128].rearrange("p (g d) -> p g d", g=2), [pt], [vT])
        P.barrier()

        AT0 = PH
        attnT = V3(AT0, 4, S, F32R); attnTT = T()
        ET0 = PH + 4 * S
        eT = [[V(ET0 + (s * 2 + kbi) * 512, 512, F32R) for kbi in range(2)] for s in range(2)]
        eTT = [[T(), T()], [T(), T()]]
        AN0 = ET0 + 2048
        attn_t = [V(AN0 + k * 512, 512) for k in range(2)]; attn_tT = [T(), T()]
        junk512 = V(AN0 + 1024, 512); junk512T = T()
        den, denT = small(8)
        ssa, ssaT = small(2)
        rsa, rsaT = small(2)
        scale = 0.125
        for n in range(NT):
            k = n % 2
            for g in range(2):
                s = g
                kbs = ([n - 1] if n > 0 else []) + [n]
                for kbi, kb in enumerate(kbs):
                    diag = (kb == n)
                    pb, pt = bank()
                    mm(pb[:, :], identR, maskDR if diag else maskPR, True, False, [crT], [pt])
                    for hh in range(4):
                        c = 2 * g + hh // 2
                        h0 = (hh % 2) * 64
                        mm(pb[:, hh * 128:(hh + 1) * 128], kk[h0:h0 + 64, g, kb * 128:(kb + 1) * 128],
                           qT[h0:h0 + 64, c, n * 128:(n + 1) * 128], False, hh == 3, [kkT[g], qTT[c]], [pt])
                    act(eT[s][kbi], pb[:, :], AF.Exp, [pt], [eTT[s][kbi]], scale=scale)
                pb, pt = bank()
                for hh in range(4):
                    for kbi, kb in enumerate(kbs):
                        mm(pb[:, hh * 66:(hh + 1) * 66], eT[s][kbi][:, hh * 128:(hh + 1) * 128], vaug[:, kb, g, :],
                           kbi == 0, kbi == len(kbs) - 1, [eTT[s][kbi], vT], [pt])
                pv = pb[:, 0:264].rearrange("p (h d) -> p h d", h=4)
                tt("vector", den[:, g * 4:(g + 1) * 4], pv[:, :, 64], sinkexp[:, g * 4:(g + 1) * 4], ALU.add, [pt, sinkexpT], [denT])
                P.op("vector", lambda e, g=g: e.reciprocal(out=den[:, g * 4:(g + 1) * 4], in_=den[:, g * 4:(g + 1) * 4]), [denT], [denT])
                for hh in range(4):
                    hd = g * 4 + hh
                    if hh % 2 == 0:
                        ts("vector", attn_t[k][:, hd * 64:(hd + 1) * 64], pv[:, hh, 0:64], den[:, hd:hd + 1], None, ALU.mult, None, [pt, denT], [attn_tT[k]])
                    else:
                        act(attn_t[k][:, hd * 64:(hd + 1) * 64], pv[:, hh, 0:64], AF.Copy, [pt, denT], [attn_tT[k]], scale=den[:, hd:hd + 1])
            rms_rstd(attn_t[k], attn_tT[k], junk512, junk512T, ssa[:, k:k + 1], ssaT, rsa[:, k:k + 1], rsaT, 512)
            ts("vector", attn_t[k], attn_t[k], rsa[:, k:k + 1], None, ALU.mult, None, [attn_tT[k], rsaT], [attn_tT[k]])
            pb, pt = bank()
            for c in range(4):
                tr(pb[:, c * 128:(c + 1) * 128], attn_t[k][:, c * 128:(c + 1) * 128], identF, [attn_tT[k], cT], [pt])
            for c in range(4):
                if c % 2 == 0:
                    ts("vector", attnT[:, c, n * 128:(n + 1) * 128], pb[:, c * 128:(c + 1) * 128], colc(C_GA, c), None, ALU.mult, None, [pt, cT], [attnTT])
                else:
                    act(attnT[:, c, n * 128:(n + 1) * 128], pb[:, c * 128:(c + 1) * 128], AF.Copy, [pt, cT], [attnTT], scale=colc(C_GA, c))
        P.barrier()
        if stop_after == 3:
            return finish_debug(nc, P, dbg_d, attnT.bitcast(F32).rearrange('p a b -> p (a b)'), out_d)

        WO0 = PH + 4 * S
        wo = V3(WO0, 8, D, F32R); woT = T()
        wout_v = wout_d.rearrange("(kc p) n -> p kc n", p=128)
        for hf in range(2):
            dma("gpsimd", wo[:, :, hf * 512:(hf + 1) * 512], wout_v[:, :, hf * 512:(hf + 1) * 512], [], [woT])
        T0 = WO0 + 8 * D
        xt = [V(T0 + k * D, D) for k in range(2)]; xtT = [T(), T()]
        x1t = [V(T0 + 2 * D + k * D, D) for k in range(2)]; x1T = [T(), T()]
        h2t = [V(T0 + 4 * D + k * D, D) for k in range(2)]; h2T_ = [T(), T()]
        h2Tr = [V3(T0 + 6 * D + k * D, 8, 128) for k in range(2)]; h2TrT = [T(), T()]
        WR0 = T0 + 8 * D
        wr = V3(WR0, 8, E); wrT = T()
        dma("sync", wr, wr_d.rearrange("(kc p) n -> p kc n", p=128), [], [wrT])
        R0 = WR0 + 8 * E
        assert R0 + 1024 <= LB0 + 8 * S
        rsm = [R0]

        def rsmall(n):
            o = rsm[0]
            rsm[0] += (n + 7) // 8 * 8
            return V(o, n), T()

        lg, lgT = rsmall(32)
        top8, top8T = rsmall(8)
        idx8_o = rsm[0]; rsm[0] += 8
        idx8 = V(idx8_o, 8, U32); idx8T = T()
        idxf, idxfT = rsmall(4)
        negm, negmT = rsmall(1)
        e4, e4T = rsmall(4)
        ssum, ssumT = rsmall(1)
        mask, maskT = rsmall(32)
        rkp, rkpT = rsmall(32)
        ohj, ohjT = rsmall(32)
        ss2, ss2T = rsmall(2)
        rs2, rs2T = rsmall(2)
        xsT_d = T()
        x1dT = T()
        mergedT = lambda kc: (attnT[:, kc, :] if kc < 4 else lruT[:, kc - 4, :])
        mT = lambda kc: (attnTT if kc < 4 else lruTT[kc - 4])
        for i in range(NT):
            k = i % 2
            dma("sync", xt[k], x_d[i * 128:(i + 1) * 128, :], [], [xtT[k]])
            for hf in range(2):
                pb, pt = bank()
                for kc in range(8):
                    mm(pb[:, :], mergedT(kc)[:, i * 128:(i + 1) * 128], wo[:, kc, hf * 512:(hf + 1) * 512], kc == 0, kc == 7, [mT(kc), woT], [pt])
                sl = slice(hf * 512, (hf + 1) * 512)
                tt("vector", x1t[k][:, sl], pb[:, :], gt1b[:, sl], ALU.mult, [pt, modT[2]], [x1T[k]])
                tt("gpsimd", x1t[k][:, sl], x1t[k][:, sl], xt[k][:, sl], ALU.add, [x1T[k], xtT[k]], [x1T[k]])
            dma("sync", x1_d[i * 128:(i + 1) * 128, :], x1t[k], [x1T[k]], [])
            rms_rstd(x1t[k], x1T[k], h2t[k], h2T_[k], ss2[:, k:k + 1], ss2T, rs2[:, k:k + 1], rs2T, D)
            stt(h2t[k], x1t[k], rs2[:, k:k + 1], s2b, ALU.mult, ALU.mult, [x1T[k], rs2T, modT[4]], [h2T_[k]])
            tt("gpsimd", h2t[k], h2t[k], sh2b, ALU.add, [h2T_[k], modT[3]], [h2T_[k]])
            transpose8(h2t[k], h2T_[k], lambda half, k=k: h2Tr[k][:, half * 4:(half + 1) * 4, :], h2TrT[k], k)
            pb, pt = bank()
            for kc in range(8):
                mm(pb[:, 0:E], h2Tr[k][:, kc, :], wr[:, kc, :], kc == 0, kc == 7, [h2TrT[k], wrT], [pt])
            tt("vector", lg, pb[:, 0:E], C(C_BR, E), ALU.add, [pt, cT], [lgT])
            P.op("vector", lambda e: e.max(out=top8, in_=lg), [lgT], [top8T])
            P.op("vector", lambda e: e.max_index(out=idx8, in_max=top8, in_values=lg), [lgT, top8T], [idx8T])
            cp("vector", idxf, idx8[:, 0:4], [idx8T], [idxfT])
            ts("vector", negm, top8[:, 0:1], -1.0, None, ALU.mult, None, [top8T], [negmT])
            act(e4, top8[:, 0:4], AF.Exp, [top8T, negmT], [e4T, ssumT], bias=negm, accum_out=ssum)
            P.op("vector", lambda e: e.reciprocal(out=ssum, in_=ssum), [ssumT], [ssumT])
            ts("vector", gates_all[:, i * 4:(i + 1) * 4], e4, ssum, None, ALU.mult, None, [e4T, ssumT], [gatesT])
            ts("vector", mask, lg, top8[:, 3:4], None, ALU.is_ge, None, [lgT, top8T], [maskT])
            pb, pt = bank()
            if i > 0:
                mm(pb[:, 0:E], onesF, cum, True, False, [cT, cumT], [pt])
            mm(pb[:, 0:E], UF, mask, i == 0, True, [cT, maskT], [pt])
            tt("vector", rkp, pb[:, 0:E], C(C_RB, E), ALU.add, [pt, cT], [rkpT])
            if i == 0:
                cp("vector", cum, mask, [maskT], [cumT])
            else:
                tt("vector", cum, cum, mask, ALU.add, [cumT, maskT], [cumT])
            for j in range(4):
                stt(ohj, C(C_IOTA, E), idxf[:, j:j + 1], rkp, ALU.is_equal, ALU.mult, [cT, idxfT, rkpT], [ohjT, destfT],
                    accum_out=destf_all[:, i * 4 + j:i * 4 + j + 1])
            cp("vector", desti_all[:, i * 4:(i + 1) * 4], destf_all[:, i * 4:(i + 1) * 4], [destfT], [destiT])
            for j in range(4):
                P.dma("gpsimd", lambda e, i=i, j=j, k=k: e.indirect_dma_start(
                    out=xs_d, out_offset=bass.IndirectOffsetOnAxis(ap=desti_all[:, i * 4 + j:i * 4 + j + 1], axis=0),
                    in_=h2t[k], in_offset=None), [h2T_[k], destiT], [])
        cntb, cntbT = rsmall(32)
        nblk, nblkT = rsmall(32)
        pend, pendT = rsmall(32)
        pst, pstT = rsmall(32)
        ej, ejT = rsmall(NB)
        pstj, pstjT = rsmall(NB)
        tmpj, tmpjT = rsmall(NB)
        sj, sjT = rsmall(NB)
        rowj, rowjT = rsmall(NB)
        valid, validT = rsmall(NB)
        skp, skpT = rsmall(NB)
        ebW, ebWT = rsmall(NB)
        ebD, ebDT = rsmall(NB)
        idxW = V(MOD, NB * 8, I32).rearrange("p (j k) -> p j k", k=8)
        idxD = V(MOD + NB * 8, NB * 4, I32).rearrange("p (j k) -> p j k", k=4)
        idxX = V(MOD + NB * 12, NB * 2, I32).rearrange("p (j k) -> p j k", k=2)
        idxB = V(MOD + NB * 14, NB, I32)
        idxBd = V(MOD + NB * 15, NB, I32)
        tblT = T()
        jrow = C(C_J, NB)
        pid = C(C_PID, 1)
        pb, pt = bank()
        mm(pb[:, 0:E], onesF, cum, True, True, [cT, cumT], [pt])
        cp("vector", cntb, pb[:, 0:E], [pt], [cntbT])
        ts("vector", nblk, cntb, 0.0, None, ALU.is_gt, None, [cntbT], [nblkT])
        for sx in range(1, EREG // BSZ):
            stt(nblk, cntb, float(BSZ * sx), nblk, ALU.is_gt, ALU.add, [cntbT, nblkT], [nblkT])
        P.op("vector", lambda e: e.tensor_tensor_scan(out=pend, data0=onesF[:, 0:E], data1=nblk, initial=0.0, op0=ALU.mult, op1=ALU.add),
             [cT, nblkT], [pendT])
        tt("vector", pst, pend, nblk, ALU.subtract, [pendT, nblkT], [pstT])
        ts("vector", ej, jrow, pend[:, 0:1], None, ALU.is_ge, None, [cT, pendT], [ejT])
        for e_ in range(1, E):
            stt(ej, jrow, pend[:, e_:e_ + 1], ej, ALU.is_ge, ALU.add, [cT, pendT, ejT], [ejT])
        ts("vector", valid, ej, E - 0.5, None, ALU.is_lt, None, [ejT], [validT])
        ts("vector", ej, ej, float(E - 1), None, ALU.min, None, [ejT], [ejT])
        for e_ in range(E):
            ts("vector", tmpj, ej, float(e_), None, ALU.is_equal, None, [ejT], [tmpjT])
            if e_ == 0:
                ts("vector", pstj, tmpj, pst[:, 0:1], None, ALU.mult, None, [tmpjT, pstT], [pstjT])
            else:
                stt(pstj, tmpj, pst[:, e_:e_ + 1], pstj, ALU.mult, ALU.add, [tmpjT, pstT, pstjT], [pstjT])
        tt("vector", sj, jrow, pstj, ALU.subtract, [cT, pstjT], [sjT])
        ts("vector", skp, sj, 0.0, None, ALU.is_equal, None, [sjT], [skpT])
        ts("vector", skp, skp, -BIG, BIG, ALU.mult, ALU.add, [skpT], [skpT])
        ts("vector", tmpj, sj, float(BSZ), None, ALU.mult, None, [sjT], [tmpjT])
        stt(rowj, ej, float(EREG), tmpj, ALU.mult, ALU.add, [ejT, tmpjT], [rowjT])
        ts("vector", rowj, rowj, -BIG, None, ALU.add, None, [rowjT], [rowjT])
        tt("vector", rowj, rowj, valid, ALU.mult, [rowjT, validT], [rowjT])
        ts("vector", rowj, rowj, BIG, pid, ALU.add, ALU.add, [rowjT, cT], [rowjT])
        stt(ebW, ej, 1024.0, skp, ALU.mult, ALU.add, [ejT, skpT], [ebWT])
        ts("vector", ebW, ebW, pid, None, ALU.add, None, [ebWT, cT], [ebWT])
        stt(ebD, ej, 512.0, skp, ALU.mult, ALU.add, [ejT, skpT], [ebDT])
        ts("vector", ebD, ebD, pid, None, ALU.add, None, [ebDT, cT], [ebDT])
        for k_ in range(8):
            ts("vector", idxW[:, :, k_], ebW, float(k_ * 128), None, ALU.add, None, [ebWT], [tblT])
        for k_ in range(4):
            ts("vector", idxD[:, :, k_], ebD, float(k_ * 128), None, ALU.add, None, [ebDT], [tblT])
        for k_ in range(2):
            ts("vector", idxX[:, :, k_], rowj, float(k_ * 128), None, ALU.add, None, [rowjT], [tblT])
        ts("vector", idxB, ej, 128.0, pid, ALU.mult, ALU.add, [ejT, cT], [tblT])
        cp("vector", idxBd, ej, [ejT], [tblT])
        P.barrier()
        if stop_after == 4:
            return finish_debug(nc, P, dbg_d, None, out_d)

        WSg = [V3(PH + k * 2048, 4, 512, F32R) for k in range(8)]; WSgT = [T() for _ in range(8)]
        WSd = [V3(PH + (8 + k) * 2048, 4, 512, F32R) for k in range(4)]; WSdT = [T() for _ in range(4)]
        X0 = PH + 12 * 2048
        xsb = [V3(X0 + k * NJ * D, NJ, D) for k in range(2)]; xsbT = [T(), T()]
        XT0 = X0 + 2 * NJ * D
        xsT = [V3(XT0 + k * 8 * BSZ, 8, BSZ, F32R) for k in range(2)]; xsTT = [T(), T()]
        A0 = XT0 + 2 * 8 * BSZ
        actT = V3(A0, 8, BSZ, F32R); actTT = T()
        Y0 = A0 + 8 * BSZ
        yb = [V3(Y0 + k * NJ * D, NJ, D) for k in range(2)]; ybT = [T(), T()]
        G0 = Y0 + 2 * NJ * D
        gsb = [V(G0 + k * BSZ, BSZ) for k in range(6)]; gsbT = [T() for _ in range(6)]
        BG0 = G0 + 6 * BSZ
        bgub = [V(BG0 + k * 16, 16) for k in range(2)]; bgubT = [T(), T()]
        BD0_ = BG0 + 32
        bdnb = [V(BD0_ + k * D, D) for k in range(2)]; bdnbT = [T(), T()]
        assert BD0_ + 2 * D <= ARENA, BD0_ + 2 * D

        bregs = {}

        def breg(e, bound):
            if bound not in bregs:
                bregs[bound] = e.to_reg(bound)
            return bregs[bound]

        def igather(out, src, idx_ap, bound, reads, writes):
            P.dma("gpsimd", lambda e: e.indirect_dma_start(out=out, out_offset=None, in_=src,
                  in_offset=bass.IndirectOffsetOnAxis(ap=idx_ap, axis=0), bounds_check=breg(e, bound), oob_is_err=False), reads, writes)

        def load_block_small(j):
            k = j % 2
            for jj in range(NJ):
                igather(xsb[k][:, jj, :], xs_d, idxX[:, j, jj:jj + 1], NSLOT - 1, [tblT], [xsbT[k]])
            igather(bgub[k], bgur_d, idxB[:, j:j + 1], E * 128 - 1, [tblT], [bgubT[k]])
            igather(bdnb[k], bdn_d, idxBd[:, j:j + 1], E - 1, [tblT], [bdnbT[k]])

        def load_wg(j, k_):
            igather(WSg[k_].rearrange("p a b -> p (a b)"), wgur_d, idxW[:, j, k_:k_ + 1], E * 1024 - 1, [tblT], [WSgT[k_]])

        def load_wd(j, k_):
            igather(WSd[k_].rearrange("p a b -> p (a b)"), wdnr_d, idxD[:, j, k_:k_ + 1], E * 512 - 1, [tblT], [WSdT[k_]])

        load_block_small(0)
        for k_ in range(8):
            load_wg(0, k_)
        for k_ in range(4):
            load_wd(0, k_)
        for j in range(NB):
            k = j % 2
            for kc in range(8):
                pb, pt = bank()
                for jj in range(NJ):
                    tr(pb[:, jj * 128:(jj + 1) * 128], xsb[k][:, jj, kc * 128:(kc + 1) * 128], identF, [xsbT[k], cT], [pt])
                cp("scalar" if kc % 2 == 0 else "vector", xsT[k][:, kc, :], pb[:, 0:BSZ], [pt], [xsTT[k]])
            if j + 1 < NB:
                load_block_small(j + 1)
            for fc in range(8):
                qg = (fc // 4) * 2
                lc = (fc % 4) * 128
                pg, pgt = bank()
                for kc in range(8):
                    wi_ = qg * 2 + kc // 4
                    mm(pg[:, 0:BSZ], WSg[wi_][:, kc % 4, lc:lc + 128], xsT[k][:, kc, :], kc == 0, kc == 7, [WSgT[wi_], xsTT[k]], [pgt])
                pu, put = bank()
                for kc in range(8):
                    wi_ = (qg + 1) * 2 + kc // 4
                    mm(pu[:, 0:BSZ], WSg[wi_][:, kc % 4, lc:lc + 128], xsT[k][:, kc, :], kc == 0, kc == 7, [WSgT[wi_], xsTT[k]], [put])
                if fc % 4 == 3 and j + 1 < NB:
                    for k_ in range(qg * 2, qg * 2 + 4):
                        load_wg(j + 1, k_)
                g3 = (fc % 2) * 3
                gb, gbT = gsb[g3], gsbT[g3]
                ub, ubT = gsb[g3 + 1], gsbT[g3 + 1]
                sl_, slT = gsb[g3 + 2], gsbT[g3 + 2]
                ts("vector", gb, pg[:, 0:BSZ], bgub[k][:, fc:fc + 1], 7.0, ALU.add, ALU.min, [pgt, bgubT[k]], [gbT])
                ts("vector", ub, pu[:, 0:BSZ], bgub[k][:, 8 + fc:8 + fc + 1], 7.0, ALU.add, ALU.min, [put, bgubT[k]], [ubT])
                ts("vector", ub, ub, -7.0, 1.0, ALU.max, ALU.add, [ubT], [ubT])
                act(sl_, gb, AF.Silu, [gbT], [slT], scale=1.702)
                stt(actT[:, fc, :], sl_, 1.0 / 1.702, ub, ALU.mult, ALU.mult, [slT, ubT], [actTT])
            for hf in range(2):
                for jj in range(NJ):
                    pb, pt = bank()
                    for fc in range(8):
                        wi_ = hf * 2 + fc // 4
                        mm(pb[:, :], actT[:, fc, jj * 128:(jj + 1) * 128], WSd[wi_][:, fc % 4, :], fc == 0, fc == 7, [actTT, WSdT[wi_]], [pt])
                    sl = slice(hf * 512, (hf + 1) * 512)
                    tt("vector", yb[k][:, jj, sl], pb[:, :], bdnb[k][:, sl], ALU.add, [pt, bdnbT[k]], [ybT[k]])
                if j + 1 < NB:
                    load_wd(j + 1, hf * 2)
                    load_wd(j + 1, hf * 2 + 1)
            for jj in range(NJ):
                P.dma("gpsimd", lambda e, j=j, jj=jj, k=k: e.indirect_dma_start(
                    out=ys_d, out_offset=bass.IndirectOffsetOnAxis(ap=idxX[:, j, jj:jj + 1], axis=0),
                    in_=yb[k][:, jj, :], in_offset=None, bounds_check=breg(e, NSLOT - 1), oob_is_err=False), [ybT[k], tblT], [])
        P.barrier()
        if stop_after == 5:
            return finish_debug(nc, P, dbg_d, None, out_d)

        gt2s = V(PH, D); gt2sT = T()
        gfin = V(PH + D, D); gfinT = T()
        dma("sync", gt2s, gt2_d, [], [gt2sT])
        dma("sync", gfin, gfin_d, [], [gfinT])
        F0 = PH + 2 * D
        x1r = [V(F0 + k * D, D) for k in range(2)]; x1rT = [T(), T()]
        yg = [[V(F0 + 2 * D + (k * 4 + j) * D, D) for j in range(4)] for k in range(2)]
        ygT = [[T() for _ in range(4)] for _ in range(2)]
        ob = [V(F0 + 10 * D + k * D, D) for k in range(2)]; obT = [T(), T()]
        ss3, ss3T = small(2)
        rs3, rs3T = small(2)
        outT = T()
        for i in range(NT):
            k = i % 2
            dma("sync", x1r[k], x1_d[i * 128:(i + 1) * 128, :], [x1dT], [x1rT[k]])
            for j in range(4):
                P.dma("gpsimd", lambda e, i=i, j=j, k=k: e.indirect_dma_start(
                    out=yg[k][j], out_offset=None, in_=ys_d,
                    in_offset=bass.IndirectOffsetOnAxis(ap=desti_all[:, i * 4 + j:i * 4 + j + 1], axis=0)), [destiT], [ygT[k][j]])
            ts("vector", ob[k], yg[k][0], gates_all[:, i * 4:i * 4 + 1], None, ALU.mult, None, [ygT[k][0], gatesT], [obT[k]])
            for j in range(1, 4):
                stt(ob[k], yg[k][j], gates_all[:, i * 4 + j:i * 4 + j + 1], ob[k], ALU.mult, ALU.add, [ygT[k][j], gatesT, obT[k]], [obT[k]])
            tt("gpsimd", ob[k], ob[k], gt2s, ALU.mult, [obT[k], gt2sT], [obT[k]])
            tt("vector", x1r[k], x1r[k], ob[k], ALU.add, [x1rT[k], obT[k]], [x1rT[k]])
            rms_rstd(x1r[k], x1rT[k], ob[k], obT[k], ss3[:, k:k + 1], ss3T, rs3[:, k:k + 1], rs3T, D)
            stt(ob[k], x1r[k], rs3[:, k:k + 1], gfin, ALU.mult, ALU.mult, [x1rT[k], rs3T, gfinT], [obT[k]])
            dma("sync", out_d[i * 128:(i + 1) * 128, :], ob[k], [obT[k]], [])
        P.barrier()
        P.emit()
    return nc


def finish_debug(nc, P, dbg_d, src, out_d):
    if src is not None:
        n = src.shape[1]
        P.dma("sync", lambda e: e.dma_start(out=dbg_d[:, 0:n], in_=src), [], [])
    P.barrier()
    P.emit()
    return nc


def host_consts(inp):
    c = np.zeros((128, NCONST), np.float32)
    c[:, C_ID:C_ID + 128] = np.eye(128, dtype=np.float32)
    c[:, C_ONE:C_ONE + 128] = 1.0
    kk_, qq = np.meshgrid(np.arange(128), np.arange(128), indexing="ij")
    c[:, C_U:C_U + 128] = (kk_ < qq).astype(np.float32)
    md = np.where(kk_ <= qq, 0.0, NEG).astype(np.float32)
    mp = np.where(kk_ > qq, 0.0, NEG).astype(np.float32)
    c[:, C_MD:C_MD + 512] = np.tile(md, (1, 4))
    c[:, C_MP:C_MP + 512] = np.tile(mp, (1, 4))
    c[:, C_IOTA:C_IOTA + 32] = np.arange(32, dtype=np.float32)[None, :]
    c[:, C_RB:C_RB + 32] = (np.arange(32, dtype=np.float32) * EREG)[None, :]
    c[:, C_PID] = np.arange(128, dtype=np.float32)
    c[:, C_J:C_J + NB] = np.arange(NB, dtype=np.float32)[None, :]
    c[:, C_BR:C_BR + 32] = inp["b_router"][0][None, :]
    c[:, C_SINK:C_SINK + 8] = inp["sinks"][0][None, :]
    cwt = inp["conv_w"][0]
    c[:, C_CW:C_CW + 16] = cwt.T.reshape(4, 128, 4).transpose(1, 0, 2).reshape(128, 16)
    col = lambda v: np.asarray(v, np.float32).reshape(4, 128).T
    c[:, C_CB:C_CB + 4] = col(inp["conv_b"][0])
    c[:, C_BA:C_BA + 4] = col(inp["b_rg_a"][0].reshape(512))
    c[:, C_BX:C_BX + 4] = col(inp["b_rg_x"][0].reshape(512))
    c[:, C_LAM:C_LAM + 4] = col(inp["lru_lambda"][0])
    c[:, C_GA:C_GA + 4] = col(inp["g_attn_out"][0])
    c[:, C_GL:C_GL + 4] = col(inp["g_lru_out"][0])
    return c


def host_blockdiag(w):
    bd = np.zeros((128, 4, 128), np.float32)
    for c in range(4):
        bd[0:64, c, 0:64] = w[2 * c]
        bd[64:128, c, 64:128] = w[2 * c + 1]
    return bd.reshape(128, 512)


def host_wgu(w):
    w6 = w.reshape(E, 2, 4, 128, 4, 512)
    cg = [0, 2, 1, 3]
    out = np.empty((E, 4, 2, 128, 4, 512), np.float32)
    for q in range(4):
        out[:, q] = w6[:, :, :, :, cg[q], :].transpose(0, 1, 3, 2, 4)
    return out.reshape(E * 1024, 2048)


def host_wdn(w):
    w6 = w.reshape(E, 2, 4, 128, 2, 512)
    out = np.ascontiguousarray(w6.transpose(0, 4, 1, 3, 2, 5))
    return out.reshape(E * 512, 2048)


def make_in_maps(inp):
    inp = {k: np.asarray(v) for k, v in inp.items()}
    bc = lambda v: np.ascontiguousarray(np.broadcast_to(np.asarray(v, np.float32)[None, :], (128, v.shape[0])))
    shared = {
        "w_ada": np.ascontiguousarray(inp["w_ada"][0]),
        "b_ada_b": bc(inp["b_ada"][0]),
        "g_mix_b": bc(inp["g_mix"][0]),
        "g_ffn_b": bc(inp["g_ffn"][0]),
        "g_final_b": bc(inp["g_final"]),
        "w_in": np.ascontiguousarray(inp["w_in"][0]),
        "bd_a": host_blockdiag(inp["w_rg_a"][0]),
        "bd_x": host_blockdiag(inp["w_rg_x"][0]),
        "w_out": np.ascontiguousarray(inp["w_out"][0]),
        "w_router": np.ascontiguousarray(inp["w_router"][0]),
        "w_gu_r": host_wgu(inp["w_gu"][0]),
        "w_dn_r": host_wdn(inp["w_down"][0]),
        "b_gu_r": np.ascontiguousarray(inp["b_gu"][0].reshape(E, 16, 128).transpose(0, 2, 1).reshape(E * 128, 16)),
        "b_down": np.ascontiguousarray(inp["b_down"][0]),
        "consts": host_consts(inp),
    }
    maps = []
    for b in range(8):
        m = dict(shared)
        m["x"] = np.ascontiguousarray(inp["x"][b])
        cb = inp["c"][b].reshape(8, 128).T
        m["cB"] = np.ascontiguousarray(np.broadcast_to(cb[:, :, None], (128, 8, 128)).reshape(128, 1024)).astype(np.float32)
        maps.append(m)
    return maps


def kernel(**inputs):
    nc = build()
    maps = make_in_maps(inputs)
    res = run_bass_kernel_spmd(nc, maps, core_ids=list(range(8)))
    return np.stack([np.asarray(r["out"]) for r in res.results], axis=0).astype(np.float32)
```

```python
import numpy as np
import concourse.bass as bass
import concourse.mybir as mybir
from concourse.bass_utils import run_bass_kernel_spmd
from contextlib import ExitStack

F32 = mybir.dt.float32
F32R = mybir.dt.float32r
U32 = mybir.dt.uint32
I32 = mybir.dt.int32
ALU = mybir.AluOpType
AF = mybir.ActivationFunctionType

S = 2048
D = 1024
NT = 16
E = 32
EREG = 2048
BSZ = 256
NJ = BSZ // 128
NB = S * 4 // BSZ + E
NSLOT = E * EREG
BIG = 1000000.0
EPS = 1e-6
NEG = -30000.0

ENGS = ("tensor", "vector", "scalar", "gpsimd", "sync")
NDS = 40
SAME_ENG_SYNC = True

C_ID, C_ONE, C_U, C_MD, C_MP = 0, 128, 256, 384, 896
C_IOTA, C_RB, C_BR, C_SINK = 1408, 1440, 1472, 1504
C_CW, C_CB, C_BA, C_BX, C_LAM, C_GA, C_GL = 1512, 1528, 1532, 1536, 1540, 1544, 1548
C_PID, C_J = 1552, 1560
NCONST = 1624
ARENA = 52352


class T:
    __slots__ = ("w", "r")

    def __init__(self):
        self.w = None
        self.r = {}


class Prog:
    def __init__(self, nc, es):
        self.nc = nc
        self.q = {e: [] for e in ENGS}
        self.sem = {e: es.enter_context(nc.semaphore("sem_" + e)) for e in ENGS[:4]}
        self.cnt = {e: 0 for e in ENGS}
        self.dsem = [es.enter_context(nc.semaphore(f"dsem{i}")) for i in range(NDS)]
        self.dcnt = [0] * NDS
        self.dnext = 0
        self.seen = {}

    def _need(self, eng, key, val):
        if key == ("e", eng) and (eng == "tensor" or not SAME_ENG_SYNC):
            return
        if self.seen.get((eng, key), 0) >= val:
            return
        self.seen[(eng, key)] = val
        self.q[eng].append(("wait", key, val))

    def _deps(self, eng, reads, writes):
        for t in reads:
            if t.w:
                self._need(eng, *t.w)
        for t in writes:
            if t.w:
                self._need(eng, *t.w)
            for k, v in t.r.items():
                self._need(eng, k, v)

    def _mark(self, ev, reads, writes):
        for t in reads:
            if t.r.get(ev[0], 0) < ev[1]:
                t.r[ev[0]] = ev[1]
        for t in writes:
            t.w = ev
            t.r = {}

    def op(self, eng, fn, reads=(), writes=()):
        self._deps(eng, reads, writes)
        self.cnt[eng] += 1
        ev = (("e", eng), self.cnt[eng])
        self.q[eng].append(("op", fn))
        self._mark(ev, reads, writes)

    def dma(self, eng, fn, reads=(), writes=()):
        i = self.dnext
        self.dnext = (i + 1) % NDS
        key = ("d", i)
        if self.dcnt[i] > 0:
            self._need(eng, key, self.dcnt[i])
        self._deps(eng, reads, writes)
        self.dcnt[i] += 16
        ev = (key, self.dcnt[i])
        self.q[eng].append(("dma", fn, i))
        self._mark(ev, reads, writes)

    def barrier(self):
        for eng in ENGS:
            for i in range(NDS):
                if self.dcnt[i] > 0:
                    self._need(eng, ("d", i), self.dcnt[i])
            for e in ENGS[:4]:
                if self.cnt[e] > 0 and e != eng:
                    self._need(eng, ("e", e), self.cnt[e])

    def _semobj(self, key):
        return self.sem[key[1]] if key[0] == "e" else self.dsem[key[1]]

    def emit(self):
        with self.nc.Block() as block:
            for e in ENGS:
                if not self.q[e]:
                    continue

                def body(engh, e=e):
                    for item in self.q[e]:
                        if item[0] == "wait":
                            engh.wait_ge(self._semobj(item[1]), item[2])
                        elif item[0] == "op":
                            item[1](engh).then_inc(self.sem[e], 1)
                        else:
                            item[1](engh).then_inc(self.dsem[item[2]], 16)

                getattr(block, e)(body)


def build(debug=False, stop_after=99):
    nc = bass.Bass("TRN2", target_bir_lowering=False)

    def din(name, shape, dtype=F32):
        return nc.dram_tensor(name, shape, dtype, kind="ExternalInput").ap()

    skind = "ExternalOutput" if debug else "Internal"
    x_d = din("x", [S, D])
    cB_d = din("cB", [128, 8 * 128])
    wada_d = din("w_ada", [D, 6 * D])
    bada_d = din("b_ada_b", [128, 6 * D])
    gmix_d = din("g_mix_b", [128, D])
    gffn_d = din("g_ffn_b", [128, D])
    gfin_d = din("g_final_b", [128, D])
    win_d = din("w_in", [D, 1792])
    bda_d = din("bd_a", [128, 4 * 128])
    bdx_d = din("bd_x", [128, 4 * 128])
    wout_d = din("w_out", [D, D])
    wr_d = din("w_router", [D, E])
    wgur_d = din("w_gu_r", [E * 1024, 2048])
    wdnr_d = din("w_dn_r", [E * 512, 2048])
    bgur_d = din("b_gu_r", [E * 128, 16])
    bdn_d = din("b_down", [E, D])
    const_d = din("consts", [128, NCONST])
    out_d = nc.dram_tensor("out", [S, D], F32, kind="ExternalOutput").ap()
    x1_d = nc.dram_tensor("x1_s", [S, D], F32, kind=skind).ap()
    xs_d = nc.dram_tensor("xs_s", [NSLOT, D], F32, kind=skind).ap()
    ys_d = nc.dram_tensor("ys_s", [NSLOT, D], F32, kind=skind).ap()
    gt2_d = nc.dram_tensor("gt2_s", [128, D], F32, kind=skind).ap()
    dbg_d = nc.dram_tensor("dbg", [128, 8 * S], F32, kind="ExternalOutput").ap() if debug else None

    es = ExitStack()
    with es:
        P = Prog(nc, es)
        AR = es.enter_context(nc.sbuf_tensor("arena", [128, ARENA], F32))
        pbank = [es.enter_context(nc.psum_tensor(f"pb{i}", [128, 512], F32)) for i in range(8)]
        pT = [T() for _ in range(8)]
        pstate = [0]

        def bank():
            i = pstate[0]
            pstate[0] = (i + 1) % 8
            return pbank[i], pT[i]

        ar_addr = [m.memorylocations[0].addr for m in nc.allocations if m.name == "arena_set"][0]
        vcount = [0]

        def V(off, n, dt=F32):
            assert off + n <= ARENA
            vcount[0] += 1
            t = nc.alloc_sbuf_tensor_at(f"v{vcount[0]}", [128, n], dt, offset=ar_addr + off * 4)
            return t[:, :]

        def V3(off, a, b, dt=F32):
            return V(off, a * b, dt).rearrange("p (a b) -> p a b", a=a)

        pe_mode = [128]

        def mm(out, lhsT, rhs, start, stop, reads, writes, mode=128):
            if mode != pe_mode[0]:
                pe_mode[0] = mode
                if P.cnt["tensor"] > 0:
                    P.q["tensor"].append(("wait", ("e", "tensor"), P.cnt["tensor"]))
            P.op("tensor", lambda e: e.matmul(out, lhsT=lhsT, rhs=rhs, start=start, stop=stop), reads, writes)

        def tr(out, in_, ident, reads, writes):
            if pe_mode[0] != 128:
                pe_mode[0] = 128
                P.q["tensor"].append(("wait", ("e", "tensor"), P.cnt["tensor"]))
            P.op("tensor", lambda e: e.transpose(out=out, in_=in_, identity=ident), reads, writes)

        def act(out, in_, func, reads, writes, **kw):
            P.op("scalar", lambda e: e.activation(out=out, in_=in_, func=func, **kw), reads, writes)

        def tt(eng, out, in0, in1, op, reads, writes):
            P.op(eng, lambda e: e.tensor_tensor(out=out, in0=in0, in1=in1, op=op), reads, writes)

        def ts(eng, out, in0, s1, s2, op0, op1, reads, writes, **kw):
            if op1 is None:
                P.op(eng, lambda e: e.tensor_scalar(out=out, in0=in0, scalar1=s1, scalar2=None, op0=op0, **kw), reads, writes)
            else:
                P.op(eng, lambda e: e.tensor_scalar(out=out, in0=in0, scalar1=s1, scalar2=s2, op0=op0, op1=op1, **kw), reads, writes)

        def stt(out, in0, scalar, in1, op0, op1, reads, writes, **kw):
            P.op("vector", lambda e: e.scalar_tensor_tensor(out=out, in0=in0, scalar=scalar, in1=in1, op0=op0, op1=op1, **kw), reads, writes)

        def cp(eng, out, in_, reads, writes):
            if eng == "scalar":
                P.op("scalar", lambda e: e.copy(out=out, in_=in_), reads, writes)
            else:
                P.op(eng, lambda e: e.tensor_copy(out=out, in_=in_), reads, writes)

        def dma(eng, out, in_, reads, writes):
            P.dma(eng, lambda e: e.dma_start(out=out, in_=in_), reads, writes)

        CO = 0
        CR = 600
        SM = 1752
        MOD = 2112
        PH = 8256
        cT = T()
        crT = T()
        cot = V(CO, 600)
        crt = V(CR, 1152, F32R)

        def C(off, n):
            o = off if off < 384 else 384 + off - C_IOTA
            return cot[:, o:o + n]

        dma("sync", cot[:, 0:384], const_d[:, 0:384], [], [cT])
        dma("sync", cot[:, 384:600], const_d[:, C_IOTA:NCONST], [], [cT])
        dma("gpsimd", crt[:, 0:128], const_d[:, C_ID:C_ID + 128], [], [crT])
        dma("gpsimd", crt[:, 128:1152], const_d[:, C_MD:C_MD + 1024], [], [crT])
        identF = C(C_ID, 128)
        onesF = C(C_ONE, 128)
        UF = C(C_U, 128)
        identR = crt[:, 0:128]
        maskDR = crt[:, 128:640]
        maskPR = crt[:, 640:1152]
        sm = [SM]

        def small(n):
            o = sm[0]
            sm[0] += (n + 7) // 8 * 8
            assert sm[0] <= MOD
            return V(o, n), T()

        sinkexp, sinkexpT = small(8)
        sp8, sp8T = small(4)
        ls8, ls8T = small(4)
        gates_all, gatesT = small(64)
        destf_all, destfT = small(64)
        desti_o = sm[0]; sm[0] += 64
        desti_all = V(desti_o, 64, I32); destiT = T()
        cum, cumT = small(32)

        modrow = V(MOD, 6 * D)
        modT = [T() for _ in range(6)]
        msl = lambda j: modrow[:, j * D:(j + 1) * D]
        sh1b, s1b, gt1b, sh2b, s2b, gt2b = [msl(j) for j in range(6)]

        cBs = V3(PH, 8, 128, F32R); cBT = T()
        dma("gpsimd", cBs, cB_d.rearrange("p (a b) -> p a b", a=8), [], [cBT])
        for j in range(6):
            dma("sync", msl(j), bada_d[:, j * D:(j + 1) * D], [], [modT[j]])
        WA = [V3(PH + 1024 + k * 4096, 8, 512, F32R) for k in range(2)]
        WAT = [T(), T()]
        wada_v = wada_d.rearrange("(kc p) n -> p kc n", p=128)
        for g in range(12):
            k = g % 2
            dma("gpsimd", WA[k], wada_v[:, :, g * 512:(g + 1) * 512], [], [WAT[k]])
            pb, pt = bank()
            for kc in range(8):
                mm(pb[:, :], cBs[:, kc, :], WA[k][:, kc, :], kc == 0, kc == 7, [cBT, WAT[k]], [pt])
            mt = modT[g // 2]
            sl = modrow[:, g * 512:(g + 1) * 512]
            tt("vector", sl, pb[:, :], sl, ALU.add, [pt, mt], [mt])
        gtmp = V(PH + 1024 + 8192, D); gtmpT = T()
        dma("sync", gtmp, gmix_d, [], [gtmpT])
        stt(s1b, s1b, 1.0, gtmp, ALU.add, ALU.mult, [modT[1], gtmpT], [modT[1]])
        dma("sync", gtmp, gffn_d, [], [gtmpT])
        stt(s2b, s2b, 1.0, gtmp, ALU.add, ALU.mult, [modT[4], gtmpT], [modT[4]])
        dma("sync", gt2_d, gt2b, [modT[5]], [])
        act(sinkexp, C(C_SINK, 8), AF.Exp, [cT], [sinkexpT])
        act(sp8, C(C_LAM, 4), AF.Exp, [cT], [sp8T], scale=-1.0)
        act(sp8, sp8, AF.Ln, [sp8T], [sp8T], bias=1.0)
        ts("vector", sp8, sp8, 8.0, None, ALU.mult, None, [sp8T], [sp8T])
        ts("vector", ls8, sp8, -1.0, None, ALU.mult, None, [sp8T], [ls8T])
        P.barrier()

        HT = PH
        hT = V3(HT, 8, S, F32R)
        hTT = [T() for _ in range(4)]
        XS0 = PH + 16384
        xst = [V(XS0 + k * 1024, D) for k in range(2)]; xstT = [T(), T()]
        h1t = [V(XS0 + 2048 + k * 1024, D) for k in range(2)]; h1T = [T(), T()]
        ss_, ssT = small(2)
        rs_, rsT = small(2)

        def rms_rstd(src, srcT, junk, junkT, ss, ssT_, rs, rsT_, n):
            act(junk, src, AF.Square, [srcT], [junkT, ssT_], accum_out=ss)
            act(rs, ss, AF.Sqrt, [ssT_], [rsT_], scale=1.0 / n, bias=EPS)
            P.op("vector", lambda e: e.reciprocal(out=rs, in_=rs), [rsT_], [rsT_])

        def transpose8(src, srcT, dstfn, dstT, k):
            for half in range(2):
                pb, pt = bank()
                for q in range(4):
                    kc = half * 4 + q
                    tr(pb[:, q * 128:(q + 1) * 128], src[:, kc * 128:(kc + 1) * 128], identF, [srcT, cT], [pt])
                dst = dstfn(half)
                cp("scalar" if (half + k) % 2 == 0 else "vector", dst, pb[:, :].rearrange("p (a b) -> p a b", a=4), [pt], [dstT])

        for i in range(NT):
            k = i % 2
            dma("sync", xst[k], x_d[i * 128:(i + 1) * 128, :], [], [xstT[k]])
            rms_rstd(xst[k], xstT[k], h1t[k], h1T[k], ss_[:, k:k + 1], ssT, rs_[:, k:k + 1], rsT, D)
            stt(h1t[k], xst[k], rs_[:, k:k + 1], s1b, ALU.mult, ALU.mult, [xstT[k], rsT, modT[1]], [h1T[k]])
            tt("gpsimd", h1t[k], h1t[k], sh1b, ALU.add, [h1T[k], modT[0]], [h1T[k]])
            transpose8(h1t[k], h1T[k], lambda half, i=i: hT[:, half * 4:(half + 1) * 4, i * 128:(i + 1) * 128], hTT[i // 4], k)
        P.barrier()
        if stop_after == 1:
            return finish_debug(nc, P, dbg_d, hT.bitcast(F32).rearrange('p a b -> p (a b)'), out_d)

        WI0 = PH + 16384
        WI = [V3(WI0 + k * 1024, 8, 128, F32R) for k in range(2)]; WIT = [T(), T()]
        wi_state = [0]
        BD0 = WI0 + 2048
        bdA = V3(BD0, 4, 128, F32R); bdX = V3(BD0 + 512, 4, 128, F32R); bdT = T()
        dma("gpsimd", bdA, bda_d.rearrange("p (a b) -> p a b", a=4), [], [bdT])
        dma("gpsimd", bdX, bdx_d.rearrange("p (a b) -> p a b", a=4), [], [bdT])
        LB0 = BD0 + 1024
        LBR = [V(LB0 + k * S, S, F32R) if k == 1 else None for k in range(8)]
        LB = [LBR[k].bitcast(F32) if k == 1 else V(LB0 + k * S, S) for k in range(8)]
        LBT = [T() for _ in range(8)]
        LRU0 = LB0 + 8 * S
        assert LRU0 + 4 * S <= ARENA, LRU0 + 4 * S
        lruT = V3(LRU0, 4, S, F32R)
        lru0 = lruT.bitcast(F32)
        lruTT = [T() for _ in range(4)]
        win_v = win_d.rearrange("(kc p) n -> p kc n", p=128)
        evac_state = [0]

        def load_wi(col_specs):
            k = wi_state[0]
            wi_state[0] = 1 - k
            for (dst0, c0, n) in col_specs:
                dma("gpsimd", WI[k][:, :, dst0:dst0 + n], win_v[:, :, c0:c0 + n], [], [WIT[k]])
            return WI[k], WIT[k]

        def inproj_fm(w, wT_, dstfn, dstT):
            for tb in range(4):
                pb, pt = bank()
                for kc in range(8):
                    mm(pb[:, :], w[:, kc, :], hT[:, kc, tb * 512:(tb + 1) * 512], kc == 0, kc == 7, [wT_, hTT[tb]], [pt])
                evac_state[0] += 1
                cp("scalar" if evac_state[0] % 2 == 0 else "vector", dstfn(tb), pb[:, :], [pt], [dstT])

        cw = lambda c, k: C(C_CW + c * 4 + k, 1)
        colc = lambda base, c: C(base + c, 1)
        acc = LB[7]; accT = LBT[7]
        for c in range(4):
            w, wT_ = load_wi([(0, 768 + c * 128, 128)])
            inproj_fm(w, wT_, lambda tb: LB[0][:, tb * 512:(tb + 1) * 512], LBT[0])
            w, wT_ = load_wi([(0, 1280 + c * 128, 128)])
            inproj_fm(w, wT_, lambda tb: LB[2][:, tb * 512:(tb + 1) * 512], LBT[2])
            xr, xc, xg = LB[0], LB[1], LB[2]
            ts("vector", LBR[1], xr, cw(c, 3), colc(C_CB, c), ALU.mult, ALU.add, [LBT[0], cT], [LBT[1]])
            for sh in (1, 2, 3):
                stt(LBR[1][:, sh:], xr[:, :S - sh], cw(c, 3 - sh), xc[:, sh:], ALU.mult, ALU.add, [LBT[0], LBT[1], cT], [LBT[1]])
            for gi, (bd, bcol, dst) in enumerate(((bdA, C_BA, 3), (bdX, C_BX, 4))):
                for tb in range(4):
                    pb, pt = bank()
                    mm(pb[:, :], bd[:, c, :], LBR[1][:, tb * 512:(tb + 1) * 512], True, True, [bdT, LBT[1]], [pt])
                    act(LB[dst][:, tb * 512:(tb + 1) * 512], pb[:, :], AF.Sigmoid, [pt, cT], [LBT[dst]], bias=colc(bcol, c))
            r, ig = LB[3], LB[4]
            act(LB[6], r, AF.Tanh, [LBT[3], sp8T], [LBT[6]], scale=sp8[:, c:c + 1])
            act(LB[3], r, AF.Exp, [LBT[3], ls8T], [LBT[3]], scale=ls8[:, c:c + 1])
            a = LB[3]
            tt("gpsimd", LB[5], a, a, ALU.mult, [LBT[3]], [LBT[5]])
            stt(LB[5], LB[5], 1.0, LB[6], ALU.add, ALU.mult, [LBT[5], LBT[6]], [LBT[5]])
            act(LB[5], LB[5], AF.Sqrt, [LBT[5]], [LBT[5]])
            tt("gpsimd", LB[4], ig, xc, ALU.mult, [LBT[4], LBT[1]], [LBT[4]])
            tt("vector", LB[4], LB[4], LB[5], ALU.mult, [LBT[4], LBT[5]], [LBT[4]])
            P.op("vector", lambda e, a=a: e.tensor_tensor_scan(out=LB[0], data0=a, data1=LB[4], initial=0.0, op0=ALU.mult, op1=ALU.add),
                 [LBT[3], LBT[4], LBT[0]], [LBT[0]])
            act(LB[6], xg, AF.Square, [LBT[2]], [LBT[6]])
            ts("gpsimd", LB[6], LB[6], 0.044715, 1.0, ALU.mult, ALU.add, [LBT[6]], [LBT[6]])
            tt("gpsimd", LB[6], LB[6], xg, ALU.mult, [LBT[6], LBT[2]], [LBT[6]])
            act(LB[6], LB[6], AF.Sigmoid, [LBT[6]], [LBT[6]], scale=1.5957691216057308)
            tt("gpsimd", LB[6], LB[6], xg, ALU.mult, [LBT[6], LBT[2]], [LBT[6]])
            tt("vector", lruT[:, c, :], LB[0], LB[6], ALU.mult, [LBT[0], LBT[6]], [lruTT[c]])
            if c == 0:
                tt("gpsimd", acc, lru0[:, c, :], lru0[:, c, :], ALU.mult, [lruTT[c]], [accT])
            else:
                tt("gpsimd", LB[5], lru0[:, c, :], lru0[:, c, :], ALU.mult, [lruTT[c]], [LBT[5]])
                tt("gpsimd", acc, acc, LB[5], ALU.add, [accT, LBT[5]], [accT])
        for tb in range(4):
            pb, pt = bank()
            mm(pb[:, :], onesF, acc[:, tb * 512:(tb + 1) * 512], True, True, [cT, accT], [pt])
            act(LB[6][:, tb * 512:(tb + 1) * 512], pb[:, :], AF.Sqrt, [pt], [LBT[6]], scale=1.0 / 512, bias=EPS)
        P.op("vector", lambda e: e.reciprocal(out=LB[6], in_=LB[6]), [LBT[6]], [LBT[6]])
        for c in range(4):
            stt(lruT[:, c, :], lru0[:, c, :], colc(C_GL, c), LB[6], ALU.mult, ALU.mult, [lruTT[c], LBT[6], cT], [lruTT[c]])
        P.barrier()
        if stop_after == 2:
            return finish_debug(nc, P, dbg_d, lruT.bitcast(F32).rearrange('p a b -> p (a b)'), out_d)

        Q0 = LB0
        qT = V3(Q0, 4, S, F32R); qTT = [T() for _ in range(4)]
        kk = V3(Q0 + 4 * S, 2, S, F32R); kkT = [T(), T()]
        VA0 = Q0 + 6 * S
        vflat = V(VA0, NT * 132, F32R)
        vaug = vflat.rearrange("p (t g d) -> p t g d", t=NT, g=2)
        vT = T()
        assert VA0 + NT * 132 <= LRU0
        ones4 = onesF[:, 0:32].rearrange("p (t g d) -> p t g d", t=NT, g=2)
        cp("vector", vaug[:, :, :, 64:65], ones4, [cT], [vT])
        ts("vector", vaug[:, :, :, 65:66], ones4, 0.0, None, ALU.mult, None, [cT], [vT])
        for c in range(4):
            w, wT_ = load_wi([(0, c * 128, 128)])
            inproj_fm(w, wT_, lambda tb, c=c: qT[:, c, tb * 512:(tb + 1) * 512], qTT[c])
        for g in range(2):
            w, wT_ = load_wi([(0, 512 + g * 64, 64), (64, 512 + g * 64, 64)])
            inproj_fm(w, wT_, lambda tb, g=g: kk[:, g, tb * 512:(tb + 1) * 512], kkT[g])
        w, wT_ = load_wi([(0, 640, 128)])
        for i in range(NT):
            pb, pt = bank()
            for kc in range(8):
                mm(pb[:, 0:128], hT[:, kc, i * 128:(i + 1) * 128], w[:, kc, :], kc == 0, kc == 7, [wT_, hTT[i // 4]], [pt])
            cp("scalar" if i % 2 == 0 else "vector", vaug[:, i, :, 0:64], pb[:, 0:128].rearrange("p (g d) -> p g d", g=2), [pt], [vT])
        P.barrier()

        AT0 = PH
        attnT = V3(AT0, 4, S, F32R); attnTT = T()
        ET0 = PH + 4 * S
        eT = [[V(ET0 + (s * 2 + kbi) * 512, 512, F32R) for kbi in range(2)] for s in range(2)]
        eTT = [[T(), T()], [T(), T()]]
        AN0 = ET0 + 2048
        attn_t = [V(AN0 + k * 512, 512) for k in range(2)]; attn_tT = [T(), T()]
        junk512 = V(AN0 + 1024, 512); junk512T = T()
        den, denT = small(8)
        ssa, ssaT = small(2)
        rsa, rsaT = small(2)
        scale = 0.125
        for n in range(NT):
            k = n % 2
            for g in range(2):
                s = g
                kbs = ([n - 1] if n > 0 else []) + [n]
                for kbi, kb in enumerate(kbs):
                    diag = (kb == n)
                    for half in range(2):
                        pb, pt = bank()
                        mm(pb[:, 0:256], identR, (maskDR if diag else maskPR)[:, 0:256], True, False, [crT], [pt])
                        for u_ in range(2):
                            hh = u_ * 2 + half
                            c = 2 * g + hh // 2
                            h0 = half * 64
                            mm(pb[:, u_ * 128:(u_ + 1) * 128], kk[h0:h0 + 64, g, kb * 128:(kb + 1) * 128],
                               qT[h0:h0 + 64, c, n * 128:(n + 1) * 128], False, u_ == 1, [kkT[g], qTT[c]], [pt], mode=64)
                        act(eT[s][kbi][:, half * 256:(half + 1) * 256], pb[:, 0:256], AF.Exp, [pt], [eTT[s][kbi]], scale=scale)
                pb, pt = bank()
                for hh in range(4):
                    for kbi, kb in enumerate(kbs):
                        mm(pb[:, hh * 66:(hh + 1) * 66], eT[s][kbi][:, ((hh % 2) * 2 + hh // 2) * 128:((hh % 2) * 2 + hh // 2 + 1) * 128], vaug[:, kb, g, :],
                           kbi == 0, kbi == len(kbs) - 1, [eTT[s][kbi], vT], [pt])
                pv = pb[:, 0:264].rearrange("p (h d) -> p h d", h=4)
                tt("vector", den[:, g * 4:(g + 1) * 4], pv[:, :, 64], sinkexp[:, g * 4:(g + 1) * 4], ALU.add, [pt, sinkexpT], [denT])
                P.op("vector", lambda e, g=g: e.reciprocal(out=den[:, g * 4:(g + 1) * 4], in_=den[:, g * 4:(g + 1) * 4]), [denT], [denT])
                for hh in range(4):
                    hd = g * 4 + hh
                    if hh % 2 == 0:
                        ts("vector", attn_t[k][:, hd * 64:(hd + 1) * 64], pv[:, hh, 0:64], den[:, hd:hd + 1], None, ALU.mult, None, [pt, denT], [attn_tT[k]])
                    else:
                        act(attn_t[k][:, hd * 64:(hd + 1) * 64], pv[:, hh, 0:64], AF.Copy, [pt, denT], [attn_tT[k]], scale=den[:, hd:hd + 1])
            rms_rstd(attn_t[k], attn_tT[k], junk512, junk512T, ssa[:, k:k + 1], ssaT, rsa[:, k:k + 1], rsaT, 512)
            ts("vector", attn_t[k], attn_t[k], rsa[:, k:k + 1], None, ALU.mult, None, [attn_tT[k], rsaT], [attn_tT[k]])
            pb, pt = bank()
            for c in range(4):
                tr(pb[:, c * 128:(c + 1) * 128], attn_t[k][:, c * 128:(c + 1) * 128], identF, [attn_tT[k], cT], [pt])
            for c in range(4):
                if c % 2 == 0:
                    ts("vector", attnT[:, c, n * 128:(n + 1) * 128], pb[:, c * 128:(c + 1) * 128], colc(C_GA, c), None, ALU.mult, None, [pt, cT], [attnTT])
                else:
                    act(attnT[:, c, n * 128:(n + 1) * 128], pb[:, c * 128:(c + 1) * 128], AF.Copy, [pt, cT], [attnTT], scale=colc(C_GA, c))
        P.barrier()
        if stop_after == 3:
            return finish_debug(nc, P, dbg_d, attnT.bitcast(F32).rearrange('p a b -> p (a b)'), out_d)

        WO0 = PH + 4 * S
        wo = V3(WO0, 8, D, F32R); woT = T()
        wout_v = wout_d.rearrange("(kc p) n -> p kc n", p=128)
        for hf in range(2):
            dma("gpsimd", wo[:, :, hf * 512:(hf + 1) * 512], wout_v[:, :, hf * 512:(hf + 1) * 512], [], [woT])
        T0 = WO0 + 8 * D
        xt = [V(T0 + k * D, D) for k in range(2)]; xtT = [T(), T()]
        x1t = [V(T0 + 2 * D + k * D, D) for k in range(2)]; x1T = [T(), T()]
        h2t = [V(T0 + 4 * D + k * D, D) for k in range(2)]; h2T_ = [T(), T()]
        h2Tr = [V3(T0 + 6 * D + k * D, 8, 128) for k in range(2)]; h2TrT = [T(), T()]
        WR0 = T0 + 8 * D
        wr = V3(WR0, 8, E); wrT = T()
        dma("sync", wr, wr_d.rearrange("(kc p) n -> p kc n", p=128), [], [wrT])
        R0 = WR0 + 8 * E
        assert R0 + 1024 <= LB0 + 8 * S
        rsm = [R0]

        def rsmall(n):
            o = rsm[0]
            rsm[0] += (n + 7) // 8 * 8
            return V(o, n), T()

        lg, lgT = rsmall(32)
        top8, top8T = rsmall(8)
        idx8_o = rsm[0]; rsm[0] += 8
        idx8 = V(idx8_o, 8, U32); idx8T = T()
        idxf, idxfT = rsmall(4)
        negm, negmT = rsmall(1)
        e4, e4T = rsmall(4)
        ssum, ssumT = rsmall(1)
        mask, maskT = rsmall(32)
        rkp, rkpT = rsmall(32)
        ohj, ohjT = rsmall(32)
        ss2, ss2T = rsmall(2)
        rs2, rs2T = rsmall(2)
        xsT_d = T()
        x1dT = T()
        mergedT = lambda kc: (attnT[:, kc, :] if kc < 4 else lruT[:, kc - 4, :])
        mT = lambda kc: (attnTT if kc < 4 else lruTT[kc - 4])
        for i in range(NT):
            k = i % 2
            dma("sync", xt[k], x_d[i * 128:(i + 1) * 128, :], [], [xtT[k]])
            for hf in range(2):
                pb, pt = bank()
                for kc in range(8):
                    mm(pb[:, :], mergedT(kc)[:, i * 128:(i + 1) * 128], wo[:, kc, hf * 512:(hf + 1) * 512], kc == 0, kc == 7, [mT(kc), woT], [pt])
                sl = slice(hf * 512, (hf + 1) * 512)
                tt("vector", x1t[k][:, sl], pb[:, :], gt1b[:, sl], ALU.mult, [pt, modT[2]], [x1T[k]])
                tt("gpsimd", x1t[k][:, sl], x1t[k][:, sl], xt[k][:, sl], ALU.add, [x1T[k], xtT[k]], [x1T[k]])
            dma("sync", x1_d[i * 128:(i + 1) * 128, :], x1t[k], [x1T[k]], [])
            rms_rstd(x1t[k], x1T[k], h2t[k], h2T_[k], ss2[:, k:k + 1], ss2T, rs2[:, k:k + 1], rs2T, D)
            stt(h2t[k], x1t[k], rs2[:, k:k + 1], s2b, ALU.mult, ALU.mult, [x1T[k], rs2T, modT[4]], [h2T_[k]])
            tt("gpsimd", h2t[k], h2t[k], sh2b, ALU.add, [h2T_[k], modT[3]], [h2T_[k]])
            transpose8(h2t[k], h2T_[k], lambda half, k=k: h2Tr[k][:, half * 4:(half + 1) * 4, :], h2TrT[k], k)
            pb, pt = bank()
            for kc in range(8):
                mm(pb[:, 0:E], h2Tr[k][:, kc, :], wr[:, kc, :], kc == 0, kc == 7, [h2TrT[k], wrT], [pt])
            tt("vector", lg, pb[:, 0:E], C(C_BR, E), ALU.add, [pt, cT], [lgT])
            P.op("vector", lambda e: e.max(out=top8, in_=lg), [lgT], [top8T])
            P.op("vector", lambda e: e.max_index(out=idx8, in_max=top8, in_values=lg), [lgT, top8T], [idx8T])
            cp("vector", idxf, idx8[:, 0:4], [idx8T], [idxfT])
            ts("vector", negm, top8[:, 0:1], -1.0, None, ALU.mult, None, [top8T], [negmT])
            act(e4, top8[:, 0:4], AF.Exp, [top8T, negmT], [e4T, ssumT], bias=negm, accum_out=ssum)
            P.op("vector", lambda e: e.reciprocal(out=ssum, in_=ssum), [ssumT], [ssumT])
            ts("vector", gates_all[:, i * 4:(i + 1) * 4], e4, ssum, None, ALU.mult, None, [e4T, ssumT], [gatesT])
            ts("vector", mask, lg, top8[:, 3:4], None, ALU.is_ge, None, [lgT, top8T], [maskT])
            pb, pt = bank()
            if i > 0:
                mm(pb[:, 0:E], onesF, cum, True, False, [cT, cumT], [pt])
            mm(pb[:, 0:E], UF, mask, i == 0, True, [cT, maskT], [pt])
            tt("vector", rkp, pb[:, 0:E], C(C_RB, E), ALU.add, [pt, cT], [rkpT])
            if i == 0:
                cp("vector", cum, mask, [maskT], [cumT])
            else:
                tt("vector", cum, cum, mask, ALU.add, [cumT, maskT], [cumT])
            for j in range(4):
                stt(ohj, C(C_IOTA, E), idxf[:, j:j + 1], rkp, ALU.is_equal, ALU.mult, [cT, idxfT, rkpT], [ohjT, destfT],
                    accum_out=destf_all[:, i * 4 + j:i * 4 + j + 1])
            cp("vector", desti_all[:, i * 4:(i + 1) * 4], destf_all[:, i * 4:(i + 1) * 4], [destfT], [destiT])
            for j in range(4):
                P.dma("gpsimd", lambda e, i=i, j=j, k=k: e.indirect_dma_start(
                    out=xs_d, out_offset=bass.IndirectOffsetOnAxis(ap=desti_all[:, i * 4 + j:i * 4 + j + 1], axis=0),
                    in_=h2t[k], in_offset=None), [h2T_[k], destiT], [])
        cntb, cntbT = rsmall(32)
        nblk, nblkT = rsmall(32)
        pend, pendT = rsmall(32)
        pst, pstT = rsmall(32)
        ej, ejT = rsmall(NB)
        pstj, pstjT = rsmall(NB)
        tmpj, tmpjT = rsmall(NB)
        sj, sjT = rsmall(NB)
        rowj, rowjT = rsmall(NB)
        valid, validT = rsmall(NB)
        skp, skpT = rsmall(NB)
        ebW, ebWT = rsmall(NB)
        ebD, ebDT = rsmall(NB)
        idxW = V(MOD, NB * 8, I32).rearrange("p (j k) -> p j k", k=8)
        idxD = V(MOD + NB * 8, NB * 4, I32).rearrange("p (j k) -> p j k", k=4)
        idxX = V(MOD + NB * 12, NB * 2, I32).rearrange("p (j k) -> p j k", k=2)
        idxB = V(MOD + NB * 14, NB, I32)
        idxBd = V(MOD + NB * 15, NB, I32)
        tblT = T()
        jrow = C(C_J, NB)
        pid = C(C_PID, 1)
        pb, pt = bank()
        mm(pb[:, 0:E], onesF, cum, True, True, [cT, cumT], [pt])
        cp("vector", cntb, pb[:, 0:E], [pt], [cntbT])
        ts("vector", nblk, cntb, 0.0, None, ALU.is_gt, None, [cntbT], [nblkT])
        for sx in range(1, EREG // BSZ):
            stt(nblk, cntb, float(BSZ * sx), nblk, ALU.is_gt, ALU.add, [cntbT, nblkT], [nblkT])
        P.op("vector", lambda e: e.tensor_tensor_scan(out=pend, data0=onesF[:, 0:E], data1=nblk, initial=0.0, op0=ALU.mult, op1=ALU.add),
             [cT, nblkT], [pendT])
        tt("vector", pst, pend, nblk, ALU.subtract, [pendT, nblkT], [pstT])
        ts("vector", ej, jrow, pend[:, 0:1], None, ALU.is_ge, None, [cT, pendT], [ejT])
        for e_ in range(1, E):
            stt(ej, jrow, pend[:, e_:e_ + 1], ej, ALU.is_ge, ALU.add, [cT, pendT, ejT], [ejT])
        ts("vector", valid, ej, E - 0.5, None, ALU.is_lt, None, [ejT], [validT])
        ts("vector", ej, ej, float(E - 1), None, ALU.min, None, [ejT], [ejT])
        for e_ in range(E):
            ts("vector", tmpj, ej, float(e_), None, ALU.is_equal, None, [ejT], [tmpjT])
            if e_ == 0:
                ts("vector", pstj, tmpj, pst[:, 0:1], None, ALU.mult, None, [tmpjT, pstT], [pstjT])
            else:
                stt(pstj, tmpj, pst[:, e_:e_ + 1], pstj, ALU.mult, ALU.add, [tmpjT, pstT, pstjT], [pstjT])
        tt("vector", sj, jrow, pstj, ALU.subtract, [cT, pstjT], [sjT])
        ts("vector", skp, sj, 0.0, None, ALU.is_equal, None, [sjT], [skpT])
        ts("vector", skp, skp, -BIG, BIG, ALU.mult, ALU.add, [skpT], [skpT])
        ts("vector", tmpj, sj, float(BSZ), None, ALU.mult, None, [sjT], [tmpjT])
        stt(rowj, ej, float(EREG), tmpj, ALU.mult, ALU.add, [ejT, tmpjT], [rowjT])
        ts("vector", rowj, rowj, -BIG, None, ALU.add, None, [rowjT], [rowjT])
        tt("vector", rowj, rowj, valid, ALU.mult, [rowjT, validT], [rowjT])
        ts("vector", rowj, rowj, BIG, pid, ALU.add, ALU.add, [rowjT, cT], [rowjT])
        stt(ebW, ej, 1024.0, skp, ALU.mult, ALU.add, [ejT, skpT], [ebWT])
        ts("vector", ebW, ebW, pid, None, ALU.add, None, [ebWT, cT], [ebWT])
        stt(ebD, ej, 512.0, skp, ALU.mult, ALU.add, [ejT, skpT], [ebDT])
        ts("vector", ebD, ebD, pid, None, ALU.add, None, [ebDT, cT], [ebDT])
        for k_ in range(8):
            ts("vector", idxW[:, :, k_], ebW, float(k_ * 128), None, ALU.add, None, [ebWT], [tblT])
        for k_ in range(4):
            ts("vector", idxD[:, :, k_], ebD, float(k_ * 128), None, ALU.add, None, [ebDT], [tblT])
        for k_ in range(2):
            ts("vector", idxX[:, :, k_], rowj, float(k_ * 128), None, ALU.add, None, [rowjT], [tblT])
        ts("vector", idxB, ej, 128.0, pid, ALU.mult, ALU.add, [ejT, cT], [tblT])
        cp("vector", idxBd, ej, [ejT], [tblT])
        P.barrier()
        if stop_after == 4:
            return finish_debug(nc, P, dbg_d, None, out_d)

        WSg = [V3(PH + k * 2048, 4, 512, F32R) for k in range(8)]; WSgT = [T() for _ in range(8)]
        WSd = [V3(PH + (8 + k) * 2048, 4, 512, F32R) for k in range(4)]; WSdT = [T() for _ in range(4)]
        X0 = PH + 12 * 2048
        xsb = [V3(X0 + k * NJ * D, NJ, D) for k in range(2)]; xsbT = [T(), T()]
        XT0 = X0 + 2 * NJ * D
        xsT = [V3(XT0 + k * 8 * BSZ, 8, BSZ, F32R) for k in range(2)]; xsTT = [T(), T()]
        A0 = XT0 + 2 * 8 * BSZ
        actT = V3(A0, 8, BSZ, F32R); actTT = T()
        Y0 = A0 + 8 * BSZ
        yb = [V3(Y0 + k * NJ * D, NJ, D) for k in range(2)]; ybT = [T(), T()]
        G0 = Y0 + 2 * NJ * D
        gsb = [V(G0 + k * BSZ, BSZ) for k in range(6)]; gsbT = [T() for _ in range(6)]
        BG0 = G0 + 6 * BSZ
        bgub = [V(BG0 + k * 16, 16) for k in range(2)]; bgubT = [T(), T()]
        BD0_ = BG0 + 32
        bdnb = [V(BD0_ + k * D, D) for k in range(2)]; bdnbT = [T(), T()]
        assert BD0_ + 2 * D <= ARENA, BD0_ + 2 * D

        bregs = {}

        def breg(e, bound):
            if bound not in bregs:
                bregs[bound] = e.to_reg(bound)
            return bregs[bound]

        def igather(out, src, idx_ap, bound, reads, writes):
            P.dma("gpsimd", lambda e: e.indirect_dma_start(out=out, out_offset=None, in_=src,
                  in_offset=bass.IndirectOffsetOnAxis(ap=idx_ap, axis=0), bounds_check=breg(e, bound), oob_is_err=False), reads, writes)

        def load_block_small(j):
            k = j % 2
            for jj in range(NJ):
                igather(xsb[k][:, jj, :], xs_d, idxX[:, j, jj:jj + 1], NSLOT - 1, [tblT], [xsbT[k]])
            igather(bgub[k], bgur_d, idxB[:, j:j + 1], E * 128 - 1, [tblT], [bgubT[k]])
            igather(bdnb[k], bdn_d, idxBd[:, j:j + 1], E - 1, [tblT], [bdnbT[k]])

        def load_wg(j, k_):
            igather(WSg[k_].rearrange("p a b -> p (a b)"), wgur_d, idxW[:, j, k_:k_ + 1], E * 1024 - 1, [tblT], [WSgT[k_]])

        def load_wd(j, k_):
            igather(WSd[k_].rearrange("p a b -> p (a b)"), wdnr_d, idxD[:, j, k_:k_ + 1], E * 512 - 1, [tblT], [WSdT[k_]])

        load_block_small(0)
        for k_ in range(8):
            load_wg(0, k_)
        for k_ in range(4):
            load_wd(0, k_)
        for j in range(NB):
            k = j % 2
            for kc in range(8):
                pb, pt = bank()
                for jj in range(NJ):
                    tr(pb[:, jj * 128:(jj + 1) * 128], xsb[k][:, jj, kc * 128:(kc + 1) * 128], identF, [xsbT[k], cT], [pt])
                cp("scalar" if kc % 2 == 0 else "vector", xsT[k][:, kc, :], pb[:, 0:BSZ], [pt], [xsTT[k]])
            if j + 1 < NB:
                load_block_small(j + 1)
            for fc in range(8):
                qg = (fc // 4) * 2
                lc = (fc % 4) * 128
                pg, pgt = bank()
                for kc in range(8):
                    wi_ = qg * 2 + kc // 4
                    mm(pg[:, 0:BSZ], WSg[wi_][:, kc % 4, lc:lc + 128], xsT[k][:, kc, :], kc == 0, kc == 7, [WSgT[wi_], xsTT[k]], [pgt])
                pu, put = bank()
                for kc in range(8):
                    wi_ = (qg + 1) * 2 + kc // 4
                    mm(pu[:, 0:BSZ], WSg[wi_][:, kc % 4, lc:lc + 128], xsT[k][:, kc, :], kc == 0, kc == 7, [WSgT[wi_], xsTT[k]], [put])
                if fc % 4 == 3 and j + 1 < NB:
                    for k_ in range(qg * 2, qg * 2 + 4):
                        load_wg(j + 1, k_)
                g3 = (fc % 2) * 3
                gb, gbT = gsb[g3], gsbT[g3]
                ub, ubT = gsb[g3 + 1], gsbT[g3 + 1]
                sl_, slT = gsb[g3 + 2], gsbT[g3 + 2]
                ts("vector", gb, pg[:, 0:BSZ], bgub[k][:, fc:fc + 1], 7.0, ALU.add, ALU.min, [pgt, bgubT[k]], [gbT])
                ts("vector", ub, pu[:, 0:BSZ], bgub[k][:, 8 + fc:8 + fc + 1], 7.0, ALU.add, ALU.min, [put, bgubT[k]], [ubT])
                ts("vector", ub, ub, -7.0, 1.0, ALU.max, ALU.add, [ubT], [ubT])
                act(sl_, gb, AF.Silu, [gbT], [slT], scale=1.702)
                stt(actT[:, fc, :], sl_, 1.0 / 1.702, ub, ALU.mult, ALU.mult, [slT, ubT], [actTT])
            for hf in range(2):
                for jj in range(NJ):
                    pb, pt = bank()
                    for fc in range(8):
                        wi_ = hf * 2 + fc // 4
                        mm(pb[:, :], actT[:, fc, jj * 128:(jj + 1) * 128], WSd[wi_][:, fc % 4, :], fc == 0, fc == 7, [actTT, WSdT[wi_]], [pt])
                    sl = slice(hf * 512, (hf + 1) * 512)
                    tt("vector", yb[k][:, jj, sl], pb[:, :], bdnb[k][:, sl], ALU.add, [pt, bdnbT[k]], [ybT[k]])
                if j + 1 < NB:
                    load_wd(j + 1, hf * 2)
                    load_wd(j + 1, hf * 2 + 1)
            for jj in range(NJ):
                P.dma("gpsimd", lambda e, j=j, jj=jj, k=k: e.indirect_dma_start(
                    out=ys_d, out_offset=bass.IndirectOffsetOnAxis(ap=idxX[:, j, jj:jj + 1], axis=0),
                    in_=yb[k][:, jj, :], in_offset=None, bounds_check=breg(e, NSLOT - 1), oob_is_err=False), [ybT[k], tblT], [])
        P.barrier()
        if stop_after == 5:
            return finish_debug(nc, P, dbg_d, None, out_d)

        gt2s = V(PH, D); gt2sT = T()
        gfin = V(PH + D, D); gfinT = T()
        dma("sync", gt2s, gt2_d, [], [gt2sT])
        dma("sync", gfin, gfin_d, [], [gfinT])
        F0 = PH + 2 * D
        x1r = [V(F0 + k * D, D) for k in range(2)]; x1rT = [T(), T()]
        yg = [[V(F0 + 2 * D + (k * 4 + j) * D, D) for j in range(4)] for k in range(2)]
        ygT = [[T() for _ in range(4)] for _ in range(2)]
        ob = [V(F0 + 10 * D + k * D, D) for k in range(2)]; obT = [T(), T()]
        ss3, ss3T = small(2)
        rs3, rs3T = small(2)
        outT = T()
        for i in range(NT):
            k = i % 2
            dma("sync", x1r[k], x1_d[i * 128:(i + 1) * 128, :], [x1dT], [x1rT[k]])
            for j in range(4):
                P.dma("gpsimd", lambda e, i=i, j=j, k=k: e.indirect_dma_start(
                    out=yg[k][j], out_offset=None, in_=ys_d,
                    in_offset=bass.IndirectOffsetOnAxis(ap=desti_all[:, i * 4 + j:i * 4 + j + 1], axis=0)), [destiT], [ygT[k][j]])
            ts("vector", ob[k], yg[k][0], gates_all[:, i * 4:i * 4 + 1], None, ALU.mult, None, [ygT[k][0], gatesT], [obT[k]])
            for j in range(1, 4):
                stt(ob[k], yg[k][j], gates_all[:, i * 4 + j:i * 4 + j + 1], ob[k], ALU.mult, ALU.add, [ygT[k][j], gatesT, obT[k]], [obT[k]])
            tt("gpsimd", ob[k], ob[k], gt2s, ALU.mult, [obT[k], gt2sT], [obT[k]])
            tt("vector", x1r[k], x1r[k], ob[k], ALU.add, [x1rT[k], obT[k]], [x1rT[k]])
            rms_rstd(x1r[k], x1rT[k], ob[k], obT[k], ss3[:, k:k + 1], ss3T, rs3[:, k:k + 1], rs3T, D)
            stt(ob[k], x1r[k], rs3[:, k:k + 1], gfin, ALU.mult, ALU.mult, [x1rT[k], rs3T, gfinT], [obT[k]])
            dma("sync", out_d[i * 128:(i + 1) * 128, :], ob[k], [obT[k]], [])
        P.barrier()
        P.emit()
    return nc


def finish_debug(nc, P, dbg_d, src, out_d):
    if src is not None:
        n = src.shape[1]
        P.dma("sync", lambda e: e.dma_start(out=dbg_d[:, 0:n], in_=src), [], [])
    P.barrier()
    P.emit()
    return nc


def host_consts(inp):
    c = np.zeros((128, NCONST), np.float32)
    c[:, C_ID:C_ID + 128] = np.eye(128, dtype=np.float32)
    c[:, C_ONE:C_ONE + 128] = 1.0
    kk_, qq = np.meshgrid(np.arange(128), np.arange(128), indexing="ij")
    c[:, C_U:C_U + 128] = (kk_ < qq).astype(np.float32)
    md = np.where(kk_ <= qq, 0.0, NEG).astype(np.float32)
    mp = np.where(kk_ > qq, 0.0, NEG).astype(np.float32)
    c[:, C_MD:C_MD + 512] = np.tile(md, (1, 4))
    c[:, C_MP:C_MP + 512] = np.tile(mp, (1, 4))
    c[:, C_IOTA:C_IOTA + 32] = np.arange(32, dtype=np.float32)[None, :]
    c[:, C_RB:C_RB + 32] = (np.arange(32, dtype=np.float32) * EREG)[None, :]
    c[:, C_PID] = np.arange(128, dtype=np.float32)
    c[:, C_J:C_J + NB] = np.arange(NB, dtype=np.float32)[None, :]
    c[:, C_BR:C_BR + 32] = inp["b_router"][0][None, :]
    c[:, C_SINK:C_SINK + 8] = inp["sinks"][0][None, :]
    cwt = inp["conv_w"][0]
    c[:, C_CW:C_CW + 16] = cwt.T.reshape(4, 128, 4).transpose(1, 0, 2).reshape(128, 16)
    col = lambda v: np.asarray(v, np.float32).reshape(4, 128).T
    c[:, C_CB:C_CB + 4] = col(inp["conv_b"][0])
    c[:, C_BA:C_BA + 4] = col(inp["b_rg_a"][0].reshape(512))
    c[:, C_BX:C_BX + 4] = col(inp["b_rg_x"][0].reshape(512))
    c[:, C_LAM:C_LAM + 4] = col(inp["lru_lambda"][0])
    c[:, C_GA:C_GA + 4] = col(inp["g_attn_out"][0])
    c[:, C_GL:C_GL + 4] = col(inp["g_lru_out"][0])
    return c


def host_blockdiag(w):
    bd = np.zeros((128, 4, 128), np.float32)
    for c in range(4):
        bd[0:64, c, 0:64] = w[2 * c]
        bd[64:128, c, 64:128] = w[2 * c + 1]
    return bd.reshape(128, 512)


def host_wgu(w):
    w6 = w.reshape(E, 2, 4, 128, 4, 512)
    cg = [0, 2, 1, 3]
    out = np.empty((E, 4, 2, 128, 4, 512), np.float32)
    for q in range(4):
        out[:, q] = w6[:, :, :, :, cg[q], :].transpose(0, 1, 3, 2, 4)
    return out.reshape(E * 1024, 2048)


def host_wdn(w):
    w6 = w.reshape(E, 2, 4, 128, 2, 512)
    out = np.ascontiguousarray(w6.transpose(0, 4, 1, 3, 2, 5))
    return out.reshape(E * 512, 2048)


def make_in_maps(inp):
    inp = {k: np.asarray(v) for k, v in inp.items()}
    bc = lambda v: np.ascontiguousarray(np.broadcast_to(np.asarray(v, np.float32)[None, :], (128, v.shape[0])))
    shared = {
        "w_ada": np.ascontiguousarray(inp["w_ada"][0]),
        "b_ada_b": bc(inp["b_ada"][0]),
        "g_mix_b": bc(inp["g_mix"][0]),
        "g_ffn_b": bc(inp["g_ffn"][0]),
        "g_final_b": bc(inp["g_final"]),
        "w_in": np.ascontiguousarray(inp["w_in"][0]),
        "bd_a": host_blockdiag(inp["w_rg_a"][0]),
        "bd_x": host_blockdiag(inp["w_rg_x"][0]),
        "w_out": np.ascontiguousarray(inp["w_out"][0]),
        "w_router": np.ascontiguousarray(inp["w_router"][0]),
        "w_gu_r": host_wgu(inp["w_gu"][0]),
        "w_dn_r": host_wdn(inp["w_down"][0]),
        "b_gu_r": np.ascontiguousarray(inp["b_gu"][0].reshape(E, 16, 128).transpose(0, 2, 1).reshape(E * 128, 16)),
        "b_down": np.ascontiguousarray(inp["b_down"][0]),
        "consts": host_consts(inp),
    }
    maps = []
    for b in range(8):
        m = dict(shared)
        m["x"] = np.ascontiguousarray(inp["x"][b])
        cb = inp["c"][b].reshape(8, 128).T
        m["cB"] = np.ascontiguousarray(np.broadcast_to(cb[:, :, None], (128, 8, 128)).reshape(128, 1024)).astype(np.float32)
        maps.append(m)
    return maps


def kernel(**inputs):
    nc = build()
    maps = make_in_maps(inputs)
    res = run_bass_kernel_spmd(nc, maps, core_ids=list(range(8)))
    return np.stack([np.asarray(r["out"]) for r in res.results], axis=0).astype(np.float32)
```

```python
import numpy as np
import concourse.bass as bass
import concourse.mybir as mybir
from concourse.bass_utils import run_bass_kernel_spmd
from contextlib import ExitStack

F32 = mybir.dt.float32
F32R = mybir.dt.float32r
U32 = mybir.dt.uint32
I32 = mybir.dt.int32
ALU = mybir.AluOpType
AF = mybir.ActivationFunctionType

S = 2048
D = 1024
NT = 16
E = 32
EREG = 2048
BSZ = 256
NJ = BSZ // 128
NB = S * 4 // BSZ + E
NSLOT = E * EREG
BIG = 1000000.0
EPS = 1e-6
NEG = -30000.0

ENGS = ("tensor", "vector", "scalar", "gpsimd", "sync")
NDS = 40
SAME_ENG_SYNC = True

C_ID, C_ONE, C_U, C_MD, C_MP = 0, 128, 256, 384, 896
C_IOTA, C_RB, C_BR, C_SINK = 1408, 1440, 1472, 1504
C_CW, C_CB, C_BA, C_BX, C_LAM, C_GA, C_GL = 1512, 1528, 1532, 1536, 1540, 1544, 1548
C_PID, C_J = 1552, 1560
NCONST = 1624
ARENA = 52352


class T:
    __slots__ = ("w", "r")

    def __init__(self):
        self.w = None
        self.r = {}


class Prog:
    def __init__(self, nc, es):
        self.nc = nc
        self.q = {e: [] for e in ENGS}
        self.sem = {e: es.enter_context(nc.semaphore("sem_" + e)) for e in ENGS[:4]}
        self.cnt = {e: 0 for e in ENGS}
        self.dsem = [es.enter_context(nc.semaphore(f"dsem{i}")) for i in range(NDS)]
        self.dcnt = [0] * NDS
        self.dnext = 0
        self.seen = {}

    def _need(self, eng, key, val):
        if key == ("e", eng) and (eng == "tensor" or not SAME_ENG_SYNC):
            return
        if self.seen.get((eng, key), 0) >= val:
            return
        self.seen[(eng, key)] = val
        self.q[eng].append(("wait", key, val))

    def _deps(self, eng, reads, writes):
        for t in reads:
            if t.w:
                self._need(eng, *t.w)
        for t in writes:
            if t.w:
                self._need(eng, *t.w)
            for k, v in t.r.items():
                self._need(eng, k, v)

    def _mark(self, ev, reads, writes):
        for t in reads:
            if t.r.get(ev[0], 0) < ev[1]:
                t.r[ev[0]] = ev[1]
        for t in writes:
            t.w = ev
            t.r = {}

    def op(self, eng, fn, reads=(), writes=()):
        self._deps(eng, reads, writes)
        self.cnt[eng] += 1
        ev = (("e", eng), self.cnt[eng])
        self.q[eng].append(("op", fn))
        self._mark(ev, reads, writes)

    def dma(self, eng, fn, reads=(), writes=()):
        i = self.dnext
        self.dnext = (i + 1) % NDS
        key = ("d", i)
        if self.dcnt[i] > 0:
            self._need(eng, key, self.dcnt[i])
        self._deps(eng, reads, writes)
        self.dcnt[i] += 16
        ev = (key, self.dcnt[i])
        self.q[eng].append(("dma", fn, i))
        self._mark(ev, reads, writes)

    def barrier(self):
        for eng in ENGS:
            for i in range(NDS):
                if self.dcnt[i] > 0:
                    self._need(eng, ("d", i), self.dcnt[i])
            for e in ENGS[:4]:
                if self.cnt[e] > 0 and e != eng:
                    self._need(eng, ("e", e), self.cnt[e])

    def _semobj(self, key):
        return self.sem[key[1]] if key[0] == "e" else self.dsem[key[1]]

    def emit(self):
        with self.nc.Block() as block:
            for e in ENGS:
                if not self.q[e]:
                    continue

                def body(engh, e=e):
                    for item in self.q[e]:
                        if item[0] == "wait":
                            engh.wait_ge(self._semobj(item[1]), item[2])
                        elif item[0] == "op":
                            item[1](engh).then_inc(self.sem[e], 1)
                        else:
                            item[1](engh).then_inc(self.dsem[item[2]], 16)

                getattr(block, e)(body)


def build(debug=False, stop_after=99):
    nc = bass.Bass("TRN2", target_bir_lowering=False)

    def din(name, shape, dtype=F32):
        return nc.dram_tensor(name, shape, dtype, kind="ExternalInput").ap()

    skind = "ExternalOutput" if debug else "Internal"
    x_d = din("x", [S, D])
    cB_d = din("cB", [128, 8 * 128])
    wada_d = din("w_ada", [D, 6 * D])
    bada_d = din("b_ada_b", [128, 6 * D])
    gmix_d = din("g_mix_b", [128, D])
    gffn_d = din("g_ffn_b", [128, D])
    gfin_d = din("g_final_b", [128, D])
    win_d = din("w_in", [D, 1792])
    bda_d = din("bd_a", [128, 4 * 128])
    bdx_d = din("bd_x", [128, 4 * 128])
    wout_d = din("w_out", [D, D])
    wr_d = din("w_router", [D, E])
    wgur_d = din("w_gu_r", [E * 1024, 2048])
    wdnr_d = din("w_dn_r", [E * 512, 2048])
    bgur_d = din("b_gu_r", [E * 128, 16])
    bdn_d = din("b_down", [E, D])
    const_d = din("consts", [128, NCONST])
    out_d = nc.dram_tensor("out", [S, D], F32, kind="ExternalOutput").ap()
    x1_d = nc.dram_tensor("x1_s", [S, D], F32, kind=skind).ap()
    xs_d = nc.dram_tensor("xs_s", [NSLOT, D], F32, kind=skind).ap()
    ys_d = nc.dram_tensor("ys_s", [NSLOT, D], F32, kind=skind).ap()
    gt2_d = nc.dram_tensor("gt2_s", [128, D], F32, kind=skind).ap()
    dbg_d = nc.dram_tensor("dbg", [128, 8 * S], F32, kind="ExternalOutput").ap() if debug else None

    es = ExitStack()
    with es:
        P = Prog(nc, es)
        AR = es.enter_context(nc.sbuf_tensor("arena", [128, ARENA], F32))
        pbank = [es.enter_context(nc.psum_tensor(f"pb{i}", [128, 512], F32)) for i in range(8)]
        pT = [T() for _ in range(8)]
        pstate = [0]

        def bank():
            i = pstate[0]
            pstate[0] = (i + 1) % 8
            return pbank[i], pT[i]

        ar_addr = [m.memorylocations[0].addr for m in nc.allocations if m.name == "arena_set"][0]
        vcount = [0]

        def V(off, n, dt=F32):
            assert off + n <= ARENA
            vcount[0] += 1
            t = nc.alloc_sbuf_tensor_at(f"v{vcount[0]}", [128, n], dt, offset=ar_addr + off * 4)
            return t[:, :]

        def V3(off, a, b, dt=F32):
            return V(off, a * b, dt).rearrange("p (a b) -> p a b", a=a)

        pe_mode = [128]

        def mm(out, lhsT, rhs, start, stop, reads, writes, mode=128):
            if mode != pe_mode[0]:
                pe_mode[0] = mode
                if P.cnt["tensor"] > 0:
                    P.q["tensor"].append(("wait", ("e", "tensor"), P.cnt["tensor"]))
            P.op("tensor", lambda e: e.matmul(out, lhsT=lhsT, rhs=rhs, start=start, stop=stop), reads, writes)

        def tr(out, in_, ident, reads, writes):
            if pe_mode[0] != 128:
                pe_mode[0] = 128
                P.q["tensor"].append(("wait", ("e", "tensor"), P.cnt["tensor"]))
            P.op("tensor", lambda e: e.transpose(out=out, in_=in_, identity=ident), reads, writes)

        def act(out, in_, func, reads, writes, **kw):
            P.op("scalar", lambda e: e.activation(out=out, in_=in_, func=func, **kw), reads, writes)

        def tt(eng, out, in0, in1, op, reads, writes):
            P.op(eng, lambda e: e.tensor_tensor(out=out, in0=in0, in1=in1, op=op), reads, writes)

        def ts(eng, out, in0, s1, s2, op0, op1, reads, writes, **kw):
            if op1 is None:
                P.op(eng, lambda e: e.tensor_scalar(out=out, in0=in0, scalar1=s1, scalar2=None, op0=op0, **kw), reads, writes)
            else:
                P.op(eng, lambda e: e.tensor_scalar(out=out, in0=in0, scalar1=s1, scalar2=s2, op0=op0, op1=op1, **kw), reads, writes)

        def stt(out, in0, scalar, in1, op0, op1, reads, writes, **kw):
            P.op("vector", lambda e: e.scalar_tensor_tensor(out=out, in0=in0, scalar=scalar, in1=in1, op0=op0, op1=op1, **kw), reads, writes)

        def cp(eng, out, in_, reads, writes):
            if eng == "scalar":
                P.op("scalar", lambda e: e.copy(out=out, in_=in_), reads, writes)
            else:
                P.op(eng, lambda e: e.tensor_copy(out=out, in_=in_), reads, writes)

        def dma(eng, out, in_, reads, writes):
            P.dma(eng, lambda e: e.dma_start(out=out, in_=in_), reads, writes)

        CO = 0
        CR = 600
        SM = 1752
        MOD = 2112
        PH = 8256
        cT = T()
        crT = T()
        cot = V(CO, 600)
        crt = V(CR, 1152, F32R)

        def C(off, n):
            o = off if off < 384 else 384 + off - C_IOTA
            return cot[:, o:o + n]

        dma("sync", cot[:, 0:384], const_d[:, 0:384], [], [cT])
        dma("sync", cot[:, 384:600], const_d[:, C_IOTA:NCONST], [], [cT])
        dma("gpsimd", crt[:, 0:128], const_d[:, C_ID:C_ID + 128], [], [crT])
        dma("gpsimd", crt[:, 128:1152], const_d[:, C_MD:C_MD + 1024], [], [crT])
        identF = C(C_ID, 128)
        onesF = C(C_ONE, 128)
        UF = C(C_U, 128)
        identR = crt[:, 0:128]
        maskDR = crt[:, 128:640]
        maskPR = crt[:, 640:1152]
        sm = [SM]

        def small(n):
            o = sm[0]
            sm[0] += (n + 7) // 8 * 8
            assert sm[0] <= MOD
            return V(o, n), T()

        sinkexp, sinkexpT = small(8)
        sp8, sp8T = small(4)
        ls8, ls8T = small(4)
        gates_all, gatesT = small(64)
        destf_all, destfT = small(64)
        desti_o = sm[0]; sm[0] += 64
        desti_all = V(desti_o, 64, I32); destiT = T()
        cum, cumT = small(32)

        modrow = V(MOD, 6 * D)
        modT = [T() for _ in range(6)]
        msl = lambda j: modrow[:, j * D:(j + 1) * D]
        sh1b, s1b, gt1b, sh2b, s2b, gt2b = [msl(j) for j in range(6)]

        cBs = V3(PH, 8, 128, F32R); cBT = T()
        dma("gpsimd", cBs, cB_d.rearrange("p (a b) -> p a b", a=8), [], [cBT])
        for j in range(6):
            dma("sync", msl(j), bada_d[:, j * D:(j + 1) * D], [], [modT[j]])
        WA = [V3(PH + 1024 + k * 4096, 8, 512, F32R) for k in range(2)]
        WAT = [T(), T()]
        wada_v = wada_d.rearrange("(kc p) n -> p kc n", p=128)
        for g in range(12):
            k = g % 2
            dma("gpsimd", WA[k], wada_v[:, :, g * 512:(g + 1) * 512], [], [WAT[k]])
            pb, pt = bank()
            for kc in range(8):
                mm(pb[:, :], cBs[:, kc, :], WA[k][:, kc, :], kc == 0, kc == 7, [cBT, WAT[k]], [pt])
            mt = modT[g // 2]
            sl = modrow[:, g * 512:(g + 1) * 512]
            tt("vector", sl, pb[:, :], sl, ALU.add, [pt, mt], [mt])
        gtmp = V(PH + 1024 + 8192, D); gtmpT = T()
        dma("sync", gtmp, gmix_d, [], [gtmpT])
        stt(s1b, s1b, 1.0, gtmp, ALU.add, ALU.mult, [modT[1], gtmpT], [modT[1]])
        dma("sync", gtmp, gffn_d, [], [gtmpT])
        stt(s2b, s2b, 1.0, gtmp, ALU.add, ALU.mult, [modT[4], gtmpT], [modT[4]])
        dma("sync", gt2_d, gt2b, [modT[5]], [])
        act(sinkexp, C(C_SINK, 8), AF.Exp, [cT], [sinkexpT])
        act(sp8, C(C_LAM, 4), AF.Exp, [cT], [sp8T], scale=-1.0)
        act(sp8, sp8, AF.Ln, [sp8T], [sp8T], bias=1.0)
        ts("vector", sp8, sp8, 8.0, None, ALU.mult, None, [sp8T], [sp8T])
        ts("vector", ls8, sp8, -1.0, None, ALU.mult, None, [sp8T], [ls8T])
        P.barrier()

        HT = PH
        hT = V3(HT, 8, S, F32R)
        hTT = [T() for _ in range(4)]
        XS0 = PH + 16384
        xst = [V(XS0 + k * 1024, D) for k in range(2)]; xstT = [T(), T()]
        h1t = [V(XS0 + 2048 + k * 1024, D) for k in range(2)]; h1T = [T(), T()]
        ss_, ssT = small(2)
        rs_, rsT = small(2)

        def rms_rstd(src, srcT, junk, junkT, ss, ssT_, rs, rsT_, n):
            act(junk, src, AF.Square, [srcT], [junkT, ssT_], accum_out=ss)
            act(rs, ss, AF.Sqrt, [ssT_], [rsT_], scale=1.0 / n, bias=EPS)
            P.op("vector", lambda e: e.reciprocal(out=rs, in_=rs), [rsT_], [rsT_])

        def transpose8(src, srcT, dstfn, dstT, k):
            for half in range(2):
                pb, pt = bank()
                for q in range(4):
                    kc = half * 4 + q
                    tr(pb[:, q * 128:(q + 1) * 128], src[:, kc * 128:(kc + 1) * 128], identF, [srcT, cT], [pt])
                dst = dstfn(half)
                cp("scalar" if (half + k) % 2 == 0 else "vector", dst, pb[:, :].rearrange("p (a b) -> p a b", a=4), [pt], [dstT])

        ssT1 = [T(), T()]; rsT1 = [T(), T()]

        def p1_s1(i):
            k = i % 2
            dma("sync", xst[k], x_d[i * 128:(i + 1) * 128, :], [], [xstT[k]])
            rms_rstd(xst[k], xstT[k], h1t[k], h1T[k], ss_[:, k:k + 1], ssT1[k], rs_[:, k:k + 1], rsT1[k], D)
            stt(h1t[k], xst[k], rs_[:, k:k + 1], s1b, ALU.mult, ALU.mult, [xstT[k], rsT1[k], modT[1]], [h1T[k]])
            tt("gpsimd", h1t[k], h1t[k], sh1b, ALU.add, [h1T[k], modT[0]], [h1T[k]])

        def p1_s2(i):
            k = i % 2
            transpose8(h1t[k], h1T[k], lambda half, i=i: hT[:, half * 4:(half + 1) * 4, i * 128:(i + 1) * 128], hTT[i // 4], k)

        for step in range(NT + 1):
            if step < NT:
                p1_s1(step)
            if step >= 1:
                p1_s2(step - 1)
        P.barrier()
        if stop_after == 1:
            return finish_debug(nc, P, dbg_d, hT.bitcast(F32).rearrange('p a b -> p (a b)'), out_d)

        WI0 = PH + 16384
        WI = [V3(WI0 + k * 1024, 8, 128, F32R) for k in range(2)]; WIT = [T(), T()]
        wi_state = [0]
        BD0 = WI0 + 2048
        bdA = V3(BD0, 4, 128, F32R); bdX = V3(BD0 + 512, 4, 128, F32R); bdT = T()
        dma("gpsimd", bdA, bda_d.rearrange("p (a b) -> p a b", a=4), [], [bdT])
        dma("gpsimd", bdX, bdx_d.rearrange("p (a b) -> p a b", a=4), [], [bdT])
        LB0 = BD0 + 1024
        LBR = [V(LB0 + k * S, S, F32R) if k == 1 else None for k in range(8)]
        LB = [LBR[k].bitcast(F32) if k == 1 else V(LB0 + k * S, S) for k in range(8)]
        LBT = [T() for _ in range(8)]
        LRU0 = LB0 + 8 * S
        assert LRU0 + 4 * S <= ARENA, LRU0 + 4 * S
        lruT = V3(LRU0, 4, S, F32R)
        lru0 = lruT.bitcast(F32)
        lruTT = [T() for _ in range(4)]
        win_v = win_d.rearrange("(kc p) n -> p kc n", p=128)
        evac_state = [0]

        def load_wi(col_specs):
            k = wi_state[0]
            wi_state[0] = 1 - k
            for (dst0, c0, n) in col_specs:
                dma("gpsimd", WI[k][:, :, dst0:dst0 + n], win_v[:, :, c0:c0 + n], [], [WIT[k]])
            return WI[k], WIT[k]

        def inproj_fm(w, wT_, dstfn, dstT):
            for tb in range(4):
                pb, pt = bank()
                for kc in range(8):
                    mm(pb[:, :], w[:, kc, :], hT[:, kc, tb * 512:(tb + 1) * 512], kc == 0, kc == 7, [wT_, hTT[tb]], [pt])
                evac_state[0] += 1
                cp("scalar" if evac_state[0] % 2 == 0 else "vector", dstfn(tb), pb[:, :], [pt], [dstT])

        cw = lambda c, k: C(C_CW + c * 4 + k, 1)
        colc = lambda base, c: C(base + c, 1)
        acc = LB[7]; accT = LBT[7]
        for c in range(4):
            w, wT_ = load_wi([(0, 768 + c * 128, 128)])
            inproj_fm(w, wT_, lambda tb: LB[0][:, tb * 512:(tb + 1) * 512], LBT[0])
            w, wT_ = load_wi([(0, 1280 + c * 128, 128)])
            inproj_fm(w, wT_, lambda tb: LB[2][:, tb * 512:(tb + 1) * 512], LBT[2])
            xr, xc, xg = LB[0], LB[1], LB[2]
            ts("vector", LBR[1], xr, cw(c, 3), colc(C_CB, c), ALU.mult, ALU.add, [LBT[0], cT], [LBT[1]])
            for sh in (1, 2, 3):
                stt(LBR[1][:, sh:], xr[:, :S - sh], cw(c, 3 - sh), xc[:, sh:], ALU.mult, ALU.add, [LBT[0], LBT[1], cT], [LBT[1]])
            for gi, (bd, bcol, dst) in enumerate(((bdA, C_BA, 3), (bdX, C_BX, 4))):
                for tb in range(4):
                    pb, pt = bank()
                    mm(pb[:, :], bd[:, c, :], LBR[1][:, tb * 512:(tb + 1) * 512], True, True, [bdT, LBT[1]], [pt])
                    act(LB[dst][:, tb * 512:(tb + 1) * 512], pb[:, :], AF.Sigmoid, [pt, cT], [LBT[dst]], bias=colc(bcol, c))
            r, ig = LB[3], LB[4]
            act(LB[6], r, AF.Tanh, [LBT[3], sp8T], [LBT[6]], scale=sp8[:, c:c + 1])
            act(LB[3], r, AF.Exp, [LBT[3], ls8T], [LBT[3]], scale=ls8[:, c:c + 1])
            a = LB[3]
            tt("gpsimd", LB[5], a, a, ALU.mult, [LBT[3]], [LBT[5]])
            stt(LB[5], LB[5], 1.0, LB[6], ALU.add, ALU.mult, [LBT[5], LBT[6]], [LBT[5]])
            act(LB[5], LB[5], AF.Sqrt, [LBT[5]], [LBT[5]])
            tt("gpsimd", LB[4], ig, xc, ALU.mult, [LBT[4], LBT[1]], [LBT[4]])
            tt("vector", LB[4], LB[4], LB[5], ALU.mult, [LBT[4], LBT[5]], [LBT[4]])
            P.op("vector", lambda e, a=a: e.tensor_tensor_scan(out=LB[0], data0=a, data1=LB[4], initial=0.0, op0=ALU.mult, op1=ALU.add),
                 [LBT[3], LBT[4], LBT[0]], [LBT[0]])
            act(LB[6], xg, AF.Square, [LBT[2]], [LBT[6]])
            ts("gpsimd", LB[6], LB[6], 0.044715, 1.0, ALU.mult, ALU.add, [LBT[6]], [LBT[6]])
            tt("gpsimd", LB[6], LB[6], xg, ALU.mult, [LBT[6], LBT[2]], [LBT[6]])
            act(LB[6], LB[6], AF.Sigmoid, [LBT[6]], [LBT[6]], scale=1.5957691216057308)
            tt("gpsimd", LB[6], LB[6], xg, ALU.mult, [LBT[6], LBT[2]], [LBT[6]])
            tt("vector", lruT[:, c, :], LB[0], LB[6], ALU.mult, [LBT[0], LBT[6]], [lruTT[c]])
            if c == 0:
                tt("gpsimd", acc, lru0[:, c, :], lru0[:, c, :], ALU.mult, [lruTT[c]], [accT])
            else:
                tt("gpsimd", LB[5], lru0[:, c, :], lru0[:, c, :], ALU.mult, [lruTT[c]], [LBT[5]])
                tt("gpsimd", acc, acc, LB[5], ALU.add, [accT, LBT[5]], [accT])
        for tb in range(4):
            pb, pt = bank()
            mm(pb[:, :], onesF, acc[:, tb * 512:(tb + 1) * 512], True, True, [cT, accT], [pt])
            act(LB[6][:, tb * 512:(tb + 1) * 512], pb[:, :], AF.Sqrt, [pt], [LBT[6]], scale=1.0 / 512, bias=EPS)
        P.op("vector", lambda e: e.reciprocal(out=LB[6], in_=LB[6]), [LBT[6]], [LBT[6]])
        for c in range(4):
            stt(lruT[:, c, :], lru0[:, c, :], colc(C_GL, c), LB[6], ALU.mult, ALU.mult, [lruTT[c], LBT[6], cT], [lruTT[c]])
        P.barrier()
        if stop_after == 2:
            return finish_debug(nc, P, dbg_d, lruT.bitcast(F32).rearrange('p a b -> p (a b)'), out_d)

        Q0 = LB0
        qT = V3(Q0, 4, S, F32R); qTT = [T() for _ in range(4)]
        kk = V3(Q0 + 4 * S, 2, S, F32R); kkT = [T(), T()]
        VA0 = Q0 + 6 * S
        vflat = V(VA0, NT * 132, F32R)
        vaug = vflat.rearrange("p (t g d) -> p t g d", t=NT, g=2)
        vT = T()
        assert VA0 + NT * 132 <= LRU0
        ones4 = onesF[:, 0:32].rearrange("p (t g d) -> p t g d", t=NT, g=2)
        cp("vector", vaug[:, :, :, 64:65], ones4, [cT], [vT])
        ts("vector", vaug[:, :, :, 65:66], ones4, 0.0, None, ALU.mult, None, [cT], [vT])
        for c in range(4):
            w, wT_ = load_wi([(0, c * 128, 128)])
            inproj_fm(w, wT_, lambda tb, c=c: qT[:, c, tb * 512:(tb + 1) * 512], qTT[c])
        for g in range(2):
            w, wT_ = load_wi([(0, 512 + g * 64, 64), (64, 512 + g * 64, 64)])
            inproj_fm(w, wT_, lambda tb, g=g: kk[:, g, tb * 512:(tb + 1) * 512], kkT[g])
        w, wT_ = load_wi([(0, 640, 128)])
        for i in range(NT):
            pb, pt = bank()
            for kc in range(8):
                mm(pb[:, 0:128], hT[:, kc, i * 128:(i + 1) * 128], w[:, kc, :], kc == 0, kc == 7, [wT_, hTT[i // 4]], [pt])
            cp("scalar" if i % 2 == 0 else "vector", vaug[:, i, :, 0:64], pb[:, 0:128].rearrange("p (g d) -> p g d", g=2), [pt], [vT])
        P.barrier()

        AT0 = PH
        attnT = V3(AT0, 4, S, F32R); attnTT = T()
        ET0 = PH + 4 * S
        eT = [[V(ET0 + (s * 2 + kbi) * 512, 512, F32R) for kbi in range(2)] for s in range(2)]
        eTT = [[T(), T()], [T(), T()]]
        AN0 = ET0 + 2048
        attn_t = [V(AN0 + k * 512, 512) for k in range(2)]; attn_tT = [T(), T()]
        junk512 = V(AN0 + 1024, 512); junk512T = T()
        den, denT = small(8)
        ssa, ssaT = small(2)
        rsa, rsaT = small(2)
        scale = 0.125
        for n in range(NT):
            k = n % 2
            for g in range(2):
                s = g
                kbs = ([n - 1] if n > 0 else []) + [n]
                for kbi, kb in enumerate(kbs):
                    diag = (kb == n)
                    for half in range(2):
                        pb, pt = bank()
                        mm(pb[:, 0:256], identR, (maskDR if diag else maskPR)[:, 0:256], True, False, [crT], [pt])
                        for u_ in range(2):
                            hh = u_ * 2 + half
                            c = 2 * g + hh // 2
                            h0 = half * 64
                            mm(pb[:, u_ * 128:(u_ + 1) * 128], kk[h0:h0 + 64, g, kb * 128:(kb + 1) * 128],
                               qT[h0:h0 + 64, c, n * 128:(n + 1) * 128], False, u_ == 1, [kkT[g], qTT[c]], [pt], mode=64)
                        act(eT[s][kbi][:, half * 256:(half + 1) * 256], pb[:, 0:256], AF.Exp, [pt], [eTT[s][kbi]], scale=scale)
                pb, pt = bank()
                for hh in range(4):
                    for kbi, kb in enumerate(kbs):
                        mm(pb[:, hh * 66:(hh + 1) * 66], eT[s][kbi][:, ((hh % 2) * 2 + hh // 2) * 128:((hh % 2) * 2 + hh // 2 + 1) * 128], vaug[:, kb, g, :],
                           kbi == 0, kbi == len(kbs) - 1, [eTT[s][kbi], vT], [pt])
                pv = pb[:, 0:264].rearrange("p (h d) -> p h d", h=4)
                tt("vector", den[:, g * 4:(g + 1) * 4], pv[:, :, 64], sinkexp[:, g * 4:(g + 1) * 4], ALU.add, [pt, sinkexpT], [denT])
                P.op("vector", lambda e, g=g: e.reciprocal(out=den[:, g * 4:(g + 1) * 4], in_=den[:, g * 4:(g + 1) * 4]), [denT], [denT])
                for hh in range(4):
                    hd = g * 4 + hh
                    if hh % 2 == 0:
                        ts("vector", attn_t[k][:, hd * 64:(hd + 1) * 64], pv[:, hh, 0:64], den[:, hd:hd + 1], None, ALU.mult, None, [pt, denT], [attn_tT[k]])
                    else:
                        act(attn_t[k][:, hd * 64:(hd + 1) * 64], pv[:, hh, 0:64], AF.Copy, [pt, denT], [attn_tT[k]], scale=den[:, hd:hd + 1])
            rms_rstd(attn_t[k], attn_tT[k], junk512, junk512T, ssa[:, k:k + 1], ssaT, rsa[:, k:k + 1], rsaT, 512)
            ts("vector", attn_t[k], attn_t[k], rsa[:, k:k + 1], None, ALU.mult, None, [attn_tT[k], rsaT], [attn_tT[k]])
            pb, pt = bank()
            for c in range(4):
                tr(pb[:, c * 128:(c + 1) * 128], attn_t[k][:, c * 128:(c + 1) * 128], identF, [attn_tT[k], cT], [pt])
            for c in range(4):
                if c % 2 == 0:
                    ts("vector", attnT[:, c, n * 128:(n + 1) * 128], pb[:, c * 128:(c + 1) * 128], colc(C_GA, c), None, ALU.mult, None, [pt, cT], [attnTT])
                else:
                    act(attnT[:, c, n * 128:(n + 1) * 128], pb[:, c * 128:(c + 1) * 128], AF.Copy, [pt, cT], [attnTT], scale=colc(C_GA, c))
        P.barrier()
        if stop_after == 3:
            return finish_debug(nc, P, dbg_d, attnT.bitcast(F32).rearrange('p a b -> p (a b)'), out_d)

        WO0 = PH + 4 * S
        wo = V3(WO0, 8, D, F32R); woT = T()
        wout_v = wout_d.rearrange("(kc p) n -> p kc n", p=128)
        for hf in range(2):
            dma("gpsimd", wo[:, :, hf * 512:(hf + 1) * 512], wout_v[:, :, hf * 512:(hf + 1) * 512], [], [woT])
        T0 = WO0 + 8 * D
        xt = [V(T0 + k * D, D) for k in range(2)]; xtT = [T(), T()]
        x1t = [V(T0 + 2 * D + k * D, D) for k in range(2)]; x1T = [T(), T()]
        h2t = [V(T0 + 4 * D + k * D, D) for k in range(2)]; h2T_ = [T(), T()]
        h2Tr = [V3(T0 + 6 * D + k * D, 8, 128) for k in range(2)]; h2TrT = [T(), T()]
        WR0 = T0 + 8 * D
        wr = V3(WR0, 8, E); wrT = T()
        dma("sync", wr, wr_d.rearrange("(kc p) n -> p kc n", p=128), [], [wrT])
        R0 = WR0 + 8 * E
        assert R0 + 1024 <= LB0 + 8 * S
        rsm = [R0]

        def rsmall(n):
            o = rsm[0]
            rsm[0] += (n + 7) // 8 * 8
            return V(o, n), T()

        lg, lgT = rsmall(32)
        top8, top8T = rsmall(8)
        idx8_o = rsm[0]; rsm[0] += 8
        idx8 = V(idx8_o, 8, U32); idx8T = T()
        idxf, idxfT = rsmall(4)
        negm, negmT = rsmall(1)
        e4, e4T = rsmall(4)
        ssum, ssumT = rsmall(1)
        mask, maskT = rsmall(32)
        rkp, rkpT = rsmall(32)
        ohj, ohjT = rsmall(32)
        ss2, _ = rsmall(2)
        rs2, _ = rsmall(2)
        xsT_d = T()
        x1dT = T()
        mergedT = lambda kc: (attnT[:, kc, :] if kc < 4 else lruT[:, kc - 4, :])
        mT = lambda kc: (attnTT if kc < 4 else lruTT[kc - 4])
        ss2T = [T(), T()]; rs2T = [T(), T()]
        gatesTi = [T() for _ in range(NT)]; destfTi = [T() for _ in range(NT)]; destiTi = [T() for _ in range(NT)]

        def p3_s1(i):
            k = i % 2
            dma("sync", xt[k], x_d[i * 128:(i + 1) * 128, :], [], [xtT[k]])
            for hf in range(2):
                pb, pt = bank()
                for kc in range(8):
                    mm(pb[:, :], mergedT(kc)[:, i * 128:(i + 1) * 128], wo[:, kc, hf * 512:(hf + 1) * 512], kc == 0, kc == 7, [mT(kc), woT], [pt])
                sl = slice(hf * 512, (hf + 1) * 512)
                tt("vector", x1t[k][:, sl], pb[:, :], gt1b[:, sl], ALU.mult, [pt, modT[2]], [x1T[k]])
                tt("gpsimd", x1t[k][:, sl], x1t[k][:, sl], xt[k][:, sl], ALU.add, [x1T[k], xtT[k]], [x1T[k]])
            dma("sync", x1_d[i * 128:(i + 1) * 128, :], x1t[k], [x1T[k]], [])
            rms_rstd(x1t[k], x1T[k], h2t[k], h2T_[k], ss2[:, k:k + 1], ss2T[k], rs2[:, k:k + 1], rs2T[k], D)
            stt(h2t[k], x1t[k], rs2[:, k:k + 1], s2b, ALU.mult, ALU.mult, [x1T[k], rs2T[k], modT[4]], [h2T_[k]])
            tt("gpsimd", h2t[k], h2t[k], sh2b, ALU.add, [h2T_[k], modT[3]], [h2T_[k]])

        def p3_s2(i):
            k = i % 2
            transpose8(h2t[k], h2T_[k], lambda half, k=k: h2Tr[k][:, half * 4:(half + 1) * 4, :], h2TrT[k], k)
            pb, pt = bank()
            for kc in range(8):
                mm(pb[:, 0:E], h2Tr[k][:, kc, :], wr[:, kc, :], kc == 0, kc == 7, [h2TrT[k], wrT], [pt])
            tt("vector", lg, pb[:, 0:E], C(C_BR, E), ALU.add, [pt, cT], [lgT])
            P.op("vector", lambda e: e.max(out=top8, in_=lg), [lgT], [top8T])
            P.op("vector", lambda e: e.max_index(out=idx8, in_max=top8, in_values=lg), [lgT, top8T], [idx8T])
            cp("vector", idxf, idx8[:, 0:4], [idx8T], [idxfT])
            ts("vector", negm, top8[:, 0:1], -1.0, None, ALU.mult, None, [top8T], [negmT])
            act(e4, top8[:, 0:4], AF.Exp, [top8T, negmT], [e4T, ssumT], bias=negm, accum_out=ssum)
            P.op("vector", lambda e: e.reciprocal(out=ssum, in_=ssum), [ssumT], [ssumT])
            ts("vector", gates_all[:, i * 4:(i + 1) * 4], e4, ssum, None, ALU.mult, None, [e4T, ssumT], [gatesTi[i]])
            ts("vector", mask, lg, top8[:, 3:4], None, ALU.is_ge, None, [lgT, top8T], [maskT])
            pb, pt = bank()
            if i > 0:
                mm(pb[:, 0:E], onesF, cum, True, False, [cT, cumT], [pt])
            mm(pb[:, 0:E], UF, mask, i == 0, True, [cT, maskT], [pt])
            tt("vector", rkp, pb[:, 0:E], C(C_RB, E), ALU.add, [pt, cT], [rkpT])
            if i == 0:
                cp("vector", cum, mask, [maskT], [cumT])
            else:
                tt("vector", cum, cum, mask, ALU.add, [cumT, maskT], [cumT])
            for j in range(4):
                stt(ohj, C(C_IOTA, E), idxf[:, j:j + 1], rkp, ALU.is_equal, ALU.mult, [cT, idxfT, rkpT], [ohjT, destfTi[i]],
                    accum_out=destf_all[:, i * 4 + j:i * 4 + j + 1])
            cp("vector", desti_all[:, i * 4:(i + 1) * 4], destf_all[:, i * 4:(i + 1) * 4], [destfTi[i]], [destiTi[i]])

        def p3_s3(i):
            k = i % 2
            for j in range(4):
                P.dma("gpsimd", lambda e, i=i, j=j, k=k: e.indirect_dma_start(
                    out=xs_d, out_offset=bass.IndirectOffsetOnAxis(ap=desti_all[:, i * 4 + j:i * 4 + j + 1], axis=0),
                    in_=h2t[k], in_offset=None), [h2T_[k], destiTi[i]], [])

        for step in range(NT + 1):
            if step < NT:
                p3_s1(step)
            if step >= 1:
                p3_s2(step - 1)
                p3_s3(step - 1)
        cntb, cntbT = rsmall(32)
        nblk, nblkT = rsmall(32)
        pend, pendT = rsmall(32)
        pst, pstT = rsmall(32)
        ej, ejT = rsmall(NB)
        pstj, pstjT = rsmall(NB)
        tmpj, tmpjT = rsmall(NB)
        sj, sjT = rsmall(NB)
        rowj, rowjT = rsmall(NB)
        valid, validT = rsmall(NB)
        skp, skpT = rsmall(NB)
        ebW, ebWT = rsmall(NB)
        ebD, ebDT = rsmall(NB)
        idxW = V(MOD, NB * 8, I32).rearrange("p (j k) -> p j k", k=8)
        idxD = V(MOD + NB * 8, NB * 4, I32).rearrange("p (j k) -> p j k", k=4)
        idxX = V(MOD + NB * 12, NB * 2, I32).rearrange("p (j k) -> p j k", k=2)
        idxB = V(MOD + NB * 14, NB, I32)
        idxBd = V(MOD + NB * 15, NB, I32)
        tblT = T()
        jrow = C(C_J, NB)
        pid = C(C_PID, 1)
        pb, pt = bank()
        mm(pb[:, 0:E], onesF, cum, True, True, [cT, cumT], [pt])
        cp("vector", cntb, pb[:, 0:E], [pt], [cntbT])
        ts("vector", nblk, cntb, 0.0, None, ALU.is_gt, None, [cntbT], [nblkT])
        for sx in range(1, EREG // BSZ):
            stt(nblk, cntb, float(BSZ * sx), nblk, ALU.is_gt, ALU.add, [cntbT, nblkT], [nblkT])
        P.op("vector", lambda e: e.tensor_tensor_scan(out=pend, data0=onesF[:, 0:E], data1=nblk, initial=0.0, op0=ALU.mult, op1=ALU.add),
             [cT, nblkT], [pendT])
        tt("vector", pst, pend, nblk, ALU.subtract, [pendT, nblkT], [pstT])
        ts("vector", ej, jrow, pend[:, 0:1], None, ALU.is_ge, None, [cT, pendT], [ejT])
        for e_ in range(1, E):
            stt(ej, jrow, pend[:, e_:e_ + 1], ej, ALU.is_ge, ALU.add, [cT, pendT, ejT], [ejT])
        ts("vector", valid, ej, E - 0.5, None, ALU.is_lt, None, [ejT], [validT])
        ts("vector", ej, ej, float(E - 1), None, ALU.min, None, [ejT], [ejT])
        for e_ in range(E):
            ts("vector", tmpj, ej, float(e_), None, ALU.is_equal, None, [ejT], [tmpjT])
            if e_ == 0:
                ts("vector", pstj, tmpj, pst[:, 0:1], None, ALU.mult, None, [tmpjT, pstT], [pstjT])
            else:
                stt(pstj, tmpj, pst[:, e_:e_ + 1], pstj, ALU.mult, ALU.add, [tmpjT, pstT, pstjT], [pstjT])
        tt("vector", sj, jrow, pstj, ALU.subtract, [cT, pstjT], [sjT])
        ts("vector", skp, sj, 0.0, None, ALU.is_equal, None, [sjT], [skpT])
        ts("vector", skp, skp, -BIG, BIG, ALU.mult, ALU.add, [skpT], [skpT])
        ts("vector", tmpj, sj, float(BSZ), None, ALU.mult, None, [sjT], [tmpjT])
        stt(rowj, ej, float(EREG), tmpj, ALU.mult, ALU.add, [ejT, tmpjT], [rowjT])
        ts("vector", rowj, rowj, -BIG, None, ALU.add, None, [rowjT], [rowjT])
        tt("vector", rowj, rowj, valid, ALU.mult, [rowjT, validT], [rowjT])
        ts("vector", rowj, rowj, BIG, pid, ALU.add, ALU.add, [rowjT, cT], [rowjT])
        stt(ebW, ej, 1024.0, skp, ALU.mult, ALU.add, [ejT, skpT], [ebWT])
        ts("vector", ebW, ebW, pid, None, ALU.add, None, [ebWT, cT], [ebWT])
        stt(ebD, ej, 512.0, skp, ALU.mult, ALU.add, [ejT, skpT], [ebDT])
        ts("vector", ebD, ebD, pid, None, ALU.add, None, [ebDT, cT], [ebDT])
        for k_ in range(8):
            ts("vector", idxW[:, :, k_], ebW, float(k_ * 128), None, ALU.add, None, [ebWT], [tblT])
        for k_ in range(4):
            ts("vector", idxD[:, :, k_], ebD, float(k_ * 128), None, ALU.add, None, [ebDT], [tblT])
        for k_ in range(2):
            ts("vector", idxX[:, :, k_], rowj, float(k_ * 128), None, ALU.add, None, [rowjT], [tblT])
        ts("vector", idxB, ej, 128.0, pid, ALU.mult, ALU.add, [ejT, cT], [tblT])
        cp("vector", idxBd, ej, [ejT], [tblT])
        P.barrier()
        if stop_after == 4:
            return finish_debug(nc, P, dbg_d, None, out_d)

        WSg = [V3(PH + k * 2048, 4, 512, F32R) for k in range(8)]; WSgT = [T() for _ in range(8)]
        WSd = [V3(PH + (8 + k) * 2048, 4, 512, F32R) for k in range(4)]; WSdT = [T() for _ in range(4)]
        X0 = PH + 12 * 2048
        xsb = [V3(X0 + k * NJ * D, NJ, D) for k in range(2)]; xsbT = [T(), T()]
        XT0 = X0 + 2 * NJ * D
        xsT = [V3(XT0 + k * 8 * BSZ, 8, BSZ, F32R) for k in range(2)]; xsTT = [T(), T()]
        A0 = XT0 + 2 * 8 * BSZ
        actT = V3(A0, 8, BSZ, F32R); actTT = T()
        Y0 = A0 + 8 * BSZ
        yb = [V3(Y0 + k * NJ * D, NJ, D) for k in range(2)]; ybT = [T(), T()]
        G0 = Y0 + 2 * NJ * D
        gsb = [V(G0 + k * BSZ, BSZ) for k in range(6)]; gsbT = [T() for _ in range(6)]
        BG0 = G0 + 6 * BSZ
        bgub = [V(BG0 + k * 16, 16) for k in range(2)]; bgubT = [T(), T()]
        BD0_ = BG0 + 32
        bdnb = [V(BD0_ + k * D, D) for k in range(2)]; bdnbT = [T(), T()]
        assert BD0_ + 2 * D <= ARENA, BD0_ + 2 * D

        bregs = {}

        def breg(e, bound):
            if bound not in bregs:
                bregs[bound] = e.to_reg(bound)
            return bregs[bound]

        def igather(out, src, idx_ap, bound, reads, writes):
            P.dma("gpsimd", lambda e: e.indirect_dma_start(out=out, out_offset=None, in_=src,
                  in_offset=bass.IndirectOffsetOnAxis(ap=idx_ap, axis=0), bounds_check=breg(e, bound), oob_is_err=False), reads, writes)

        def load_block_small(j):
            k = j % 2
            for jj in range(NJ):
                igather(xsb[k][:, jj, :], xs_d, idxX[:, j, jj:jj + 1], NSLOT - 1, [tblT], [xsbT[k]])
            igather(bgub[k], bgur_d, idxB[:, j:j + 1], E * 128 - 1, [tblT], [bgubT[k]])
            igather(bdnb[k], bdn_d, idxBd[:, j:j + 1], E - 1, [tblT], [bdnbT[k]])

        def load_wg(j, k_):
            igather(WSg[k_].rearrange("p a b -> p (a b)"), wgur_d, idxW[:, j, k_:k_ + 1], E * 1024 - 1, [tblT], [WSgT[k_]])

        def load_wd(j, k_):
            igather(WSd[k_].rearrange("p a b -> p (a b)"), wdnr_d, idxD[:, j, k_:k_ + 1], E * 512 - 1, [tblT], [WSdT[k_]])

        load_block_small(0)
        for k_ in range(8):
            load_wg(0, k_)
        for k_ in range(4):
            load_wd(0, k_)
        for j in range(NB):
            k = j % 2
            for kc in range(8):
                pb, pt = bank()
                for jj in range(NJ):
                    tr(pb[:, jj * 128:(jj + 1) * 128], xsb[k][:, jj, kc * 128:(kc + 1) * 128], identF, [xsbT[k], cT], [pt])
                cp("scalar" if kc % 2 == 0 else "vector", xsT[k][:, kc, :], pb[:, 0:BSZ], [pt], [xsTT[k]])
            if j + 1 < NB:
                load_block_small(j + 1)
            for fc in range(8):
                qg = (fc // 4) * 2
                lc = (fc % 4) * 128
                pg, pgt = bank()
                for kc in range(8):
                    wi_ = qg * 2 + kc // 4
                    mm(pg[:, 0:BSZ], WSg[wi_][:, kc % 4, lc:lc + 128], xsT[k][:, kc, :], kc == 0, kc == 7, [WSgT[wi_], xsTT[k]], [pgt])
                pu, put = bank()
                for kc in range(8):
                    wi_ = (qg + 1) * 2 + kc // 4
                    mm(pu[:, 0:BSZ], WSg[wi_][:, kc % 4, lc:lc + 128], xsT[k][:, kc, :], kc == 0, kc == 7, [WSgT[wi_], xsTT[k]], [put])
                if fc % 4 == 3 and j + 1 < NB:
                    for k_ in range(qg * 2, qg * 2 + 4):
                        load_wg(j + 1, k_)
                g3 = (fc % 2) * 3
                gb, gbT = gsb[g3], gsbT[g3]
                ub, ubT = gsb[g3 + 1], gsbT[g3 + 1]
                sl_, slT = gsb[g3 + 2], gsbT[g3 + 2]
                ts("vector", gb, pg[:, 0:BSZ], bgub[k][:, fc:fc + 1], 7.0, ALU.add, ALU.min, [pgt, bgubT[k]], [gbT])
                ts("vector", ub, pu[:, 0:BSZ], bgub[k][:, 8 + fc:8 + fc + 1], 7.0, ALU.add, ALU.min, [put, bgubT[k]], [ubT])
                ts("vector", ub, ub, -7.0, 1.0, ALU.max, ALU.add, [ubT], [ubT])
                act(sl_, gb, AF.Silu, [gbT], [slT], scale=1.702)
                stt(actT[:, fc, :], sl_, 1.0 / 1.702, ub, ALU.mult, ALU.mult, [slT, ubT], [actTT])
            for hf in range(2):
                for jj in range(NJ):
                    pb, pt = bank()
                    for fc in range(8):
                        wi_ = hf * 2 + fc // 4
                        mm(pb[:, :], actT[:, fc, jj * 128:(jj + 1) * 128], WSd[wi_][:, fc % 4, :], fc == 0, fc == 7, [actTT, WSdT[wi_]], [pt])
                    sl = slice(hf * 512, (hf + 1) * 512)
                    tt("vector", yb[k][:, jj, sl], pb[:, :], bdnb[k][:, sl], ALU.add, [pt, bdnbT[k]], [ybT[k]])
                if j + 1 < NB:
                    load_wd(j + 1, hf * 2)
                    load_wd(j + 1, hf * 2 + 1)
            for jj in range(NJ):
                P.dma("gpsimd", lambda e, j=j, jj=jj, k=k: e.indirect_dma_start(
                    out=ys_d, out_offset=bass.IndirectOffsetOnAxis(ap=idxX[:, j, jj:jj + 1], axis=0),
                    in_=yb[k][:, jj, :], in_offset=None, bounds_check=breg(e, NSLOT - 1), oob_is_err=False), [ybT[k], tblT], [])
        P.barrier()
        if stop_after == 5:
            return finish_debug(nc, P, dbg_d, None, out_d)

        gt2s = V(PH, D); gt2sT = T()
        gfin = V(PH + D, D); gfinT = T()
        dma("sync", gt2s, gt2_d, [], [gt2sT])
        dma("sync", gfin, gfin_d, [], [gfinT])
        F0 = PH + 2 * D
        x1r = [V(F0 + k * D, D) for k in range(2)]; x1rT = [T(), T()]
        yg = [[V(F0 + 2 * D + (k * 4 + j) * D, D) for j in range(4)] for k in range(2)]
        ygT = [[T() for _ in range(4)] for _ in range(2)]
        ob = [V(F0 + 10 * D + k * D, D) for k in range(2)]; obT = [T(), T()]
        ss3, _ = small(2)
        rs3, _ = small(2)
        ss3T = [T(), T()]; rs3T = [T(), T()]

        def p5_s1(i):
            k = i % 2
            dma("sync", x1r[k], x1_d[i * 128:(i + 1) * 128, :], [], [x1rT[k]])
            for j in range(4):
                P.dma("gpsimd", lambda e, i=i, j=j, k=k: e.indirect_dma_start(
                    out=yg[k][j], out_offset=None, in_=ys_d,
                    in_offset=bass.IndirectOffsetOnAxis(ap=desti_all[:, i * 4 + j:i * 4 + j + 1], axis=0)), [destiTi[i]], [ygT[k][j]])

        def p5_s2(i):
            k = i % 2
            ts("vector", ob[k], yg[k][0], gates_all[:, i * 4:i * 4 + 1], None, ALU.mult, None, [ygT[k][0], gatesTi[i]], [obT[k]])
            for j in range(1, 4):
                stt(ob[k], yg[k][j], gates_all[:, i * 4 + j:i * 4 + j + 1], ob[k], ALU.mult, ALU.add, [ygT[k][j], gatesTi[i], obT[k]], [obT[k]])
            tt("gpsimd", ob[k], ob[k], gt2s, ALU.mult, [obT[k], gt2sT], [obT[k]])
            tt("vector", x1r[k], x1r[k], ob[k], ALU.add, [x1rT[k], obT[k]], [x1rT[k]])

        def p5_s3(i):
            k = i % 2
            rms_rstd(x1r[k], x1rT[k], ob[k], obT[k], ss3[:, k:k + 1], ss3T[k], rs3[:, k:k + 1], rs3T[k], D)
            stt(ob[k], x1r[k], rs3[:, k:k + 1], gfin, ALU.mult, ALU.mult, [x1rT[k], rs3T[k], gfinT], [obT[k]])
            dma("sync", out_d[i * 128:(i + 1) * 128, :], ob[k], [obT[k]], [])

        p5_s1(0)
        for step in range(NT + 1):
            if step >= 1:
                p5_s3(step - 1)
            if step + 1 < NT:
                p5_s1(step + 1)
            if step < NT:
                p5_s2(step)
        P.barrier()
        P.emit()
    return nc


def finish_debug(nc, P, dbg_d, src, out_d):
    if src is not None:
        n = src.shape[1]
        P.dma("sync", lambda e: e.dma_start(out=dbg_d[:, 0:n], in_=src), [], [])
    P.barrier()
    P.emit()
    return nc


def host_consts(inp):
    c = np.zeros((128, NCONST), np.float32)
    c[:, C_ID:C_ID + 128] = np.eye(128, dtype=np.float32)
    c[:, C_ONE:C_ONE + 128] = 1.0
    kk_, qq = np.meshgrid(np.arange(128), np.arange(128), indexing="ij")
    c[:, C_U:C_U + 128] = (kk_ < qq).astype(np.float32)
    md = np.where(kk_ <= qq, 0.0, NEG).astype(np.float32)
    mp = np.where(kk_ > qq, 0.0, NEG).astype(np.float32)
    c[:, C_MD:C_MD + 512] = np.tile(md, (1, 4))
    c[:, C_MP:C_MP + 512] = np.tile(mp, (1, 4))
    c[:, C_IOTA:C_IOTA + 32] = np.arange(32, dtype=np.float32)[None, :]
    c[:, C_RB:C_RB + 32] = (np.arange(32, dtype=np.float32) * EREG)[None, :]
    c[:, C_PID] = np.arange(128, dtype=np.float32)
    c[:, C_J:C_J + NB] = np.arange(NB, dtype=np.float32)[None, :]
    c[:, C_BR:C_BR + 32] = inp["b_router"][0][None, :]
    c[:, C_SINK:C_SINK + 8] = inp["sinks"][0][None, :]
    cwt = inp["conv_w"][0]
    c[:, C_CW:C_CW + 16] = cwt.T.reshape(4, 128, 4).transpose(1, 0, 2).reshape(128, 16)
    col = lambda v: np.asarray(v, np.float32).reshape(4, 128).T
    c[:, C_CB:C_CB + 4] = col(inp["conv_b"][0])
    c[:, C_BA:C_BA + 4] = col(inp["b_rg_a"][0].reshape(512))
    c[:, C_BX:C_BX + 4] = col(inp["b_rg_x"][0].reshape(512))
    c[:, C_LAM:C_LAM + 4] = col(inp["lru_lambda"][0])
    c[:, C_GA:C_GA + 4] = col(inp["g_attn_out"][0])
    c[:, C_GL:C_GL + 4] = col(inp["g_lru_out"][0])
    return c


def host_blockdiag(w):
    bd = np.zeros((128, 4, 128), np.float32)
    for c in range(4):
        bd[0:64, c, 0:64] = w[2 * c]
        bd[64:128, c, 64:128] = w[2 * c + 1]
    return bd.reshape(128, 512)


def host_wgu(w):
    w6 = w.reshape(E, 2, 4, 128, 4, 512)
    cg = [0, 2, 1, 3]
    out = np.empty((E, 4, 2, 128, 4, 512), np.float32)
    for q in range(4):
        out[:, q] = w6[:, :, :, :, cg[q], :].transpose(0, 1, 3, 2, 4)
    return out.reshape(E * 1024, 2048)


def host_wdn(w):
    w6 = w.reshape(E, 2, 4, 128, 2, 512)
    out = np.ascontiguousarray(w6.transpose(0, 4, 1, 3, 2, 5))
    return out.reshape(E * 512, 2048)


def make_in_maps(inp):
    inp = {k: np.asarray(v) for k, v in inp.items()}
    bc = lambda v: np.ascontiguousarray(np.broadcast_to(np.asarray(v, np.float32)[None, :], (128, v.shape[0])))
    shared = {
        "w_ada": np.ascontiguousarray(inp["w_ada"][0]),
        "b_ada_b": bc(inp["b_ada"][0]),
        "g_mix_b": bc(inp["g_mix"][0]),
        "g_ffn_b": bc(inp["g_ffn"][0]),
        "g_final_b": bc(inp["g_final"]),
        "w_in": np.ascontiguousarray(inp["w_in"][0]),
        "bd_a": host_blockdiag(inp["w_rg_a"][0]),
        "bd_x": host_blockdiag(inp["w_rg_x"][0]),
        "w_out": np.ascontiguousarray(inp["w_out"][0]),
        "w_router": np.ascontiguousarray(inp["w_router"][0]),
        "w_gu_r": host_wgu(inp["w_gu"][0]),
        "w_dn_r": host_wdn(inp["w_down"][0]),
        "b_gu_r": np.ascontiguousarray(inp["b_gu"][0].reshape(E, 16, 128).transpose(0, 2, 1).reshape(E * 128, 16)),
        "b_down": np.ascontiguousarray(inp["b_down"][0]),
        "consts": host_consts(inp),
    }
    maps = []
    for b in range(8):
        m = dict(shared)
        m["x"] = np.ascontiguousarray(inp["x"][b])
        cb = inp["c"][b].reshape(8, 128).T
        m["cB"] = np.ascontiguousarray(np.broadcast_to(cb[:, :, None], (128, 8, 128)).reshape(128, 1024)).astype(np.float32)
        maps.append(m)
    return maps


def kernel(**inputs):
    nc = build()
    maps = make_in_maps(inputs)
    res = run_bass_kernel_spmd(nc, maps, core_ids=list(range(8)))
    return np.stack([np.asarray(r["out"]) for r in res.results], axis=0).astype(np.float32)
```

```python
import numpy as np
import concourse.bass as bass
import concourse.mybir as mybir
from concourse.bass_utils import run_bass_kernel_spmd
from contextlib import ExitStack

F32 = mybir.dt.float32
F32R = mybir.dt.float32r
U32 = mybir.dt.uint32
I32 = mybir.dt.int32
ALU = mybir.AluOpType
AF = mybir.ActivationFunctionType

S = 2048
D = 1024
NT = 16
E = 32
EREG = 2048
BSZ = 256
NJ = BSZ // 128
NB = S * 4 // BSZ + E
NSLOT = E * EREG
BIG = 1000000.0
EPS = 1e-6
NEG = -30000.0

ENGS = ("tensor", "vector", "scalar", "gpsimd", "sync")
NDS = 40
NHW = 12
SAME_ENG_SYNC = True

C_ID, C_ONE, C_U, C_MD, C_MP = 0, 128, 256, 384, 896
C_IOTA, C_RB, C_BR, C_SINK = 1408, 1440, 1472, 1504
C_CW, C_CB, C_BA, C_BX, C_LAM, C_GA, C_GL = 1512, 1528, 1532, 1536, 1540, 1544, 1548
C_PID, C_J = 1552, 1560
NCONST = 1624
ARENA = 52352


class T:
    __slots__ = ("w", "r")

    def __init__(self):
        self.w = None
        self.r = {}


class Prog:
    def __init__(self, nc, es):
        self.nc = nc
        self.q = {e: [] for e in ENGS}
        self.sem = {e: es.enter_context(nc.semaphore("sem_" + e)) for e in ENGS[:4]}
        self.cnt = {e: 0 for e in ENGS}
        self.dsem = [es.enter_context(nc.semaphore(f"dsem{i}")) for i in range(NDS)]
        self.dcnt = [0] * NDS
        self.dnext = 0
        self.dnext_sw = 0
        self.seen = {}

    def _need(self, eng, key, val):
        if key == ("e", eng) and (eng == "tensor" or not SAME_ENG_SYNC):
            return
        if self.seen.get((eng, key), 0) >= val:
            return
        self.seen[(eng, key)] = val
        self.q[eng].append(("wait", key, val))

    def _deps(self, eng, reads, writes):
        for t in reads:
            if t.w:
                self._need(eng, *t.w)
        for t in writes:
            if t.w:
                self._need(eng, *t.w)
            for k, v in t.r.items():
                self._need(eng, k, v)

    def _mark(self, ev, reads, writes):
        for t in reads:
            if t.r.get(ev[0], 0) < ev[1]:
                t.r[ev[0]] = ev[1]
        for t in writes:
            t.w = ev
            t.r = {}

    def op(self, eng, fn, reads=(), writes=()):
        self._deps(eng, reads, writes)
        self.cnt[eng] += 1
        ev = (("e", eng), self.cnt[eng])
        self.q[eng].append(("op", fn))
        self._mark(ev, reads, writes)

    def dma(self, eng, fn, reads=(), writes=()):
        if eng == "gpsimd":
            i = NHW + self.dnext_sw
            self.dnext_sw = (self.dnext_sw + 1) % (NDS - NHW)
        else:
            i = self.dnext
            self.dnext = (i + 1) % NHW
        key = ("d", i)
        if self.dcnt[i] > 0:
            self._need(eng, key, self.dcnt[i])
        self._deps(eng, reads, writes)
        self.dcnt[i] += 16
        ev = (key, self.dcnt[i])
        self.q[eng].append(("dma", fn, i))
        self._mark(ev, reads, writes)

    def barrier(self):
        for eng in ENGS:
            for i in range(NDS):
                if self.dcnt[i] > 0:
                    self._need(eng, ("d", i), self.dcnt[i])
            for e in ENGS[:4]:
                if self.cnt[e] > 0 and e != eng:
                    self._need(eng, ("e", e), self.cnt[e])

    def _semobj(self, key):
        return self.sem[key[1]] if key[0] == "e" else self.dsem[key[1]]

    def emit(self):
        with self.nc.Block() as block:
            for e in ENGS:
                if not self.q[e]:
                    continue

                def body(engh, e=e):
                    for item in self.q[e]:
                        if item[0] == "wait":
                            engh.wait_ge(self._semobj(item[1]), item[2])
                        elif item[0] == "op":
                            item[1](engh).then_inc(self.sem[e], 1)
                        else:
                            item[1](engh).then_inc(self.dsem[item[2]], 16)

                getattr(block, e)(body)


def build(debug=False, stop_after=99):
    nc = bass.Bass("TRN2", target_bir_lowering=False)

    def din(name, shape, dtype=F32):
        return nc.dram_tensor(name, shape, dtype, kind="ExternalInput").ap()

    skind = "ExternalOutput" if debug else "Internal"
    x_d = din("x", [S, D])
    cB_d = din("cB", [128, 8 * 128])
    wada_d = din("w_ada", [D, 6 * D])
    bada_d = din("b_ada_b", [128, 6 * D])
    gmix_d = din("g_mix_b", [128, D])
    gffn_d = din("g_ffn_b", [128, D])
    gfin_d = din("g_final_b", [128, D])
    win_d = din("w_in", [D, 1792])
    bda_d = din("bd_a", [128, 4 * 128])
    bdx_d = din("bd_x", [128, 4 * 128])
    wout_d = din("w_out", [D, D])
    wr_d = din("w_router", [D, E])
    wgur_d = din("w_gu_r", [E * 1024, 2048])
    wdnr_d = din("w_dn_r", [E * 512, 2048])
    bgur_d = din("b_gu_r", [E * 128, 16])
    bdn_d = din("b_down", [E, D])
    const_d = din("consts", [128, NCONST])
    out_d = nc.dram_tensor("out", [S, D], F32, kind="ExternalOutput").ap()
    x1_d = nc.dram_tensor("x1_s", [S, D], F32, kind=skind).ap()
    xs_d = nc.dram_tensor("xs_s", [NSLOT, D], F32, kind=skind).ap()
    ys_d = nc.dram_tensor("ys_s", [NSLOT, D], F32, kind=skind).ap()
    gt2_d = nc.dram_tensor("gt2_s", [128, D], F32, kind=skind).ap()
    dbg_d = nc.dram_tensor("dbg", [128, 8 * S], F32, kind="ExternalOutput").ap() if debug else None

    es = ExitStack()
    with es:
        P = Prog(nc, es)
        AR = es.enter_context(nc.sbuf_tensor("arena", [128, ARENA], F32))
        pbank = [es.enter_context(nc.psum_tensor(f"pb{i}", [128, 512], F32)) for i in range(8)]
        pT = [T() for _ in range(8)]
        pstate = [0]

        def bank():
            i = pstate[0]
            pstate[0] = (i + 1) % 8
            return pbank[i], pT[i]

        ar_addr = [m.memorylocations[0].addr for m in nc.allocations if m.name == "arena_set"][0]
        vcount = [0]

        def V(off, n, dt=F32):
            assert off + n <= ARENA
            vcount[0] += 1
            t = nc.alloc_sbuf_tensor_at(f"v{vcount[0]}", [128, n], dt, offset=ar_addr + off * 4)
            return t[:, :]

        def V3(off, a, b, dt=F32):
            return V(off, a * b, dt).rearrange("p (a b) -> p a b", a=a)

        pe_mode = [128]

        def mm(out, lhsT, rhs, start, stop, reads, writes, mode=128):
            if mode != pe_mode[0]:
                pe_mode[0] = mode
                if P.cnt["tensor"] > 0:
                    P.q["tensor"].append(("wait", ("e", "tensor"), P.cnt["tensor"]))
            P.op("tensor", lambda e: e.matmul(out, lhsT=lhsT, rhs=rhs, start=start, stop=stop), reads, writes)

        def tr(out, in_, ident, reads, writes):
            if pe_mode[0] != 128:
                pe_mode[0] = 128
                P.q["tensor"].append(("wait", ("e", "tensor"), P.cnt["tensor"]))
            P.op("tensor", lambda e: e.transpose(out=out, in_=in_, identity=ident), reads, writes)

        def act(out, in_, func, reads, writes, **kw):
            P.op("scalar", lambda e: e.activation(out=out, in_=in_, func=func, **kw), reads, writes)

        def tt(eng, out, in0, in1, op, reads, writes):
            P.op(eng, lambda e: e.tensor_tensor(out=out, in0=in0, in1=in1, op=op), reads, writes)

        def ts(eng, out, in0, s1, s2, op0, op1, reads, writes, **kw):
            if op1 is None:
                P.op(eng, lambda e: e.tensor_scalar(out=out, in0=in0, scalar1=s1, scalar2=None, op0=op0, **kw), reads, writes)
            else:
                P.op(eng, lambda e: e.tensor_scalar(out=out, in0=in0, scalar1=s1, scalar2=s2, op0=op0, op1=op1, **kw), reads, writes)

        def stt(out, in0, scalar, in1, op0, op1, reads, writes, **kw):
            P.op("vector", lambda e: e.scalar_tensor_tensor(out=out, in0=in0, scalar=scalar, in1=in1, op0=op0, op1=op1, **kw), reads, writes)

        def cp(eng, out, in_, reads, writes):
            if eng == "scalar":
                P.op("scalar", lambda e: e.copy(out=out, in_=in_), reads, writes)
            else:
                P.op(eng, lambda e: e.tensor_copy(out=out, in_=in_), reads, writes)

        def dma(eng, out, in_, reads, writes):
            P.dma(eng, lambda e: e.dma_start(out=out, in_=in_), reads, writes)

        CO = 0
        CR = 600
        SM = 1752
        MOD = 2112
        PH = 8256
        cT = T()
        crT = T()
        cot = V(CO, 600)
        crt = V(CR, 1152, F32R)

        def C(off, n):
            o = off if off < 384 else 384 + off - C_IOTA
            return cot[:, o:o + n]

        dma("sync", cot[:, 0:384], const_d[:, 0:384], [], [cT])
        dma("sync", cot[:, 384:600], const_d[:, C_IOTA:NCONST], [], [cT])
        dma("gpsimd", crt[:, 0:128], const_d[:, C_ID:C_ID + 128], [], [crT])
        dma("gpsimd", crt[:, 128:1152], const_d[:, C_MD:C_MD + 1024], [], [crT])
        identF = C(C_ID, 128)
        onesF = C(C_ONE, 128)
        UF = C(C_U, 128)
        identR = crt[:, 0:128]
        maskDR = crt[:, 128:640]
        maskPR = crt[:, 640:1152]
        sm = [SM]

        def small(n):
            o = sm[0]
            sm[0] += (n + 7) // 8 * 8
            assert sm[0] <= MOD
            return V(o, n), T()

        sinkexp, sinkexpT = small(8)
        sp8, sp8T = small(4)
        ls8, ls8T = small(4)
        gates_all, gatesT = small(64)
        destf_all, destfT = small(64)
        desti_o = sm[0]; sm[0] += 64
        desti_all = V(desti_o, 64, I32); destiT = T()
        cum, cumT = small(32)
        epsc, epscT = small(1)
        P.op("vector", lambda e: e.memset(epsc, EPS), [], [epscT])

        modrow = V(MOD, 6 * D)
        modT = [T() for _ in range(6)]
        msl = lambda j: modrow[:, j * D:(j + 1) * D]
        sh1b, s1b, gt1b, sh2b, s2b, gt2b = [msl(j) for j in range(6)]

        cBs = V3(PH, 8, 128, F32R); cBT = T()
        dma("gpsimd", cBs, cB_d.rearrange("p (a b) -> p a b", a=8), [], [cBT])
        for j in range(6):
            dma("sync", msl(j), bada_d[:, j * D:(j + 1) * D], [], [modT[j]])
        WA = [V3(PH + 1024 + k * 4096, 8, 512, F32R) for k in range(2)]
        WAT = [T(), T()]
        wada_v = wada_d.rearrange("(kc p) n -> p kc n", p=128)
        for g in range(12):
            k = g % 2
            dma("gpsimd", WA[k], wada_v[:, :, g * 512:(g + 1) * 512], [], [WAT[k]])
            pb, pt = bank()
            for kc in range(8):
                mm(pb[:, :], cBs[:, kc, :], WA[k][:, kc, :], kc == 0, kc == 7, [cBT, WAT[k]], [pt])
            mt = modT[g // 2]
            sl = modrow[:, g * 512:(g + 1) * 512]
            tt("vector", sl, pb[:, :], sl, ALU.add, [pt, mt], [mt])
        gtmp = V(PH + 1024 + 8192, D); gtmpT = T()
        dma("sync", gtmp, gmix_d, [], [gtmpT])
        stt(s1b, s1b, 1.0, gtmp, ALU.add, ALU.mult, [modT[1], gtmpT], [modT[1]])
        dma("sync", gtmp, gffn_d, [], [gtmpT])
        stt(s2b, s2b, 1.0, gtmp, ALU.add, ALU.mult, [modT[4], gtmpT], [modT[4]])
        dma("sync", gt2_d, gt2b, [modT[5]], [])
        act(sinkexp, C(C_SINK, 8), AF.Exp, [cT], [sinkexpT])
        act(sp8, C(C_LAM, 4), AF.Exp, [cT], [sp8T], scale=-1.0)
        act(sp8, sp8, AF.Ln, [sp8T], [sp8T], bias=1.0)
        ts("vector", sp8, sp8, 8.0, None, ALU.mult, None, [sp8T], [sp8T])
        ts("vector", ls8, sp8, -1.0, None, ALU.mult, None, [sp8T], [ls8T])
        P.barrier()

        HT = PH
        hT = V3(HT, 8, S, F32R)
        hTT = [T() for _ in range(4)]
        XS0 = PH + 16384
        xst = [V(XS0 + k * 1024, D) for k in range(2)]; xstT = [T(), T()]
        h1t = [V(XS0 + 2048 + k * 1024, D) for k in range(2)]; h1T = [T(), T()]
        ss_, ssT = small(2)
        rs_, rsT = small(2)

        def rms_rstd(src, srcT, junk, junkT, ss, ssT_, rs, rsT_, n):
            stt(junk, src, 1.0, src, ALU.mult, ALU.mult, [srcT], [junkT, ssT_], accum_out=ss)
            act(rs, ss, AF.Ln, [ssT_], [rsT_], scale=1.0 / n, bias=epsc)
            act(rs, rs, AF.Exp, [rsT_], [rsT_], scale=-0.5)

        def transpose8(src, srcT, dstfn, dstT, k):
            for half in range(2):
                pb, pt = bank()
                for q in range(4):
                    kc = half * 4 + q
                    tr(pb[:, q * 128:(q + 1) * 128], src[:, kc * 128:(kc + 1) * 128], identF, [srcT, cT], [pt])
                dst = dstfn(half)
                cp("scalar" if (half + k) % 2 == 0 else "vector", dst, pb[:, :].rearrange("p (a b) -> p a b", a=4), [pt], [dstT])

        ssT1 = [T(), T()]; rsT1 = [T(), T()]

        def p1_s1(i):
            k = i % 2
            dma("sync", xst[k], x_d[i * 128:(i + 1) * 128, :], [], [xstT[k]])
            rms_rstd(xst[k], xstT[k], h1t[k], h1T[k], ss_[:, k:k + 1], ssT1[k], rs_[:, k:k + 1], rsT1[k], D)
            stt(h1t[k], xst[k], rs_[:, k:k + 1], s1b, ALU.mult, ALU.mult, [xstT[k], rsT1[k], modT[1]], [h1T[k]])
            tt("gpsimd", h1t[k], h1t[k], sh1b, ALU.add, [h1T[k], modT[0]], [h1T[k]])

        def p1_s2(i):
            k = i % 2
            transpose8(h1t[k], h1T[k], lambda half, i=i: hT[:, half * 4:(half + 1) * 4, i * 128:(i + 1) * 128], hTT[i // 4], k)

        for step in range(NT + 1):
            if step < NT:
                p1_s1(step)
            if step >= 1:
                p1_s2(step - 1)
        P.barrier()
        if stop_after == 1:
            return finish_debug(nc, P, dbg_d, hT.bitcast(F32).rearrange('p a b -> p (a b)'), out_d)

        WI0 = PH + 16384
        WI = [V3(WI0 + k * 1024, 8, 128, F32R) for k in range(2)]; WIT = [T(), T()]
        wi_state = [0]
        BD0 = WI0 + 2048
        bdA = V3(BD0, 4, 128, F32R); bdX = V3(BD0 + 512, 4, 128, F32R); bdT = T()
        dma("gpsimd", bdA, bda_d.rearrange("p (a b) -> p a b", a=4), [], [bdT])
        dma("gpsimd", bdX, bdx_d.rearrange("p (a b) -> p a b", a=4), [], [bdT])
        LB0 = BD0 + 1024
        LBR = [V(LB0 + k * S, S, F32R) if k == 1 else None for k in range(8)]
        LB = [LBR[k].bitcast(F32) if k == 1 else V(LB0 + k * S, S) for k in range(8)]
        LBT = [T() for _ in range(8)]
        LRU0 = LB0 + 8 * S
        assert LRU0 + 4 * S <= ARENA, LRU0 + 4 * S
        lruT = V3(LRU0, 4, S, F32R)
        lru0 = lruT.bitcast(F32)
        lruTT = [T() for _ in range(4)]
        win_v = win_d.rearrange("(kc p) n -> p kc n", p=128)
        evac_state = [0]

        def load_wi(col_specs):
            k = wi_state[0]
            wi_state[0] = 1 - k
            for (dst0, c0, n) in col_specs:
                dma("gpsimd", WI[k][:, :, dst0:dst0 + n], win_v[:, :, c0:c0 + n], [], [WIT[k]])
            return WI[k], WIT[k]

        def inproj_fm(w, wT_, dstfn, dstT):
            for tb in range(4):
                pb, pt = bank()
                for kc in range(8):
                    mm(pb[:, :], w[:, kc, :], hT[:, kc, tb * 512:(tb + 1) * 512], kc == 0, kc == 7, [wT_, hTT[tb]], [pt])
                evac_state[0] += 1
                cp("scalar" if evac_state[0] % 2 == 0 else "vector", dstfn(tb), pb[:, :], [pt], [dstT])

        cw = lambda c, k: C(C_CW + c * 4 + k, 1)
        colc = lambda base, c: C(base + c, 1)
        acc = LB[7]; accT = LBT[7]
        for c in range(4):
            w, wT_ = load_wi([(0, 768 + c * 128, 128)])
            inproj_fm(w, wT_, lambda tb: LB[0][:, tb * 512:(tb + 1) * 512], LBT[0])
            w, wT_ = load_wi([(0, 1280 + c * 128, 128)])
            inproj_fm(w, wT_, lambda tb: LB[2][:, tb * 512:(tb + 1) * 512], LBT[2])
            xr, xc, xg = LB[0], LB[1], LB[2]
            ts("vector", LBR[1], xr, cw(c, 3), colc(C_CB, c), ALU.mult, ALU.add, [LBT[0], cT], [LBT[1]])
            for sh in (1, 2, 3):
                stt(LBR[1][:, sh:], xr[:, :S - sh], cw(c, 3 - sh), xc[:, sh:], ALU.mult, ALU.add, [LBT[0], LBT[1], cT], [LBT[1]])
            for gi, (bd, bcol, dst) in enumerate(((bdA, C_BA, 3), (bdX, C_BX, 4))):
                for tb in range(4):
                    pb, pt = bank()
                    mm(pb[:, :], bd[:, c, :], LBR[1][:, tb * 512:(tb + 1) * 512], True, True, [bdT, LBT[1]], [pt])
                    act(LB[dst][:, tb * 512:(tb + 1) * 512], pb[:, :], AF.Sigmoid, [pt, cT], [LBT[dst]], bias=colc(bcol, c))
            r, ig = LB[3], LB[4]
            act(LB[6], r, AF.Tanh, [LBT[3], sp8T], [LBT[6]], scale=sp8[:, c:c + 1])
            act(LB[3], r, AF.Exp, [LBT[3], ls8T], [LBT[3]], scale=ls8[:, c:c + 1])
            a = LB[3]
            tt("gpsimd", LB[5], a, a, ALU.mult, [LBT[3]], [LBT[5]])
            stt(LB[5], LB[5], 1.0, LB[6], ALU.add, ALU.mult, [LBT[5], LBT[6]], [LBT[5]])
            act(LB[5], LB[5], AF.Sqrt, [LBT[5]], [LBT[5]])
            tt("gpsimd", LB[4], ig, xc, ALU.mult, [LBT[4], LBT[1]], [LBT[4]])
            tt("vector", LB[4], LB[4], LB[5], ALU.mult, [LBT[4], LBT[5]], [LBT[4]])
            P.op("vector", lambda e, a=a: e.tensor_tensor_scan(out=LB[0], data0=a, data1=LB[4], initial=0.0, op0=ALU.mult, op1=ALU.add),
                 [LBT[3], LBT[4], LBT[0]], [LBT[0]])
            act(LB[6], xg, AF.Square, [LBT[2]], [LBT[6]])
            ts("gpsimd", LB[6], LB[6], 0.044715, 1.0, ALU.mult, ALU.add, [LBT[6]], [LBT[6]])
            tt("gpsimd", LB[6], LB[6], xg, ALU.mult, [LBT[6], LBT[2]], [LBT[6]])
            act(LB[6], LB[6], AF.Sigmoid, [LBT[6]], [LBT[6]], scale=1.5957691216057308)
            tt("gpsimd", LB[6], LB[6], xg, ALU.mult, [LBT[6], LBT[2]], [LBT[6]])
            tt("vector", lruT[:, c, :], LB[0], LB[6], ALU.mult, [LBT[0], LBT[6]], [lruTT[c]])
            if c == 0:
                tt("gpsimd", acc, lru0[:, c, :], lru0[:, c, :], ALU.mult, [lruTT[c]], [accT])
            else:
                tt("gpsimd", LB[5], lru0[:, c, :], lru0[:, c, :], ALU.mult, [lruTT[c]], [LBT[5]])
                tt("gpsimd", acc, acc, LB[5], ALU.add, [accT, LBT[5]], [accT])
        for tb in range(4):
            pb, pt = bank()
            mm(pb[:, :], onesF, acc[:, tb * 512:(tb + 1) * 512], True, True, [cT, accT], [pt])
            act(LB[6][:, tb * 512:(tb + 1) * 512], pb[:, :], AF.Sqrt, [pt], [LBT[6]], scale=1.0 / 512, bias=EPS)
        P.op("vector", lambda e: e.reciprocal(out=LB[6], in_=LB[6]), [LBT[6]], [LBT[6]])
        for c in range(4):
            stt(lruT[:, c, :], lru0[:, c, :], colc(C_GL, c), LB[6], ALU.mult, ALU.mult, [lruTT[c], LBT[6], cT], [lruTT[c]])
        P.barrier()
        if stop_after == 2:
            return finish_debug(nc, P, dbg_d, lruT.bitcast(F32).rearrange('p a b -> p (a b)'), out_d)

        Q0 = LB0
        qT = V3(Q0, 4, S, F32R); qTT = [T() for _ in range(4)]
        kk = V3(Q0 + 4 * S, 2, S, F32R); kkT = [T(), T()]
        VA0 = Q0 + 6 * S
        vflat = V(VA0, NT * 132, F32R)
        vaug = vflat.rearrange("p (t g d) -> p t g d", t=NT, g=2)
        vT = T()
        assert VA0 + NT * 132 <= LRU0
        ones4 = onesF[:, 0:32].rearrange("p (t g d) -> p t g d", t=NT, g=2)
        cp("vector", vaug[:, :, :, 64:65], ones4, [cT], [vT])
        ts("vector", vaug[:, :, :, 65:66], ones4, 0.0, None, ALU.mult, None, [cT], [vT])
        for c in range(4):
            w, wT_ = load_wi([(0, c * 128, 128)])
            inproj_fm(w, wT_, lambda tb, c=c: qT[:, c, tb * 512:(tb + 1) * 512], qTT[c])
        for g in range(2):
            w, wT_ = load_wi([(0, 512 + g * 64, 64), (64, 512 + g * 64, 64)])
            inproj_fm(w, wT_, lambda tb, g=g: kk[:, g, tb * 512:(tb + 1) * 512], kkT[g])
        w, wT_ = load_wi([(0, 640, 128)])
        for i in range(NT):
            pb, pt = bank()
            for kc in range(8):
                mm(pb[:, 0:128], hT[:, kc, i * 128:(i + 1) * 128], w[:, kc, :], kc == 0, kc == 7, [wT_, hTT[i // 4]], [pt])
            cp("scalar" if i % 2 == 0 else "vector", vaug[:, i, :, 0:64], pb[:, 0:128].rearrange("p (g d) -> p g d", g=2), [pt], [vT])
        P.barrier()

        AT0 = PH
        attnT = V3(AT0, 4, S, F32R); attnTT = T()
        ET0 = PH + 4 * S
        eT = [[[V(ET0 + ((par * 2 + g) * 2 + kbi) * 512, 512, F32R) for kbi in range(2)] for g in range(2)] for par in range(2)]
        eTT = [[[T(), T()], [T(), T()]], [[T(), T()], [T(), T()]]]
        AN0 = ET0 + 4096
        attn_t = [V(AN0 + k * 512, 512) for k in range(2)]; attn_tT = [T(), T()]
        junk512 = V(AN0 + 1024, 512); junk512T = T()
        den2 = [small(8) for _ in range(2)]
        ssa, _ = small(2)
        rsa, _ = small(2)
        ssaT = [T(), T()]; rsaT = [T(), T()]
        scale = 0.125

        def kbs_of(n):
            return ([n - 1] if n > 0 else []) + [n]

        def at_A(n):
            par = n % 2
            for g in range(2):
                for kbi, kb in enumerate(kbs_of(n)):
                    diag = (kb == n)
                    for half in range(2):
                        pb, pt = bank()
                        mm(pb[:, 0:256], identR, (maskDR if diag else maskPR)[:, 0:256], True, False, [crT], [pt])
                        for u_ in range(2):
                            hh = u_ * 2 + half
                            c = 2 * g + hh // 2
                            h0 = half * 64
                            mm(pb[:, u_ * 128:(u_ + 1) * 128], kk[h0:h0 + 64, g, kb * 128:(kb + 1) * 128],
                               qT[h0:h0 + 64, c, n * 128:(n + 1) * 128], False, u_ == 1, [kkT[g], qTT[c]], [pt], mode=64)
                        act(eT[par][g][kbi][:, half * 256:(half + 1) * 256], pb[:, 0:256], AF.Exp, [pt], [eTT[par][g][kbi]], scale=scale)

        def at_B(n):
            par = n % 2
            k = n % 2
            kbs = kbs_of(n)
            for g in range(2):
                den, denT = den2[g]
                pb, pt = bank()
                for hh in range(4):
                    pos = (hh % 2) * 2 + hh // 2
                    for kbi, kb in enumerate(kbs):
                        mm(pb[:, hh * 66:(hh + 1) * 66], eT[par][g][kbi][:, pos * 128:(pos + 1) * 128], vaug[:, kb, g, :],
                           kbi == 0, kbi == len(kbs) - 1, [eTT[par][g][kbi], vT], [pt])
                pv = pb[:, 0:264].rearrange("p (h d) -> p h d", h=4)
                tt("vector", den[:, 0:4], pv[:, :, 64], sinkexp[:, g * 4:(g + 1) * 4], ALU.add, [pt, sinkexpT], [denT])
                P.op("vector", lambda e, den=den: e.reciprocal(out=den[:, 0:4], in_=den[:, 0:4]), [denT], [denT])
                for hh in range(4):
                    hd = g * 4 + hh
                    if hh % 2 == 0:
                        ts("vector", attn_t[k][:, hd * 64:(hd + 1) * 64], pv[:, hh, 0:64], den[:, hh:hh + 1], None, ALU.mult, None, [pt, denT], [attn_tT[k]])
                    else:
                        act(attn_t[k][:, hd * 64:(hd + 1) * 64], pv[:, hh, 0:64], AF.Copy, [pt, denT], [attn_tT[k]], scale=den[:, hh:hh + 1])
            rms_rstd(attn_t[k], attn_tT[k], junk512, junk512T, ssa[:, k:k + 1], ssaT[k], rsa[:, k:k + 1], rsaT[k], 512)
            ts("vector", attn_t[k], attn_t[k], rsa[:, k:k + 1], None, ALU.mult, None, [attn_tT[k], rsaT[k]], [attn_tT[k]])
            pb, pt = bank()
            for c in range(4):
                tr(pb[:, c * 128:(c + 1) * 128], attn_t[k][:, c * 128:(c + 1) * 128], identF, [attn_tT[k], cT], [pt])
            for c in range(4):
                if c % 2 == 0:
                    ts("vector", attnT[:, c, n * 128:(n + 1) * 128], pb[:, c * 128:(c + 1) * 128], colc(C_GA, c), None, ALU.mult, None, [pt, cT], [attnTT])
                else:
                    act(attnT[:, c, n * 128:(n + 1) * 128], pb[:, c * 128:(c + 1) * 128], AF.Copy, [pt, cT], [attnTT], scale=colc(C_GA, c))

        at_A(0)
        for n in range(NT):
            if n + 1 < NT:
                at_A(n + 1)
            at_B(n)
        P.barrier()
        if stop_after == 3:
            return finish_debug(nc, P, dbg_d, attnT.bitcast(F32).rearrange('p a b -> p (a b)'), out_d)

        WO0 = PH + 4 * S
        wo = V3(WO0, 8, D, F32R); woT = T()
        wout_v = wout_d.rearrange("(kc p) n -> p kc n", p=128)
        for hf in range(2):
            dma("gpsimd", wo[:, :, hf * 512:(hf + 1) * 512], wout_v[:, :, hf * 512:(hf + 1) * 512], [], [woT])
        T0 = WO0 + 8 * D
        xt = [V(T0 + k * D, D) for k in range(2)]; xtT = [T(), T()]
        x1t = [V(T0 + 2 * D + k * D, D) for k in range(2)]; x1T = [T(), T()]
        h2t = [V(T0 + 4 * D + k * D, D) for k in range(2)]; h2T_ = [T(), T()]
        h2Tr = [V3(T0 + 6 * D + k * D, 8, 128) for k in range(2)]; h2TrT = [T(), T()]
        WR0 = T0 + 8 * D
        wr = V3(WR0, 8, E); wrT = T()
        dma("sync", wr, wr_d.rearrange("(kc p) n -> p kc n", p=128), [], [wrT])
        R0 = WR0 + 8 * E
        assert R0 + 1024 <= LB0 + 8 * S
        rsm = [R0]

        def rsmall(n):
            o = rsm[0]
            rsm[0] += (n + 7) // 8 * 8
            return V(o, n), T()

        lg, lgT = rsmall(32)
        top8, top8T = rsmall(8)
        idx8_o = rsm[0]; rsm[0] += 8
        idx8 = V(idx8_o, 8, U32); idx8T = T()
        idxf, idxfT = rsmall(4)
        negm, negmT = rsmall(1)
        e4, e4T = rsmall(4)
        ssum, ssumT = rsmall(1)
        mask, maskT = rsmall(32)
        rkp, rkpT = rsmall(32)
        ohj, ohjT = rsmall(32)
        ss2, _ = rsmall(2)
        rs2, _ = rsmall(2)
        xsT_d = T()
        x1dT = T()
        mergedT = lambda kc: (attnT[:, kc, :] if kc < 4 else lruT[:, kc - 4, :])
        mT = lambda kc: (attnTT if kc < 4 else lruTT[kc - 4])
        ss2T = [T(), T()]; rs2T = [T(), T()]
        gatesTi = [T() for _ in range(NT)]; destfTi = [T() for _ in range(NT)]; destiTi = [T() for _ in range(NT)]

        def p3_s1(i):
            k = i % 2
            dma("sync", xt[k], x_d[i * 128:(i + 1) * 128, :], [], [xtT[k]])
            for hf in range(2):
                pb, pt = bank()
                for kc in range(8):
                    mm(pb[:, :], mergedT(kc)[:, i * 128:(i + 1) * 128], wo[:, kc, hf * 512:(hf + 1) * 512], kc == 0, kc == 7, [mT(kc), woT], [pt])
                sl = slice(hf * 512, (hf + 1) * 512)
                tt("vector", x1t[k][:, sl], pb[:, :], gt1b[:, sl], ALU.mult, [pt, modT[2]], [x1T[k]])
                tt("gpsimd", x1t[k][:, sl], x1t[k][:, sl], xt[k][:, sl], ALU.add, [x1T[k], xtT[k]], [x1T[k]])
            dma("sync", x1_d[i * 128:(i + 1) * 128, :], x1t[k], [x1T[k]], [])
            rms_rstd(x1t[k], x1T[k], h2t[k], h2T_[k], ss2[:, k:k + 1], ss2T[k], rs2[:, k:k + 1], rs2T[k], D)
            stt(h2t[k], x1t[k], rs2[:, k:k + 1], s2b, ALU.mult, ALU.mult, [x1T[k], rs2T[k], modT[4]], [h2T_[k]])
            tt("gpsimd", h2t[k], h2t[k], sh2b, ALU.add, [h2T_[k], modT[3]], [h2T_[k]])

        def p3_s2(i):
            k = i % 2
            transpose8(h2t[k], h2T_[k], lambda half, k=k: h2Tr[k][:, half * 4:(half + 1) * 4, :], h2TrT[k], k)
            pb, pt = bank()
            for kc in range(8):
                mm(pb[:, 0:E], h2Tr[k][:, kc, :], wr[:, kc, :], kc == 0, kc == 7, [h2TrT[k], wrT], [pt])
            tt("vector", lg, pb[:, 0:E], C(C_BR, E), ALU.add, [pt, cT], [lgT])
            P.op("vector", lambda e: e.max(out=top8, in_=lg), [lgT], [top8T])
            P.op("vector", lambda e: e.max_index(out=idx8, in_max=top8, in_values=lg), [lgT, top8T], [idx8T])
            cp("vector", idxf, idx8[:, 0:4], [idx8T], [idxfT])
            ts("vector", negm, top8[:, 0:1], -1.0, None, ALU.mult, None, [top8T], [negmT])
            act(e4, top8[:, 0:4], AF.Exp, [top8T, negmT], [e4T, ssumT], bias=negm, accum_out=ssum)
            P.op("vector", lambda e: e.reciprocal(out=ssum, in_=ssum), [ssumT], [ssumT])
            ts("vector", gates_all[:, i * 4:(i + 1) * 4], e4, ssum, None, ALU.mult, None, [e4T, ssumT], [gatesTi[i]])
            ts("vector", mask, lg, top8[:, 3:4], None, ALU.is_ge, None, [lgT, top8T], [maskT])
            pb, pt = bank()
            if i > 0:
                mm(pb[:, 0:E], onesF, cum, True, False, [cT, cumT], [pt])
            mm(pb[:, 0:E], UF, mask, i == 0, True, [cT, maskT], [pt])
            tt("vector", rkp, pb[:, 0:E], C(C_RB, E), ALU.add, [pt, cT], [rkpT])
            if i == 0:
                cp("vector", cum, mask, [maskT], [cumT])
            else:
                tt("vector", cum, cum, mask, ALU.add, [cumT, maskT], [cumT])
            for j in range(4):
                stt(ohj, C(C_IOTA, E), idxf[:, j:j + 1], rkp, ALU.is_equal, ALU.mult, [cT, idxfT, rkpT], [ohjT, destfTi[i]],
                    accum_out=destf_all[:, i * 4 + j:i * 4 + j + 1])
            cp("vector", desti_all[:, i * 4:(i + 1) * 4], destf_all[:, i * 4:(i + 1) * 4], [destfTi[i]], [destiTi[i]])

        def p3_s3(i):
            k = i % 2
            for j in range(4):
                P.dma("gpsimd", lambda e, i=i, j=j, k=k: e.indirect_dma_start(
                    out=xs_d, out_offset=bass.IndirectOffsetOnAxis(ap=desti_all[:, i * 4 + j:i * 4 + j + 1], axis=0),
                    in_=h2t[k], in_offset=None), [h2T_[k], destiTi[i]], [])

        for step in range(NT + 1):
            if step < NT:
                p3_s1(step)
            if step >= 1:
                p3_s2(step - 1)
                p3_s3(step - 1)
        cntb, cntbT = rsmall(32)
        nblk, nblkT = rsmall(32)
        pend, pendT = rsmall(32)
        pst, pstT = rsmall(32)
        ej, ejT = rsmall(NB)
        pstj, pstjT = rsmall(NB)
        tmpj, tmpjT = rsmall(NB)
        sj, sjT = rsmall(NB)
        rowj, rowjT = rsmall(NB)
        valid, validT = rsmall(NB)
        skp, skpT = rsmall(NB)
        ebW, ebWT = rsmall(NB)
        ebD, ebDT = rsmall(NB)
        idxW = V(MOD, NB * 8, I32).rearrange("p (j k) -> p j k", k=8)
        idxD = V(MOD + NB * 8, NB * 4, I32).rearrange("p (j k) -> p j k", k=4)
        idxX = V(MOD + NB * 12, NB * 2, I32).rearrange("p (j k) -> p j k", k=2)
        idxB = V(MOD + NB * 14, NB, I32)
        idxBd = V(MOD + NB * 15, NB, I32)
        tblT = T()
        jrow = C(C_J, NB)
        pid = C(C_PID, 1)
        pb, pt = bank()
        mm(pb[:, 0:E], onesF, cum, True, True, [cT, cumT], [pt])
        cp("vector", cntb, pb[:, 0:E], [pt], [cntbT])
        ts("vector", nblk, cntb, 0.0, None, ALU.is_gt, None, [cntbT], [nblkT])
        for sx in range(1, EREG // BSZ):
            stt(nblk, cntb, float(BSZ * sx), nblk, ALU.is_gt, ALU.add, [cntbT, nblkT], [nblkT])
        P.op("vector", lambda e: e.tensor_tensor_scan(out=pend, data0=onesF[:, 0:E], data1=nblk, initial=0.0, op0=ALU.mult, op1=ALU.add),
             [cT, nblkT], [pendT])
        tt("vector", pst, pend, nblk, ALU.subtract, [pendT, nblkT], [pstT])
        ts("vector", ej, jrow, pend[:, 0:1], None, ALU.is_ge, None, [cT, pendT], [ejT])
        for e_ in range(1, E):
            stt(ej, jrow, pend[:, e_:e_ + 1], ej, ALU.is_ge, ALU.add, [cT, pendT, ejT], [ejT])
        ts("vector", valid, ej, E - 0.5, None, ALU.is_lt, None, [ejT], [validT])
        ts("vector", ej, ej, float(E - 1), None, ALU.min, None, [ejT], [ejT])
        for e_ in range(E):
            ts("vector", tmpj, ej, float(e_), None, ALU.is_equal, None, [ejT], [tmpjT])
            if e_ == 0:
                ts("vector", pstj, tmpj, pst[:, 0:1], None, ALU.mult, None, [tmpjT, pstT], [pstjT])
            else:
                stt(pstj, tmpj, pst[:, e_:e_ + 1], pstj, ALU.mult, ALU.add, [tmpjT, pstT, pstjT], [pstjT])
        tt("vector", sj, jrow, pstj, ALU.subtract, [cT, pstjT], [sjT])
        ts("vector", skp, sj, 0.0, None, ALU.is_equal, None, [sjT], [skpT])
        ts("vector", skp, skp, -BIG, BIG, ALU.mult, ALU.add, [skpT], [skpT])
        ts("vector", tmpj, sj, float(BSZ), None, ALU.mult, None, [sjT], [tmpjT])
        stt(rowj, ej, float(EREG), tmpj, ALU.mult, ALU.add, [ejT, tmpjT], [rowjT])
        ts("vector", rowj, rowj, -BIG, None, ALU.add, None, [rowjT], [rowjT])
        tt("vector", rowj, rowj, valid, ALU.mult, [rowjT, validT], [rowjT])
        ts("vector", rowj, rowj, BIG, pid, ALU.add, ALU.add, [rowjT, cT], [rowjT])
        stt(ebW, ej, 1024.0, skp, ALU.mult, ALU.add, [ejT, skpT], [ebWT])
        ts("vector", ebW, ebW, pid, None, ALU.add, None, [ebWT, cT], [ebWT])
        stt(ebD, ej, 512.0, skp, ALU.mult, ALU.add, [ejT, skpT], [ebDT])
        ts("vector", ebD, ebD, pid, None, ALU.add, None, [ebDT, cT], [ebDT])
        for k_ in range(8):
            ts("vector", idxW[:, :, k_], ebW, float(k_ * 128), None, ALU.add, None, [ebWT], [tblT])
        for k_ in range(4):
            ts("vector", idxD[:, :, k_], ebD, float(k_ * 128), None, ALU.add, None, [ebDT], [tblT])
        for k_ in range(2):
            ts("vector", idxX[:, :, k_], rowj, float(k_ * 128), None, ALU.add, None, [rowjT], [tblT])
        ts("vector", idxB, ej, 128.0, pid, ALU.mult, ALU.add, [ejT, cT], [tblT])
        cp("vector", idxBd, ej, [ejT], [tblT])
        P.barrier()
        if stop_after == 4:
            return finish_debug(nc, P, dbg_d, None, out_d)

        WSg = [V3(PH + k * 2048, 4, 512, F32R) for k in range(8)]; WSgT = [T() for _ in range(8)]
        WSd = [V3(PH + (8 + k) * 2048, 4, 512, F32R) for k in range(4)]; WSdT = [T() for _ in range(4)]
        X0 = PH + 12 * 2048
        xsb = [V3(X0 + k * NJ * D, NJ, D) for k in range(2)]; xsbT = [T(), T()]
        XT0 = X0 + 2 * NJ * D
        xsT = [V3(XT0 + k * 8 * BSZ, 8, BSZ, F32R) for k in range(2)]; xsTT = [T(), T()]
        A0 = XT0 + 2 * 8 * BSZ
        actT = V3(A0, 8, BSZ, F32R); actTT = T()
        Y0 = A0 + 8 * BSZ
        yb = [V3(Y0 + k * NJ * D, NJ, D) for k in range(2)]; ybT = [T(), T()]
        G0 = Y0 + 2 * NJ * D
        gsb = [V(G0 + k * BSZ, BSZ) for k in range(6)]; gsbT = [T() for _ in range(6)]
        BG0 = G0 + 6 * BSZ
        bgub = [V(BG0 + k * 16, 16) for k in range(2)]; bgubT = [T(), T()]
        BD0_ = BG0 + 32
        bdnb = [V(BD0_ + k * D, D) for k in range(2)]; bdnbT = [T(), T()]
        assert BD0_ + 2 * D <= ARENA, BD0_ + 2 * D

        bregs = {}

        def breg(e, bound):
            if bound not in bregs:
                bregs[bound] = e.to_reg(bound)
            return bregs[bound]

        def igather(out, src, idx_ap, bound, reads, writes):
            P.dma("gpsimd", lambda e: e.indirect_dma_start(out=out, out_offset=None, in_=src,
                  in_offset=bass.IndirectOffsetOnAxis(ap=idx_ap, axis=0), bounds_check=breg(e, bound), oob_is_err=False), reads, writes)

        def load_block_small(j):
            k = j % 2
            for jj in range(NJ):
                igather(xsb[k][:, jj, :], xs_d, idxX[:, j, jj:jj + 1], NSLOT - 1, [tblT], [xsbT[k]])
            igather(bgub[k], bgur_d, idxB[:, j:j + 1], E * 128 - 1, [tblT], [bgubT[k]])
            igather(bdnb[k], bdn_d, idxBd[:, j:j + 1], E - 1, [tblT], [bdnbT[k]])

        def load_wg(j, k_):
            igather(WSg[k_].rearrange("p a b -> p (a b)"), wgur_d, idxW[:, j, k_:k_ + 1], E * 1024 - 1, [tblT], [WSgT[k_]])

        def load_wd(j, k_):
            igather(WSd[k_].rearrange("p a b -> p (a b)"), wdnr_d, idxD[:, j, k_:k_ + 1], E * 512 - 1, [tblT], [WSdT[k_]])

        load_block_small(0)
        for k_ in range(8):
            load_wg(0, k_)
        for k_ in range(4):
            load_wd(0, k_)
        for j in range(NB):
            k = j % 2
            for kc in range(8):
                pb, pt = bank()
                for jj in range(NJ):
                    tr(pb[:, jj * 128:(jj + 1) * 128], xsb[k][:, jj, kc * 128:(kc + 1) * 128], identF, [xsbT[k], cT], [pt])
                cp("scalar" if kc % 2 == 0 else "vector", xsT[k][:, kc, :], pb[:, 0:BSZ], [pt], [xsTT[k]])
            if j + 1 < NB:
                load_block_small(j + 1)
            for fc in range(8):
                qg = (fc // 4) * 2
                lc = (fc % 4) * 128
                pg, pgt = bank()
                for kc in range(8):
                    wi_ = qg * 2 + kc // 4
                    mm(pg[:, 0:BSZ], WSg[wi_][:, kc % 4, lc:lc + 128], xsT[k][:, kc, :], kc == 0, kc == 7, [WSgT[wi_], xsTT[k]], [pgt])
                pu, put = bank()
                for kc in range(8):
                    wi_ = (qg + 1) * 2 + kc // 4
                    mm(pu[:, 0:BSZ], WSg[wi_][:, kc % 4, lc:lc + 128], xsT[k][:, kc, :], kc == 0, kc == 7, [WSgT[wi_], xsTT[k]], [put])
                if fc % 4 == 3 and j + 1 < NB:
                    for k_ in range(qg * 2, qg * 2 + 4):
                        load_wg(j + 1, k_)
                g3 = (fc % 2) * 3
                gb, gbT = gsb[g3], gsbT[g3]
                ub, ubT = gsb[g3 + 1], gsbT[g3 + 1]
                sl_, slT = gsb[g3 + 2], gsbT[g3 + 2]
                ts("vector", gb, pg[:, 0:BSZ], bgub[k][:, fc:fc + 1], 7.0, ALU.add, ALU.min, [pgt, bgubT[k]], [gbT])
                ts("vector", ub, pu[:, 0:BSZ], bgub[k][:, 8 + fc:8 + fc + 1], 7.0, ALU.add, ALU.min, [put, bgubT[k]], [ubT])
                ts("vector", ub, ub, -7.0, 1.0, ALU.max, ALU.add, [ubT], [ubT])
                act(sl_, gb, AF.Silu, [gbT], [slT], scale=1.702)
                stt(actT[:, fc, :], sl_, 1.0 / 1.702, ub, ALU.mult, ALU.mult, [slT, ubT], [actTT])
            for hf in range(2):
                for jj in range(NJ):
                    pb, pt = bank()
                    for fc in range(8):
                        wi_ = hf * 2 + fc // 4
                        mm(pb[:, :], actT[:, fc, jj * 128:(jj + 1) * 128], WSd[wi_][:, fc % 4, :], fc == 0, fc == 7, [actTT, WSdT[wi_]], [pt])
                    sl = slice(hf * 512, (hf + 1) * 512)
                    tt("vector", yb[k][:, jj, sl], pb[:, :], bdnb[k][:, sl], ALU.add, [pt, bdnbT[k]], [ybT[k]])
                if j + 1 < NB:
                    load_wd(j + 1, hf * 2)
                    load_wd(j + 1, hf * 2 + 1)
            for jj in range(NJ):
                P.dma("gpsimd", lambda e, j=j, jj=jj, k=k: e.indirect_dma_start(
                    out=ys_d, out_offset=bass.IndirectOffsetOnAxis(ap=idxX[:, j, jj:jj + 1], axis=0),
                    in_=yb[k][:, jj, :], in_offset=None, bounds_check=breg(e, NSLOT - 1), oob_is_err=False), [ybT[k], tblT], [])
        P.barrier()
        if stop_after == 5:
            return finish_debug(nc, P, dbg_d, None, out_d)

        gt2s = V(PH, D); gt2sT = T()
        gfin = V(PH + D, D); gfinT = T()
        dma("sync", gt2s, gt2_d, [], [gt2sT])
        dma("sync", gfin, gfin_d, [], [gfinT])
        F0 = PH + 2 * D
        x1r = [V(F0 + k * D, D) for k in range(2)]; x1rT = [T(), T()]
        yg = [[V(F0 + 2 * D + (k * 4 + j) * D, D) for j in range(4)] for k in range(2)]
        ygT = [[T() for _ in range(4)] for _ in range(2)]
        ob = [V(F0 + 10 * D + k * D, D) for k in range(2)]; obT = [T(), T()]
        ss3, _ = small(2)
        rs3, _ = small(2)
        ss3T = [T(), T()]; rs3T = [T(), T()]

        def p5_s1(i):
            k = i % 2
            dma("sync", x1r[k], x1_d[i * 128:(i + 1) * 128, :], [], [x1rT[k]])
            for j in range(4):
                P.dma("gpsimd", lambda e, i=i, j=j, k=k: e.indirect_dma_start(
                    out=yg[k][j], out_offset=None, in_=ys_d,
                    in_offset=bass.IndirectOffsetOnAxis(ap=desti_all[:, i * 4 + j:i * 4 + j + 1], axis=0)), [destiTi[i]], [ygT[k][j]])

        def p5_s2(i):
            k = i % 2
            ts("vector", ob[k], yg[k][0], gates_all[:, i * 4:i * 4 + 1], None, ALU.mult, None, [ygT[k][0], gatesTi[i]], [obT[k]])
            for j in range(1, 4):
                stt(ob[k], yg[k][j], gates_all[:, i * 4 + j:i * 4 + j + 1], ob[k], ALU.mult, ALU.add, [ygT[k][j], gatesTi[i], obT[k]], [obT[k]])
            tt("gpsimd", ob[k], ob[k], gt2s, ALU.mult, [obT[k], gt2sT], [obT[k]])
            tt("vector", x1r[k], x1r[k], ob[k], ALU.add, [x1rT[k], obT[k]], [x1rT[k]])

        def p5_s3(i):
            k = i % 2
            rms_rstd(x1r[k], x1rT[k], ob[k], obT[k], ss3[:, k:k + 1], ss3T[k], rs3[:, k:k + 1], rs3T[k], D)
            stt(ob[k], x1r[k], rs3[:, k:k + 1], gfin, ALU.mult, ALU.mult, [x1rT[k], rs3T[k], gfinT], [obT[k]])
            dma("sync", out_d[i * 128:(i + 1) * 128, :], ob[k], [obT[k]], [])

        p5_s1(0)
        for step in range(NT + 1):
            if step >= 1:
                p5_s3(step - 1)
            if step + 1 < NT:
                p5_s1(step + 1)
            if step < NT:
                p5_s2(step)
        P.barrier()
        P.emit()
    return nc


def finish_debug(nc, P, dbg_d, src, out_d):
    if src is not None:
        n = src.shape[1]
        P.dma("sync", lambda e: e.dma_start(out=dbg_d[:, 0:n], in_=src), [], [])
    P.barrier()
    P.emit()
    return nc


def host_consts(inp):
    c = np.zeros((128, NCONST), np.float32)
    c[:, C_ID:C_ID + 128] = np.eye(128, dtype=np.float32)
    c[:, C_ONE:C_ONE + 128] = 1.0
    kk_, qq = np.meshgrid(np.arange(128), np.arange(128), indexing="ij")
    c[:, C_U:C_U + 128] = (kk_ < qq).astype(np.float32)
    md = np.where(kk_ <= qq, 0.0, NEG).astype(np.float32)
    mp = np.where(kk_ > qq, 0.0, NEG).astype(np.float32)
    c[:, C_MD:C_MD + 512] = np.tile(md, (1, 4))
    c[:, C_MP:C_MP + 512] = np.tile(mp, (1, 4))
    c[:, C_IOTA:C_IOTA + 32] = np.arange(32, dtype=np.float32)[None, :]
    c[:, C_RB:C_RB + 32] = (np.arange(32, dtype=np.float32) * EREG)[None, :]
    c[:, C_PID] = np.arange(128, dtype=np.float32)
    c[:, C_J:C_J + NB] = np.arange(NB, dtype=np.float32)[None, :]
    c[:, C_BR:C_BR + 32] = inp["b_router"][0][None, :]
    c[:, C_SINK:C_SINK + 8] = inp["sinks"][0][None, :]
    cwt = inp["conv_w"][0]
    c[:, C_CW:C_CW + 16] = cwt.T.reshape(4, 128, 4).transpose(1, 0, 2).reshape(128, 16)
    col = lambda v: np.asarray(v, np.float32).reshape(4, 128).T
    c[:, C_CB:C_CB + 4] = col(inp["conv_b"][0])
    c[:, C_BA:C_BA + 4] = col(inp["b_rg_a"][0].reshape(512))
    c[:, C_BX:C_BX + 4] = col(inp["b_rg_x"][0].reshape(512))
    c[:, C_LAM:C_LAM + 4] = col(inp["lru_lambda"][0])
    c[:, C_GA:C_GA + 4] = col(inp["g_attn_out"][0])
    c[:, C_GL:C_GL + 4] = col(inp["g_lru_out"][0])
    return c


def host_blockdiag(w):
    bd = np.zeros((128, 4, 128), np.float32)
    for c in range(4):
        bd[0:64, c, 0:64] = w[2 * c]
        bd[64:128, c, 64:128] = w[2 * c + 1]
    return bd.reshape(128, 512)


def host_wgu(w):
    w6 = w.reshape(E, 2, 4, 128, 4, 512)
    cg = [0, 2, 1, 3]
    out = np.empty((E, 4, 2, 128, 4, 512), np.float32)
    for q in range(4):
        out[:, q] = w6[:, :, :, :, cg[q], :].transpose(0, 1, 3, 2, 4)
    return out.reshape(E * 1024, 2048)


def host_wdn(w):
    w6 = w.reshape(E, 2, 4, 128, 2, 512)
    out = np.ascontiguousarray(w6.transpose(0, 4, 1, 3, 2, 5))
    return out.reshape(E * 512, 2048)


def make_in_maps(inp):
    inp = {k: np.asarray(v) for k, v in inp.items()}
    bc = lambda v: np.ascontiguousarray(np.broadcast_to(np.asarray(v, np.float32)[None, :], (128, v.shape[0])))
    shared = {
        "w_ada": np.ascontiguousarray(inp["w_ada"][0]),
        "b_ada_b": bc(inp["b_ada"][0]),
        "g_mix_b": bc(inp["g_mix"][0]),
        "g_ffn_b": bc(inp["g_ffn"][0]),
        "g_final_b": bc(inp["g_final"]),
        "w_in": np.ascontiguousarray(inp["w_in"][0]),
        "bd_a": host_blockdiag(inp["w_rg_a"][0]),
        "bd_x": host_blockdiag(inp["w_rg_x"][0]),
        "w_out": np.ascontiguousarray(inp["w_out"][0]),
        "w_router": np.ascontiguousarray(inp["w_router"][0]),
        "w_gu_r": host_wgu(inp["w_gu"][0]),
        "w_dn_r": host_wdn(inp["w_down"][0]),
        "b_gu_r": np.ascontiguousarray(inp["b_gu"][0].reshape(E, 16, 128).transpose(0, 2, 1).reshape(E * 128, 16)),
        "b_down": np.ascontiguousarray(inp["b_down"][0]),
        "consts": host_consts(inp),
    }
    maps = []
    for b in range(8):
        m = dict(shared)
        m["x"] = np.ascontiguousarray(inp["x"][b])
        cb = inp["c"][b].reshape(8, 128).T
        m["cB"] = np.ascontiguousarray(np.broadcast_to(cb[:, :, None], (128, 8, 128)).reshape(128, 1024)).astype(np.float32)
        maps.append(m)
    return maps


def kernel(**inputs):
    nc = build()
    maps = make_in_maps(inputs)
    res = run_bass_kernel_spmd(nc, maps, core_ids=list(range(8)))
    return np.stack([np.asarray(r["out"]) for r in res.results], axis=0).astype(np.float32)
```

```python
import numpy as np
import concourse.bass as bass
import concourse.mybir as mybir
from concourse.bass_utils import run_bass_kernel_spmd
from contextlib import ExitStack

F32 = mybir.dt.float32
F32R = mybir.dt.float32r
U32 = mybir.dt.uint32
I32 = mybir.dt.int32
ALU = mybir.AluOpType
AF = mybir.ActivationFunctionType

S = 2048
D = 1024
NT = 16
E = 32
EREG = 2048
BSZ = 256
NJ = BSZ // 128
NB = S * 4 // BSZ + E
NSLOT = E * EREG
BIG = 1000000.0
EPS = 1e-6
NEG = -30000.0

ENGS = ("tensor", "vector", "scalar", "gpsimd", "sync")
NDS = 92
NHW = 12
SAME_ENG_SYNC = True

C_ID, C_ONE, C_U, C_MD, C_MP = 0, 128, 256, 384, 896
C_IOTA, C_RB, C_BR, C_SINK = 1408, 1440, 1472, 1504
C_CW, C_CB, C_BA, C_BX, C_LAM, C_GA, C_GL = 1512, 1528, 1532, 1536, 1540, 1544, 1548
C_PID, C_J = 1552, 1560
NCONST = 1624
ARENA = 52352


class T:
    __slots__ = ("w", "r")

    def __init__(self):
        self.w = None
        self.r = {}


class Prog:
    def __init__(self, nc, es):
        self.nc = nc
        self.q = {e: [] for e in ENGS}
        self.sem = {e: es.enter_context(nc.semaphore("sem_" + e)) for e in ENGS[:4]}
        self.cnt = {e: 0 for e in ENGS}
        self.dsem = [es.enter_context(nc.semaphore(f"dsem{i}")) for i in range(NDS)]
        self.dcnt = [0] * NDS
        self.dnext = 0
        self.dnext_sw = 0
        self.seen = {}

    def _need(self, eng, key, val):
        if key == ("e", eng) and (eng == "tensor" or not SAME_ENG_SYNC):
            return
        if self.seen.get((eng, key), 0) >= val:
            return
        self.seen[(eng, key)] = val
        self.q[eng].append(("wait", key, val))

    def _deps(self, eng, reads, writes):
        for t in reads:
            if t.w:
                self._need(eng, *t.w)
        for t in writes:
            if t.w:
                self._need(eng, *t.w)
            for k, v in t.r.items():
                self._need(eng, k, v)

    def _mark(self, ev, reads, writes):
        for t in reads:
            if t.r.get(ev[0], 0) < ev[1]:
                t.r[ev[0]] = ev[1]
        for t in writes:
            t.w = ev
            t.r = {}

    def op(self, eng, fn, reads=(), writes=()):
        self._deps(eng, reads, writes)
        self.cnt[eng] += 1
        ev = (("e", eng), self.cnt[eng])
        self.q[eng].append(("op", fn))
        self._mark(ev, reads, writes)

    def dma(self, eng, fn, reads=(), writes=()):
        if eng == "gpsimd":
            i = NHW + self.dnext_sw
            self.dnext_sw = (self.dnext_sw + 1) % (NDS - NHW)
        else:
            i = self.dnext
            self.dnext = (i + 1) % NHW
        key = ("d", i)
        if self.dcnt[i] > 0:
            self._need(eng, key, self.dcnt[i])
        self._deps(eng, reads, writes)
        self.dcnt[i] += 16
        ev = (key, self.dcnt[i])
        self.q[eng].append(("dma", fn, i))
        self._mark(ev, reads, writes)

    def barrier(self):
        for eng in ENGS:
            for i in range(NDS):
                if self.dcnt[i] > 0:
                    self._need(eng, ("d", i), self.dcnt[i])
            for e in ENGS[:4]:
                if self.cnt[e] > 0 and e != eng:
                    self._need(eng, ("e", e), self.cnt[e])

    def _semobj(self, key):
        return self.sem[key[1]] if key[0] == "e" else self.dsem[key[1]]

    def emit(self):
        with self.nc.Block() as block:
            for e in ENGS:
                if not self.q[e]:
                    continue

                def body(engh, e=e):
                    for item in self.q[e]:
                        if item[0] == "wait":
                            engh.wait_ge(self._semobj(item[1]), item[2])
                        elif item[0] == "op":
                            item[1](engh).then_inc(self.sem[e], 1)
                        else:
                            item[1](engh).then_inc(self.dsem[item[2]], 16)

                getattr(block, e)(body)


def build(debug=False, stop_after=99):
    nc = bass.Bass("TRN2", target_bir_lowering=False)

    def din(name, shape, dtype=F32):
        return nc.dram_tensor(name, shape, dtype, kind="ExternalInput").ap()

    skind = "ExternalOutput" if debug else "Internal"
    x_d = din("x", [S, D])
    cB_d = din("cB", [128, 8 * 128])
    wada_d = din("w_ada", [D, 6 * D])
    bada_d = din("b_ada_b", [128, 6 * D])
    gmix_d = din("g_mix_b", [128, D])
    gffn_d = din("g_ffn_b", [128, D])
    gfin_d = din("g_final_b", [128, D])
    win_d = din("w_in", [D, 1792])
    bda_d = din("bd_a", [128, 4 * 128])
    bdx_d = din("bd_x", [128, 4 * 128])
    wout_d = din("w_out", [D, D])
    wr_d = din("w_router", [D, E])
    wgur_d = din("w_gu_r", [E * 1024, 2048])
    wdnr_d = din("w_dn_r", [E * 512, 2048])
    bgur_d = din("b_gu_r", [E * 128, 16])
    bdn_d = din("b_down", [E, D])
    const_d = din("consts", [128, NCONST])
    out_d = nc.dram_tensor("out", [S, D], F32, kind="ExternalOutput").ap()
    x1_d = nc.dram_tensor("x1_s", [S, D], F32, kind=skind).ap()
    xs_d = nc.dram_tensor("xs_s", [NSLOT, D], F32, kind=skind).ap()
    ys_d = nc.dram_tensor("ys_s", [NSLOT, D], F32, kind=skind).ap()
    gt2_d = nc.dram_tensor("gt2_s", [128, D], F32, kind=skind).ap()
    dbg_d = nc.dram_tensor("dbg", [128, 8 * S], F32, kind="ExternalOutput").ap() if debug else None

    es = ExitStack()
    with es:
        P = Prog(nc, es)
        AR = es.enter_context(nc.sbuf_tensor("arena", [128, ARENA], F32))
        pbank = [es.enter_context(nc.psum_tensor(f"pb{i}", [128, 512], F32)) for i in range(8)]
        pT = [T() for _ in range(8)]
        pstate = [0]

        def bank():
            i = pstate[0]
            pstate[0] = (i + 1) % 8
            return pbank[i], pT[i]

        ar_addr = [m.memorylocations[0].addr for m in nc.allocations if m.name == "arena_set"][0]
        vcount = [0]

        def V(off, n, dt=F32):
            assert off + n <= ARENA
            vcount[0] += 1
            t = nc.alloc_sbuf_tensor_at(f"v{vcount[0]}", [128, n], dt, offset=ar_addr + off * 4)
            return t[:, :]

        def V3(off, a, b, dt=F32):
            return V(off, a * b, dt).rearrange("p (a b) -> p a b", a=a)

        pe_mode = [128]

        def mm(out, lhsT, rhs, start, stop, reads, writes, mode=128):
            if mode != pe_mode[0]:
                pe_mode[0] = mode
                if P.cnt["tensor"] > 0:
                    P.q["tensor"].append(("wait", ("e", "tensor"), P.cnt["tensor"]))
            P.op("tensor", lambda e: e.matmul(out, lhsT=lhsT, rhs=rhs, start=start, stop=stop), reads, writes)

        def tr(out, in_, ident, reads, writes):
            if pe_mode[0] != 128:
                pe_mode[0] = 128
                P.q["tensor"].append(("wait", ("e", "tensor"), P.cnt["tensor"]))
            P.op("tensor", lambda e: e.transpose(out=out, in_=in_, identity=ident), reads, writes)

        def act(out, in_, func, reads, writes, **kw):
            P.op("scalar", lambda e: e.activation(out=out, in_=in_, func=func, **kw), reads, writes)

        def tt(eng, out, in0, in1, op, reads, writes):
            P.op(eng, lambda e: e.tensor_tensor(out=out, in0=in0, in1=in1, op=op), reads, writes)

        def ts(eng, out, in0, s1, s2, op0, op1, reads, writes, **kw):
            if op1 is None:
                P.op(eng, lambda e: e.tensor_scalar(out=out, in0=in0, scalar1=s1, scalar2=None, op0=op0, **kw), reads, writes)
            else:
                P.op(eng, lambda e: e.tensor_scalar(out=out, in0=in0, scalar1=s1, scalar2=s2, op0=op0, op1=op1, **kw), reads, writes)

        def stt(out, in0, scalar, in1, op0, op1, reads, writes, **kw):
            P.op("vector", lambda e: e.scalar_tensor_tensor(out=out, in0=in0, scalar=scalar, in1=in1, op0=op0, op1=op1, **kw), reads, writes)

        def cp(eng, out, in_, reads, writes):
            if eng == "scalar":
                P.op("scalar", lambda e: e.copy(out=out, in_=in_), reads, writes)
            else:
                P.op(eng, lambda e: e.tensor_copy(out=out, in_=in_), reads, writes)

        def dma(eng, out, in_, reads, writes):
            P.dma(eng, lambda e: e.dma_start(out=out, in_=in_), reads, writes)

        CO = 0
        CR = 600
        SM = 1752
        MOD = 2112
        PH = 8256
        cT = T()
        crT = T()
        cot = V(CO, 600)
        crt = V(CR, 1152, F32R)

        def C(off, n):
            o = off if off < 384 else 384 + off - C_IOTA
            return cot[:, o:o + n]

        dma("sync", cot[:, 0:384], const_d[:, 0:384], [], [cT])
        dma("sync", cot[:, 384:600], const_d[:, C_IOTA:NCONST], [], [cT])
        dma("gpsimd", crt[:, 0:128], const_d[:, C_ID:C_ID + 128], [], [crT])
        dma("gpsimd", crt[:, 128:1152], const_d[:, C_MD:C_MD + 1024], [], [crT])
        identF = C(C_ID, 128)
        onesF = C(C_ONE, 128)
        UF = C(C_U, 128)
        identR = crt[:, 0:128]
        maskDR = crt[:, 128:640]
        maskPR = crt[:, 640:1152]
        sm = [SM]

        def small(n):
            o = sm[0]
            sm[0] += (n + 7) // 8 * 8
            assert sm[0] <= MOD
            return V(o, n), T()

        sinkexp, sinkexpT = small(8)
        sp8, sp8T = small(4)
        ls8, ls8T = small(4)
        gates_all, gatesT = small(64)
        destf_all, destfT = small(64)
        desti_o = sm[0]; sm[0] += 64
        desti_all = V(desti_o, 64, I32); destiT = T()
        cum, cumT = small(32)
        epsc, epscT = small(1)
        P.op("vector", lambda e: e.memset(epsc, EPS), [], [epscT])

        modrow = V(MOD, 6 * D)
        modT = [T() for _ in range(6)]
        msl = lambda j: modrow[:, j * D:(j + 1) * D]
        sh1b, s1b, gt1b, sh2b, s2b, gt2b = [msl(j) for j in range(6)]

        cBs = V3(PH, 8, 128, F32R); cBT = T()
        dma("gpsimd", cBs, cB_d.rearrange("p (a b) -> p a b", a=8), [], [cBT])
        for j in range(6):
            dma("sync", msl(j), bada_d[:, j * D:(j + 1) * D], [], [modT[j]])
        WA = [V3(PH + 1024 + k * 4096, 8, 512, F32R) for k in range(2)]
        WAT = [T(), T()]
        wada_v = wada_d.rearrange("(kc p) n -> p kc n", p=128)
        for g in range(12):
            k = g % 2
            dma("gpsimd", WA[k], wada_v[:, :, g * 512:(g + 1) * 512], [], [WAT[k]])
            pb, pt = bank()
            for kc in range(8):
                mm(pb[:, :], cBs[:, kc, :], WA[k][:, kc, :], kc == 0, kc == 7, [cBT, WAT[k]], [pt])
            mt = modT[g // 2]
            sl = modrow[:, g * 512:(g + 1) * 512]
            tt("vector", sl, pb[:, :], sl, ALU.add, [pt, mt], [mt])
        gtmp = V(PH + 1024 + 8192, D); gtmpT = T()
        dma("sync", gtmp, gmix_d, [], [gtmpT])
        stt(s1b, s1b, 1.0, gtmp, ALU.add, ALU.mult, [modT[1], gtmpT], [modT[1]])
        dma("sync", gtmp, gffn_d, [], [gtmpT])
        stt(s2b, s2b, 1.0, gtmp, ALU.add, ALU.mult, [modT[4], gtmpT], [modT[4]])
        dma("sync", gt2_d, gt2b, [modT[5]], [])
        act(sinkexp, C(C_SINK, 8), AF.Exp, [cT], [sinkexpT])
        act(sp8, C(C_LAM, 4), AF.Exp, [cT], [sp8T], scale=-1.0)
        act(sp8, sp8, AF.Ln, [sp8T], [sp8T], bias=1.0)
        ts("vector", sp8, sp8, 8.0, None, ALU.mult, None, [sp8T], [sp8T])
        ts("vector", ls8, sp8, -1.0, None, ALU.mult, None, [sp8T], [ls8T])
        P.barrier()

        HT = PH
        hT = V3(HT, 8, S, F32R)
        hTT = [T() for _ in range(4)]
        XS0 = PH + 16384
        xst = [V(XS0 + k * 1024, D) for k in range(2)]; xstT = [T(), T()]
        h1t = [V(XS0 + 2048 + k * 1024, D) for k in range(2)]; h1T = [T(), T()]
        ss_, ssT = small(2)
        rs_, rsT = small(2)

        def rms_rstd(src, srcT, junk, junkT, ss, ssT_, rs, rsT_, n):
            stt(junk, src, 1.0, src, ALU.mult, ALU.mult, [srcT], [junkT, ssT_], accum_out=ss)
            act(rs, ss, AF.Ln, [ssT_], [rsT_], scale=1.0 / n, bias=epsc)
            act(rs, rs, AF.Exp, [rsT_], [rsT_], scale=-0.5)

        def transpose8(src, srcT, dstfn, dstT, k):
            for half in range(2):
                pb, pt = bank()
                for q in range(4):
                    kc = half * 4 + q
                    tr(pb[:, q * 128:(q + 1) * 128], src[:, kc * 128:(kc + 1) * 128], identF, [srcT, cT], [pt])
                dst = dstfn(half)
                cp("scalar" if (half + k) % 2 == 0 else "vector", dst, pb[:, :].rearrange("p (a b) -> p a b", a=4), [pt], [dstT])

        ssT1 = [T(), T()]; rsT1 = [T(), T()]

        def p1_s1(i):
            k = i % 2
            dma("sync", xst[k], x_d[i * 128:(i + 1) * 128, :], [], [xstT[k]])
            rms_rstd(xst[k], xstT[k], h1t[k], h1T[k], ss_[:, k:k + 1], ssT1[k], rs_[:, k:k + 1], rsT1[k], D)
            stt(h1t[k], xst[k], rs_[:, k:k + 1], s1b, ALU.mult, ALU.mult, [xstT[k], rsT1[k], modT[1]], [h1T[k]])
            tt("gpsimd", h1t[k], h1t[k], sh1b, ALU.add, [h1T[k], modT[0]], [h1T[k]])

        def p1_s2(i):
            k = i % 2
            transpose8(h1t[k], h1T[k], lambda half, i=i: hT[:, half * 4:(half + 1) * 4, i * 128:(i + 1) * 128], hTT[i // 4], k)

        for step in range(NT + 1):
            if step < NT:
                p1_s1(step)
            if step >= 1:
                p1_s2(step - 1)
        P.barrier()
        if stop_after == 1:
            return finish_debug(nc, P, dbg_d, hT.bitcast(F32).rearrange('p a b -> p (a b)'), out_d)

        WI0 = PH + 16384
        WI = [V3(WI0 + k * 1024, 8, 128, F32R) for k in range(2)]; WIT = [T(), T()]
        wi_state = [0]
        BD0 = WI0 + 2048
        bdA = V3(BD0, 4, 128, F32R); bdX = V3(BD0 + 512, 4, 128, F32R); bdT = T()
        dma("gpsimd", bdA, bda_d.rearrange("p (a b) -> p a b", a=4), [], [bdT])
        dma("gpsimd", bdX, bdx_d.rearrange("p (a b) -> p a b", a=4), [], [bdT])
        LB0 = BD0 + 1024
        LBR = [V(LB0 + k * S, S, F32R) if k == 1 else None for k in range(8)]
        LB = [LBR[k].bitcast(F32) if k == 1 else V(LB0 + k * S, S) for k in range(8)]
        LBT = [T() for _ in range(8)]
        LRU0 = LB0 + 8 * S
        assert LRU0 + 4 * S <= ARENA, LRU0 + 4 * S
        lruT = V3(LRU0, 4, S, F32R)
        lru0 = lruT.bitcast(F32)
        lruTT = [T() for _ in range(4)]
        win_v = win_d.rearrange("(kc p) n -> p kc n", p=128)
        evac_state = [0]

        def load_wi(col_specs):
            k = wi_state[0]
            wi_state[0] = 1 - k
            for (dst0, c0, n) in col_specs:
                dma("gpsimd", WI[k][:, :, dst0:dst0 + n], win_v[:, :, c0:c0 + n], [], [WIT[k]])
            return WI[k], WIT[k]

        def inproj_fm(w, wT_, dstfn, dstT):
            for tb in range(4):
                pb, pt = bank()
                for kc in range(8):
                    mm(pb[:, :], w[:, kc, :], hT[:, kc, tb * 512:(tb + 1) * 512], kc == 0, kc == 7, [wT_, hTT[tb]], [pt])
                evac_state[0] += 1
                cp("scalar" if evac_state[0] % 2 == 0 else "vector", dstfn(tb), pb[:, :], [pt], [dstT])

        cw = lambda c, k: C(C_CW + c * 4 + k, 1)
        colc = lambda base, c: C(base + c, 1)
        acc = LB[7]; accT = LBT[7]
        for c in range(4):
            w, wT_ = load_wi([(0, 768 + c * 128, 128)])
            inproj_fm(w, wT_, lambda tb: LB[0][:, tb * 512:(tb + 1) * 512], LBT[0])
            w, wT_ = load_wi([(0, 1280 + c * 128, 128)])
            inproj_fm(w, wT_, lambda tb: LB[2][:, tb * 512:(tb + 1) * 512], LBT[2])
            xr, xc, xg = LB[0], LB[1], LB[2]
            ts("vector", LBR[1], xr, cw(c, 3), colc(C_CB, c), ALU.mult, ALU.add, [LBT[0], cT], [LBT[1]])
            for sh in (1, 2, 3):
                stt(LBR[1][:, sh:], xr[:, :S - sh], cw(c, 3 - sh), xc[:, sh:], ALU.mult, ALU.add, [LBT[0], LBT[1], cT], [LBT[1]])
            for gi, (bd, bcol, dst) in enumerate(((bdA, C_BA, 3), (bdX, C_BX, 4))):
                for tb in range(4):
                    pb, pt = bank()
                    mm(pb[:, :], bd[:, c, :], LBR[1][:, tb * 512:(tb + 1) * 512], True, True, [bdT, LBT[1]], [pt])
                    act(LB[dst][:, tb * 512:(tb + 1) * 512], pb[:, :], AF.Sigmoid, [pt, cT], [LBT[dst]], bias=colc(bcol, c))
            r, ig = LB[3], LB[4]
            act(LB[6], r, AF.Tanh, [LBT[3], sp8T], [LBT[6]], scale=sp8[:, c:c + 1])
            act(LB[3], r, AF.Exp, [LBT[3], ls8T], [LBT[3]], scale=ls8[:, c:c + 1])
            a = LB[3]
            tt("gpsimd", LB[5], a, a, ALU.mult, [LBT[3]], [LBT[5]])
            stt(LB[5], LB[5], 1.0, LB[6], ALU.add, ALU.mult, [LBT[5], LBT[6]], [LBT[5]])
            act(LB[5], LB[5], AF.Sqrt, [LBT[5]], [LBT[5]])
            tt("gpsimd", LB[4], ig, xc, ALU.mult, [LBT[4], LBT[1]], [LBT[4]])
            tt("vector", LB[4], LB[4], LB[5], ALU.mult, [LBT[4], LBT[5]], [LBT[4]])
            P.op("vector", lambda e, a=a: e.tensor_tensor_scan(out=LB[0], data0=a, data1=LB[4], initial=0.0, op0=ALU.mult, op1=ALU.add),
                 [LBT[3], LBT[4], LBT[0]], [LBT[0]])
            act(LB[6], xg, AF.Square, [LBT[2]], [LBT[6]])
            ts("gpsimd", LB[6], LB[6], 0.044715, 1.0, ALU.mult, ALU.add, [LBT[6]], [LBT[6]])
            tt("gpsimd", LB[6], LB[6], xg, ALU.mult, [LBT[6], LBT[2]], [LBT[6]])
            act(LB[6], LB[6], AF.Sigmoid, [LBT[6]], [LBT[6]], scale=1.5957691216057308)
            tt("gpsimd", LB[6], LB[6], xg, ALU.mult, [LBT[6], LBT[2]], [LBT[6]])
            tt("vector", lruT[:, c, :], LB[0], LB[6], ALU.mult, [LBT[0], LBT[6]], [lruTT[c]])
            if c == 0:
                tt("gpsimd", acc, lru0[:, c, :], lru0[:, c, :], ALU.mult, [lruTT[c]], [accT])
            else:
                tt("gpsimd", LB[5], lru0[:, c, :], lru0[:, c, :], ALU.mult, [lruTT[c]], [LBT[5]])
                tt("gpsimd", acc, acc, LB[5], ALU.add, [accT, LBT[5]], [accT])
        for tb in range(4):
            pb, pt = bank()
            mm(pb[:, :], onesF, acc[:, tb * 512:(tb + 1) * 512], True, True, [cT, accT], [pt])
            act(LB[6][:, tb * 512:(tb + 1) * 512], pb[:, :], AF.Sqrt, [pt], [LBT[6]], scale=1.0 / 512, bias=EPS)
        P.op("vector", lambda e: e.reciprocal(out=LB[6], in_=LB[6]), [LBT[6]], [LBT[6]])
        for c in range(4):
            stt(lruT[:, c, :], lru0[:, c, :], colc(C_GL, c), LB[6], ALU.mult, ALU.mult, [lruTT[c], LBT[6], cT], [lruTT[c]])
        P.barrier()
        if stop_after == 2:
            return finish_debug(nc, P, dbg_d, lruT.bitcast(F32).rearrange('p a b -> p (a b)'), out_d)

        Q0 = LB0
        qT = V3(Q0, 4, S, F32R); qTT = [T() for _ in range(4)]
        kk = V3(Q0 + 4 * S, 2, S, F32R); kkT = [T(), T()]
        VA0 = Q0 + 6 * S
        vflat = V(VA0, NT * 132, F32R)
        vaug = vflat.rearrange("p (t g d) -> p t g d", t=NT, g=2)
        vT = T()
        assert VA0 + NT * 132 <= LRU0
        ones4 = onesF[:, 0:32].rearrange("p (t g d) -> p t g d", t=NT, g=2)
        cp("vector", vaug[:, :, :, 64:65], ones4, [cT], [vT])
        ts("vector", vaug[:, :, :, 65:66], ones4, 0.0, None, ALU.mult, None, [cT], [vT])
        for c in range(4):
            w, wT_ = load_wi([(0, c * 128, 128)])
            inproj_fm(w, wT_, lambda tb, c=c: qT[:, c, tb * 512:(tb + 1) * 512], qTT[c])
        for g in range(2):
            w, wT_ = load_wi([(0, 512 + g * 64, 64), (64, 512 + g * 64, 64)])
            inproj_fm(w, wT_, lambda tb, g=g: kk[:, g, tb * 512:(tb + 1) * 512], kkT[g])
        w, wT_ = load_wi([(0, 640, 128)])
        for i in range(NT):
            pb, pt = bank()
            for kc in range(8):
                mm(pb[:, 0:128], hT[:, kc, i * 128:(i + 1) * 128], w[:, kc, :], kc == 0, kc == 7, [wT_, hTT[i // 4]], [pt])
            cp("scalar" if i % 2 == 0 else "vector", vaug[:, i, :, 0:64], pb[:, 0:128].rearrange("p (g d) -> p g d", g=2), [pt], [vT])
        P.barrier()

        AT0 = PH
        attnT = V3(AT0, 4, S, F32R); attnTT = T()
        ET0 = PH + 4 * S
        eT = [[[V(ET0 + ((par * 2 + g) * 2 + kbi) * 512, 512, F32R) for kbi in range(2)] for g in range(2)] for par in range(2)]
        eTT = [[[T(), T()], [T(), T()]], [[T(), T()], [T(), T()]]]
        AN0 = ET0 + 4096
        attn_t = [V(AN0 + k * 512, 512) for k in range(2)]; attn_tT = [T(), T()]
        junk512 = V(AN0 + 1024, 512); junk512T = T()
        den2 = [small(8) for _ in range(2)]
        ssa, _ = small(2)
        rsa, _ = small(2)
        ssaT = [T(), T()]; rsaT = [T(), T()]
        scale = 0.125

        def kbs_of(n):
            return ([n - 1] if n > 0 else []) + [n]

        def at_A(n):
            par = n % 2
            for g in range(2):
                for kbi, kb in enumerate(kbs_of(n)):
                    diag = (kb == n)
                    for half in range(2):
                        pb, pt = bank()
                        mm(pb[:, 0:256], identR, (maskDR if diag else maskPR)[:, 0:256], True, False, [crT], [pt])
                        for u_ in range(2):
                            hh = u_ * 2 + half
                            c = 2 * g + hh // 2
                            h0 = half * 64
                            mm(pb[:, u_ * 128:(u_ + 1) * 128], kk[h0:h0 + 64, g, kb * 128:(kb + 1) * 128],
                               qT[h0:h0 + 64, c, n * 128:(n + 1) * 128], False, u_ == 1, [kkT[g], qTT[c]], [pt], mode=64)
                        act(eT[par][g][kbi][:, half * 256:(half + 1) * 256], pb[:, 0:256], AF.Exp, [pt], [eTT[par][g][kbi]], scale=scale)

        def at_B(n):
            par = n % 2
            k = n % 2
            kbs = kbs_of(n)
            for g in range(2):
                den, denT = den2[g]
                pb, pt = bank()
                for hh in range(4):
                    pos = (hh % 2) * 2 + hh // 2
                    for kbi, kb in enumerate(kbs):
                        mm(pb[:, hh * 66:(hh + 1) * 66], eT[par][g][kbi][:, pos * 128:(pos + 1) * 128], vaug[:, kb, g, :],
                           kbi == 0, kbi == len(kbs) - 1, [eTT[par][g][kbi], vT], [pt])
                pv = pb[:, 0:264].rearrange("p (h d) -> p h d", h=4)
                tt("vector", den[:, 0:4], pv[:, :, 64], sinkexp[:, g * 4:(g + 1) * 4], ALU.add, [pt, sinkexpT], [denT])
                P.op("vector", lambda e, den=den: e.reciprocal(out=den[:, 0:4], in_=den[:, 0:4]), [denT], [denT])
                for hh in range(4):
                    hd = g * 4 + hh
                    if hh % 2 == 0:
                        ts("vector", attn_t[k][:, hd * 64:(hd + 1) * 64], pv[:, hh, 0:64], den[:, hh:hh + 1], None, ALU.mult, None, [pt, denT], [attn_tT[k]])
                    else:
                        act(attn_t[k][:, hd * 64:(hd + 1) * 64], pv[:, hh, 0:64], AF.Copy, [pt, denT], [attn_tT[k]], scale=den[:, hh:hh + 1])
            rms_rstd(attn_t[k], attn_tT[k], junk512, junk512T, ssa[:, k:k + 1], ssaT[k], rsa[:, k:k + 1], rsaT[k], 512)
            ts("vector", attn_t[k], attn_t[k], rsa[:, k:k + 1], None, ALU.mult, None, [attn_tT[k], rsaT[k]], [attn_tT[k]])
            pb, pt = bank()
            for c in range(4):
                tr(pb[:, c * 128:(c + 1) * 128], attn_t[k][:, c * 128:(c + 1) * 128], identF, [attn_tT[k], cT], [pt])
            for c in range(4):
                if c % 2 == 0:
                    ts("vector", attnT[:, c, n * 128:(n + 1) * 128], pb[:, c * 128:(c + 1) * 128], colc(C_GA, c), None, ALU.mult, None, [pt, cT], [attnTT])
                else:
                    act(attnT[:, c, n * 128:(n + 1) * 128], pb[:, c * 128:(c + 1) * 128], AF.Copy, [pt, cT], [attnTT], scale=colc(C_GA, c))

        at_A(0)
        for n in range(NT):
            if n + 1 < NT:
                at_A(n + 1)
            at_B(n)
        P.barrier()
        if stop_after == 3:
            return finish_debug(nc, P, dbg_d, attnT.bitcast(F32).rearrange('p a b -> p (a b)'), out_d)

        WO0 = PH + 4 * S
        wo = V3(WO0, 8, D, F32R); woT = T()
        wout_v = wout_d.rearrange("(kc p) n -> p kc n", p=128)
        for hf in range(2):
            dma("gpsimd", wo[:, :, hf * 512:(hf + 1) * 512], wout_v[:, :, hf * 512:(hf + 1) * 512], [], [woT])
        T0 = WO0 + 8 * D
        xt = [V(T0 + k * D, D) for k in range(2)]; xtT = [T(), T()]
        x1t = [V(T0 + 2 * D + k * D, D) for k in range(2)]; x1T = [T(), T()]
        h2t = [V(T0 + 4 * D + k * D, D) for k in range(2)]; h2T_ = [T(), T()]
        h2Tr = [V3(T0 + 6 * D + k * D, 8, 128) for k in range(2)]; h2TrT = [T(), T()]
        WR0 = T0 + 8 * D
        wr = V3(WR0, 8, E); wrT = T()
        dma("sync", wr, wr_d.rearrange("(kc p) n -> p kc n", p=128), [], [wrT])
        R0 = WR0 + 8 * E
        assert R0 + 1024 <= LB0 + 8 * S
        rsm = [R0]

        def rsmall(n):
            o = rsm[0]
            rsm[0] += (n + 7) // 8 * 8
            return V(o, n), T()

        lg, lgT = rsmall(32)
        top8, top8T = rsmall(8)
        idx8_o = rsm[0]; rsm[0] += 8
        idx8 = V(idx8_o, 8, U32); idx8T = T()
        idxf, idxfT = rsmall(4)
        negm, negmT = rsmall(1)
        e4, e4T = rsmall(4)
        ssum, ssumT = rsmall(1)
        mask, maskT = rsmall(32)
        rkp, rkpT = rsmall(32)
        ohj, ohjT = rsmall(32)
        ss2, _ = rsmall(2)
        rs2, _ = rsmall(2)
        xsT_d = T()
        x1dT = T()
        mergedT = lambda kc: (attnT[:, kc, :] if kc < 4 else lruT[:, kc - 4, :])
        mT = lambda kc: (attnTT if kc < 4 else lruTT[kc - 4])
        ss2T = [T(), T()]; rs2T = [T(), T()]
        gatesTi = [T() for _ in range(NT)]; destfTi = [T() for _ in range(NT)]; destiTi = [T() for _ in range(NT)]

        def p3_s1(i):
            k = i % 2
            dma("sync", xt[k], x_d[i * 128:(i + 1) * 128, :], [], [xtT[k]])
            for hf in range(2):
                pb, pt = bank()
                for kc in range(8):
                    mm(pb[:, :], mergedT(kc)[:, i * 128:(i + 1) * 128], wo[:, kc, hf * 512:(hf + 1) * 512], kc == 0, kc == 7, [mT(kc), woT], [pt])
                sl = slice(hf * 512, (hf + 1) * 512)
                tt("vector", x1t[k][:, sl], pb[:, :], gt1b[:, sl], ALU.mult, [pt, modT[2]], [x1T[k]])
                tt("gpsimd", x1t[k][:, sl], x1t[k][:, sl], xt[k][:, sl], ALU.add, [x1T[k], xtT[k]], [x1T[k]])
            dma("sync", x1_d[i * 128:(i + 1) * 128, :], x1t[k], [x1T[k]], [])
            rms_rstd(x1t[k], x1T[k], h2t[k], h2T_[k], ss2[:, k:k + 1], ss2T[k], rs2[:, k:k + 1], rs2T[k], D)
            stt(h2t[k], x1t[k], rs2[:, k:k + 1], s2b, ALU.mult, ALU.mult, [x1T[k], rs2T[k], modT[4]], [h2T_[k]])
            tt("gpsimd", h2t[k], h2t[k], sh2b, ALU.add, [h2T_[k], modT[3]], [h2T_[k]])

        def p3_s2(i):
            k = i % 2
            transpose8(h2t[k], h2T_[k], lambda half, k=k: h2Tr[k][:, half * 4:(half + 1) * 4, :], h2TrT[k], k)
            pb, pt = bank()
            for kc in range(8):
                mm(pb[:, 0:E], h2Tr[k][:, kc, :], wr[:, kc, :], kc == 0, kc == 7, [h2TrT[k], wrT], [pt])
            tt("vector", lg, pb[:, 0:E], C(C_BR, E), ALU.add, [pt, cT], [lgT])
            P.op("vector", lambda e: e.max(out=top8, in_=lg), [lgT], [top8T])
            P.op("vector", lambda e: e.max_index(out=idx8, in_max=top8, in_values=lg), [lgT, top8T], [idx8T])
            cp("vector", idxf, idx8[:, 0:4], [idx8T], [idxfT])
            ts("vector", negm, top8[:, 0:1], -1.0, None, ALU.mult, None, [top8T], [negmT])
            act(e4, top8[:, 0:4], AF.Exp, [top8T, negmT], [e4T, ssumT], bias=negm, accum_out=ssum)
            P.op("vector", lambda e: e.reciprocal(out=ssum, in_=ssum), [ssumT], [ssumT])
            ts("vector", gates_all[:, i * 4:(i + 1) * 4], e4, ssum, None, ALU.mult, None, [e4T, ssumT], [gatesTi[i]])
            ts("vector", mask, lg, top8[:, 3:4], None, ALU.is_ge, None, [lgT, top8T], [maskT])
            pb, pt = bank()
            if i > 0:
                mm(pb[:, 0:E], onesF, cum, True, False, [cT, cumT], [pt])
            mm(pb[:, 0:E], UF, mask, i == 0, True, [cT, maskT], [pt])
            tt("vector", rkp, pb[:, 0:E], C(C_RB, E), ALU.add, [pt, cT], [rkpT])
            if i == 0:
                cp("vector", cum, mask, [maskT], [cumT])
            else:
                tt("vector", cum, cum, mask, ALU.add, [cumT, maskT], [cumT])
            for j in range(4):
                stt(ohj, C(C_IOTA, E), idxf[:, j:j + 1], rkp, ALU.is_equal, ALU.mult, [cT, idxfT, rkpT], [ohjT, destfTi[i]],
                    accum_out=destf_all[:, i * 4 + j:i * 4 + j + 1])
            cp("vector", desti_all[:, i * 4:(i + 1) * 4], destf_all[:, i * 4:(i + 1) * 4], [destfTi[i]], [destiTi[i]])

        def p3_s3(i):
            k = i % 2
            for j in range(4):
                P.dma("gpsimd", lambda e, i=i, j=j, k=k: e.indirect_dma_start(
                    out=xs_d, out_offset=bass.IndirectOffsetOnAxis(ap=desti_all[:, i * 4 + j:i * 4 + j + 1], axis=0),
                    in_=h2t[k], in_offset=None), [h2T_[k], destiTi[i]], [])

        for step in range(NT + 1):
            if step < NT:
                p3_s1(step)
            if step >= 1:
                p3_s2(step - 1)
                p3_s3(step - 1)
        cntb, cntbT = rsmall(32)
        nblk, nblkT = rsmall(32)
        pend, pendT = rsmall(32)
        pst, pstT = rsmall(32)
        ej, ejT = rsmall(NB)
        pstj, pstjT = rsmall(NB)
        tmpj, tmpjT = rsmall(NB)
        sj, sjT = rsmall(NB)
        rowj, rowjT = rsmall(NB)
        valid, validT = rsmall(NB)
        skp, skpT = rsmall(NB)
        ebW, ebWT = rsmall(NB)
        ebD, ebDT = rsmall(NB)
        idxW = V(MOD, NB * 8, I32).rearrange("p (j k) -> p j k", k=8)
        idxD = V(MOD + NB * 8, NB * 4, I32).rearrange("p (j k) -> p j k", k=4)
        idxX = V(MOD + NB * 12, NB * 2, I32).rearrange("p (j k) -> p j k", k=2)
        idxB = V(MOD + NB * 14, NB, I32)
        idxBd = V(MOD + NB * 15, NB, I32)
        tblT = T()
        jrow = C(C_J, NB)
        pid = C(C_PID, 1)
        pb, pt = bank()
        mm(pb[:, 0:E], onesF, cum, True, True, [cT, cumT], [pt])
        cp("vector", cntb, pb[:, 0:E], [pt], [cntbT])
        ts("vector", nblk, cntb, 0.0, None, ALU.is_gt, None, [cntbT], [nblkT])
        for sx in range(1, EREG // BSZ):
            stt(nblk, cntb, float(BSZ * sx), nblk, ALU.is_gt, ALU.add, [cntbT, nblkT], [nblkT])
        P.op("vector", lambda e: e.tensor_tensor_scan(out=pend, data0=onesF[:, 0:E], data1=nblk, initial=0.0, op0=ALU.mult, op1=ALU.add),
             [cT, nblkT], [pendT])
        tt("vector", pst, pend, nblk, ALU.subtract, [pendT, nblkT], [pstT])
        ts("vector", ej, jrow, pend[:, 0:1], None, ALU.is_ge, None, [cT, pendT], [ejT])
        for e_ in range(1, E):
            stt(ej, jrow, pend[:, e_:e_ + 1], ej, ALU.is_ge, ALU.add, [cT, pendT, ejT], [ejT])
        ts("vector", valid, ej, E - 0.5, None, ALU.is_lt, None, [ejT], [validT])
        ts("vector", ej, ej, float(E - 1), None, ALU.min, None, [ejT], [ejT])
        for e_ in range(E):
            ts("vector", tmpj, ej, float(e_), None, ALU.is_equal, None, [ejT], [tmpjT])
            if e_ == 0:
                ts("vector", pstj, tmpj, pst[:, 0:1], None, ALU.mult, None, [tmpjT, pstT], [pstjT])
            else:
                stt(pstj, tmpj, pst[:, e_:e_ + 1], pstj, ALU.mult, ALU.add, [tmpjT, pstT, pstjT], [pstjT])
        tt("vector", sj, jrow, pstj, ALU.subtract, [cT, pstjT], [sjT])
        ts("vector", skp, sj, 0.0, None, ALU.is_equal, None, [sjT], [skpT])
        ts("vector", skp, skp, -BIG, BIG, ALU.mult, ALU.add, [skpT], [skpT])
        ts("vector", tmpj, sj, float(BSZ), None, ALU.mult, None, [sjT], [tmpjT])
        stt(rowj, ej, float(EREG), tmpj, ALU.mult, ALU.add, [ejT, tmpjT], [rowjT])
        ts("vector", rowj, rowj, -BIG, None, ALU.add, None, [rowjT], [rowjT])
        tt("vector", rowj, rowj, valid, ALU.mult, [rowjT, validT], [rowjT])
        ts("vector", rowj, rowj, BIG, pid, ALU.add, ALU.add, [rowjT, cT], [rowjT])
        stt(ebW, ej, 1024.0, skp, ALU.mult, ALU.add, [ejT, skpT], [ebWT])
        ts("vector", ebW, ebW, pid, None, ALU.add, None, [ebWT, cT], [ebWT])
        stt(ebD, ej, 512.0, skp, ALU.mult, ALU.add, [ejT, skpT], [ebDT])
        ts("vector", ebD, ebD, pid, None, ALU.add, None, [ebDT, cT], [ebDT])
        for k_ in range(8):
            ts("vector", idxW[:, :, k_], ebW, float(k_ * 128), None, ALU.add, None, [ebWT], [tblT])
        for k_ in range(4):
            ts("vector", idxD[:, :, k_], ebD, float(k_ * 128), None, ALU.add, None, [ebDT], [tblT])
        for k_ in range(2):
            ts("vector", idxX[:, :, k_], rowj, float(k_ * 128), None, ALU.add, None, [rowjT], [tblT])
        ts("vector", idxB, ej, 128.0, pid, ALU.mult, ALU.add, [ejT, cT], [tblT])
        cp("vector", idxBd, ej, [ejT], [tblT])
        P.barrier()
        if stop_after == 4:
            return finish_debug(nc, P, dbg_d, None, out_d)

        WSg = [V3(PH + k * 2048, 4, 512, F32R) for k in range(8)]; WSgT = [T() for _ in range(8)]
        WSd = [V3(PH + (8 + k) * 2048, 4, 512, F32R) for k in range(4)]; WSdT = [T() for _ in range(4)]
        X0 = PH + 12 * 2048
        xsb = [V3(X0 + k * NJ * D, NJ, D) for k in range(2)]; xsbT = [T(), T()]
        XT0 = X0 + 2 * NJ * D
        xsT = [V3(XT0 + k * 8 * BSZ, 8, BSZ, F32R) for k in range(2)]; xsTT = [T(), T()]
        A0 = XT0 + 2 * 8 * BSZ
        actT = V3(A0, 8, BSZ, F32R); actTT = T()
        Y0 = A0 + 8 * BSZ
        yb = [V3(Y0 + k * NJ * D, NJ, D) for k in range(2)]; ybT = [T(), T()]
        G0 = Y0 + 2 * NJ * D
        gsb = [V(G0 + k * BSZ, BSZ) for k in range(6)]; gsbT = [T() for _ in range(6)]
        BG0 = G0 + 6 * BSZ
        bgub = [V(BG0 + k * 16, 16) for k in range(2)]; bgubT = [T(), T()]
        BD0_ = BG0 + 32
        bdnb = [V(BD0_ + k * D, D) for k in range(2)]; bdnbT = [T(), T()]
        assert BD0_ + 2 * D <= ARENA, BD0_ + 2 * D

        bregs = {}

        def breg(e, bound):
            if bound not in bregs:
                bregs[bound] = e.to_reg(bound)
            return bregs[bound]

        def igather(out, src, idx_ap, bound, reads, writes):
            P.dma("gpsimd", lambda e: e.indirect_dma_start(out=out, out_offset=None, in_=src,
                  in_offset=bass.IndirectOffsetOnAxis(ap=idx_ap, axis=0), bounds_check=breg(e, bound), oob_is_err=False), reads, writes)

        def load_block_small(j):
            k = j % 2
            for jj in range(NJ):
                igather(xsb[k][:, jj, :], xs_d, idxX[:, j, jj:jj + 1], NSLOT - 1, [tblT], [xsbT[k]])
            igather(bgub[k], bgur_d, idxB[:, j:j + 1], E * 128 - 1, [tblT], [bgubT[k]])
            igather(bdnb[k], bdn_d, idxBd[:, j:j + 1], E - 1, [tblT], [bdnbT[k]])

        def load_wg(j, k_):
            igather(WSg[k_].rearrange("p a b -> p (a b)"), wgur_d, idxW[:, j, k_:k_ + 1], E * 1024 - 1, [tblT], [WSgT[k_]])

        def load_wd(j, k_):
            igather(WSd[k_].rearrange("p a b -> p (a b)"), wdnr_d, idxD[:, j, k_:k_ + 1], E * 512 - 1, [tblT], [WSdT[k_]])

        load_block_small(0)
        for k_ in range(8):
            load_wg(0, k_)
        for k_ in range(4):
            load_wd(0, k_)
        for j in range(NB):
            k = j % 2
            for kc in range(8):
                pb, pt = bank()
                for jj in range(NJ):
                    tr(pb[:, jj * 128:(jj + 1) * 128], xsb[k][:, jj, kc * 128:(kc + 1) * 128], identF, [xsbT[k], cT], [pt])
                cp("scalar" if kc % 2 == 0 else "vector", xsT[k][:, kc, :], pb[:, 0:BSZ], [pt], [xsTT[k]])
            if j + 1 < NB:
                load_block_small(j + 1)
            for fc in range(8):
                qg = (fc // 4) * 2
                lc = (fc % 4) * 128
                pg, pgt = bank()
                for kc in range(8):
                    wi_ = qg * 2 + kc // 4
                    mm(pg[:, 0:BSZ], WSg[wi_][:, kc % 4, lc:lc + 128], xsT[k][:, kc, :], kc == 0, kc == 7, [WSgT[wi_], xsTT[k]], [pgt])
                pu, put = bank()
                for kc in range(8):
                    wi_ = (qg + 1) * 2 + kc // 4
                    mm(pu[:, 0:BSZ], WSg[wi_][:, kc % 4, lc:lc + 128], xsT[k][:, kc, :], kc == 0, kc == 7, [WSgT[wi_], xsTT[k]], [put])
                if fc % 4 == 3 and j + 1 < NB:
                    for k_ in range(qg * 2, qg * 2 + 4):
                        load_wg(j + 1, k_)
                g3 = (fc % 2) * 3
                gb, gbT = gsb[g3], gsbT[g3]
                ub, ubT = gsb[g3 + 1], gsbT[g3 + 1]
                sl_, slT = gsb[g3 + 2], gsbT[g3 + 2]
                ts("vector", gb, pg[:, 0:BSZ], bgub[k][:, fc:fc + 1], 7.0, ALU.add, ALU.min, [pgt, bgubT[k]], [gbT])
                ts("vector", ub, pu[:, 0:BSZ], bgub[k][:, 8 + fc:8 + fc + 1], 7.0, ALU.add, ALU.min, [put, bgubT[k]], [ubT])
                ts("vector", ub, ub, -7.0, 1.0, ALU.max, ALU.add, [ubT], [ubT])
                act(sl_, gb, AF.Silu, [gbT], [slT], scale=1.702)
                stt(actT[:, fc, :], sl_, 1.0 / 1.702, ub, ALU.mult, ALU.mult, [slT, ubT], [actTT])
            for hf in range(2):
                for jj in range(NJ):
                    pb, pt = bank()
                    for fc in range(8):
                        wi_ = hf * 2 + fc // 4
                        mm(pb[:, :], actT[:, fc, jj * 128:(jj + 1) * 128], WSd[wi_][:, fc % 4, :], fc == 0, fc == 7, [actTT, WSdT[wi_]], [pt])
                    sl = slice(hf * 512, (hf + 1) * 512)
                    tt("vector", yb[k][:, jj, sl], pb[:, :], bdnb[k][:, sl], ALU.add, [pt, bdnbT[k]], [ybT[k]])
                if j + 1 < NB:
                    load_wd(j + 1, hf * 2)
                    load_wd(j + 1, hf * 2 + 1)
            for jj in range(NJ):
                P.dma("gpsimd", lambda e, j=j, jj=jj, k=k: e.indirect_dma_start(
                    out=ys_d, out_offset=bass.IndirectOffsetOnAxis(ap=idxX[:, j, jj:jj + 1], axis=0),
                    in_=yb[k][:, jj, :], in_offset=None, bounds_check=breg(e, NSLOT - 1), oob_is_err=False), [ybT[k], tblT], [])
        P.barrier()
        if stop_after == 5:
            return finish_debug(nc, P, dbg_d, None, out_d)

        gt2s = V(PH, D); gt2sT = T()
        gfin = V(PH + D, D); gfinT = T()
        dma("sync", gt2s, gt2_d, [], [gt2sT])
        dma("sync", gfin, gfin_d, [], [gfinT])
        F0 = PH + 2 * D
        x1r = [V(F0 + k * D, D) for k in range(2)]; x1rT = [T(), T()]
        yg = [[V(F0 + 2 * D + (k * 4 + j) * D, D) for j in range(4)] for k in range(2)]
        ygT = [[T() for _ in range(4)] for _ in range(2)]
        ob = [V(F0 + 10 * D + k * D, D) for k in range(2)]; obT = [T(), T()]
        ss3, _ = small(2)
        rs3, _ = small(2)
        ss3T = [T(), T()]; rs3T = [T(), T()]

        def p5_s1(i):
            k = i % 2
            dma("sync", x1r[k], x1_d[i * 128:(i + 1) * 128, :], [], [x1rT[k]])
            for j in range(4):
                P.dma("gpsimd", lambda e, i=i, j=j, k=k: e.indirect_dma_start(
                    out=yg[k][j], out_offset=None, in_=ys_d,
                    in_offset=bass.IndirectOffsetOnAxis(ap=desti_all[:, i * 4 + j:i * 4 + j + 1], axis=0)), [destiTi[i]], [ygT[k][j]])

        def p5_s2(i):
            k = i % 2
            ts("vector", ob[k], yg[k][0], gates_all[:, i * 4:i * 4 + 1], None, ALU.mult, None, [ygT[k][0], gatesTi[i]], [obT[k]])
            for j in range(1, 4):
                stt(ob[k], yg[k][j], gates_all[:, i * 4 + j:i * 4 + j + 1], ob[k], ALU.mult, ALU.add, [ygT[k][j], gatesTi[i], obT[k]], [obT[k]])
            tt("gpsimd", ob[k], ob[k], gt2s, ALU.mult, [obT[k], gt2sT], [obT[k]])
            tt("vector", x1r[k], x1r[k], ob[k], ALU.add, [x1rT[k], obT[k]], [x1rT[k]])

        def p5_s3(i):
            k = i % 2
            rms_rstd(x1r[k], x1rT[k], ob[k], obT[k], ss3[:, k:k + 1], ss3T[k], rs3[:, k:k + 1], rs3T[k], D)
            stt(ob[k], x1r[k], rs3[:, k:k + 1], gfin, ALU.mult, ALU.mult, [x1rT[k], rs3T[k], gfinT], [obT[k]])
            dma("sync", out_d[i * 128:(i + 1) * 128, :], ob[k], [obT[k]], [])

        p5_s1(0)
        for step in range(NT + 1):
            if step >= 1:
                p5_s3(step - 1)
            if step + 1 < NT:
                p5_s1(step + 1)
            if step < NT:
                p5_s2(step)
        P.barrier()
        P.emit()
    return nc


def finish_debug(nc, P, dbg_d, src, out_d):
    if src is not None:
        n = src.shape[1]
        P.dma("sync", lambda e: e.dma_start(out=dbg_d[:, 0:n], in_=src), [], [])
    P.barrier()
    P.emit()
    return nc


def host_consts(inp):
    c = np.zeros((128, NCONST), np.float32)
    c[:, C_ID:C_ID + 128] = np.eye(128, dtype=np.float32)
    c[:, C_ONE:C_ONE + 128] = 1.0
    kk_, qq = np.meshgrid(np.arange(128), np.arange(128), indexing="ij")
    c[:, C_U:C_U + 128] = (kk_ < qq).astype(np.float32)
    md = np.where(kk_ <= qq, 0.0, NEG).astype(np.float32)
    mp = np.where(kk_ > qq, 0.0, NEG).astype(np.float32)
    c[:, C_MD:C_MD + 512] = np.tile(md, (1, 4))
    c[:, C_MP:C_MP + 512] = np.tile(mp, (1, 4))
    c[:, C_IOTA:C_IOTA + 32] = np.arange(32, dtype=np.float32)[None, :]
    c[:, C_RB:C_RB + 32] = (np.arange(32, dtype=np.float32) * EREG)[None, :]
    c[:, C_PID] = np.arange(128, dtype=np.float32)
    c[:, C_J:C_J + NB] = np.arange(NB, dtype=np.float32)[None, :]
    c[:, C_BR:C_BR + 32] = inp["b_router"][0][None, :]
    c[:, C_SINK:C_SINK + 8] = inp["sinks"][0][None, :]
    cwt = inp["conv_w"][0]
    c[:, C_CW:C_CW + 16] = cwt.T.reshape(4, 128, 4).transpose(1, 0, 2).reshape(128, 16)
    col = lambda v: np.asarray(v, np.float32).reshape(4, 128).T
    c[:, C_CB:C_CB + 4] = col(inp["conv_b"][0])
    c[:, C_BA:C_BA + 4] = col(inp["b_rg_a"][0].reshape(512))
    c[:, C_BX:C_BX + 4] = col(inp["b_rg_x"][0].reshape(512))
    c[:, C_LAM:C_LAM + 4] = col(inp["lru_lambda"][0])
    c[:, C_GA:C_GA + 4] = col(inp["g_attn_out"][0])
    c[:, C_GL:C_GL + 4] = col(inp["g_lru_out"][0])
    return c


def host_blockdiag(w):
    bd = np.zeros((128, 4, 128), np.float32)
    for c in range(4):
        bd[0:64, c, 0:64] = w[2 * c]
        bd[64:128, c, 64:128] = w[2 * c + 1]
    return bd.reshape(128, 512)


def host_wgu(w):
    w6 = w.reshape(E, 2, 4, 128, 4, 512)
    cg = [0, 2, 1, 3]
    out = np.empty((E, 4, 2, 128, 4, 512), np.float32)
    for q in range(4):
        out[:, q] = w6[:, :, :, :, cg[q], :].transpose(0, 1, 3, 2, 4)
    return out.reshape(E * 1024, 2048)


def host_wdn(w):
    w6 = w.reshape(E, 2, 4, 128, 2, 512)
    out = np.ascontiguousarray(w6.transpose(0, 4, 1, 3, 2, 5))
    return out.reshape(E * 512, 2048)


def make_in_maps(inp):
    inp = {k: np.asarray(v) for k, v in inp.items()}
    bc = lambda v: np.ascontiguousarray(np.broadcast_to(np.asarray(v, np.float32)[None, :], (128, v.shape[0])))
    shared = {
        "w_ada": np.ascontiguousarray(inp["w_ada"][0]),
        "b_ada_b": bc(inp["b_ada"][0]),
        "g_mix_b": bc(inp["g_mix"][0]),
        "g_ffn_b": bc(inp["g_ffn"][0]),
        "g_final_b": bc(inp["g_final"]),
        "w_in": np.ascontiguousarray(inp["w_in"][0]),
        "bd_a": host_blockdiag(inp["w_rg_a"][0]),
        "bd_x": host_blockdiag(inp["w_rg_x"][0]),
        "w_out": np.ascontiguousarray(inp["w_out"][0]),
        "w_router": np.ascontiguousarray(inp["w_router"][0]),
        "w_gu_r": host_wgu(inp["w_gu"][0]),
        "w_dn_r": host_wdn(inp["w_down"][0]),
        "b_gu_r": np.ascontiguousarray(inp["b_gu"][0].reshape(E, 16, 128).transpose(0, 2, 1).reshape(E * 128, 16)),
        "b_down": np.ascontiguousarray(inp["b_down"][0]),
        "consts": host_consts(inp),
    }
    maps = []
    for b in range(8):
        m = dict(shared)
        m["x"] = np.ascontiguousarray(inp["x"][b])
        cb = inp["c"][b].reshape(8, 128).T
        m["cB"] = np.ascontiguousarray(np.broadcast_to(cb[:, :, None], (128, 8, 128)).reshape(128, 1024)).astype(np.float32)
        maps.append(m)
    return maps


def kernel(**inputs):
    nc = build()
    maps = make_in_maps(inputs)
    res = run_bass_kernel_spmd(nc, maps, core_ids=list(range(8)))
    return np.stack([np.asarray(r["out"]) for r in res.results], axis=0).astype(np.float32)
```
